# Optimizing a Trainium2 kernel written in Bass

```python
import math
import jax, jax.numpy as jnp
from jax import lax
import numpy as np

D_MODEL = 1024
BATCH = 8
SEQ = 8192
DEPTH = 2

DIFF_QK_DIM = 64
DIFF_V_DIM = 2 * DIFF_QK_DIM
N_DIFF_HEADS = (D_MODEL // 2) // DIFF_V_DIM
DIFF_WIDTH = N_DIFF_HEADS * DIFF_V_DIM
SG_GROUP_DIM = 128
N_SG_GROUPS = (D_MODEL // 2) // SG_GROUP_DIM
SG_WIDTH = N_SG_GROUPS * SG_GROUP_DIM
MIX_WIDTH = DIFF_WIDTH + SG_WIDTH
CHUNK = 128
QK_COLS = N_DIFF_HEADS * 2 * DIFF_QK_DIM
IN_COLS = 2 * QK_COLS + DIFF_WIDTH + 2 * SG_WIDTH
ROPE_THETA = 10000.0
Q_BLOCK = 128
N_EXPERT_GROUPS = 4
EXPERTS_PER_GROUP = 8
N_EXPERTS = N_EXPERT_GROUPS * EXPERTS_PER_GROUP
TOP_K_IN_GROUP = 2
D_EXPERT = 512
DEEPNORM_ALPHA = (2.0 * DEPTH) ** 0.25
DEEPNORM_BETA = (8.0 * DEPTH) ** -0.25
LN_EPS = 1e-5
ADA_SCALE = 0.1

kernel_name = "hymba_diffattn_sgmlp_hmoe_deepnorm"


def layer_norm(x, g, b):
    xf = x.astype(jnp.float32)
    mu = jnp.mean(xf, axis=-1, keepdims=True)
    var = jnp.mean(jnp.square(xf - mu), axis=-1, keepdims=True)
    return ((xf - mu) * lax.rsqrt(var + LN_EPS) * g + b).astype(x.dtype)


def rms_norm(x, g):
    xf = x.astype(jnp.float32)
    return (xf * lax.rsqrt(jnp.mean(jnp.square(xf), axis=-1, keepdims=True) + LN_EPS) * g).astype(x.dtype)


def rope_tables(seq, dim):
    inv = 1.0 / (ROPE_THETA ** (jnp.arange(0, dim, 2, dtype=jnp.float32) / dim))
    ang = jnp.arange(seq, dtype=jnp.float32)[:, None] * inv[None, :]
    return jnp.cos(ang), jnp.sin(ang)


def apply_rope(t, cos, sin):
    t1, t2 = jnp.split(t, 2, axis=-1)
    c = cos[:, None, None, :].astype(t.dtype)
    s = sin[:, None, None, :].astype(t.dtype)
    return jnp.concatenate([t1 * c - t2 * s, t2 * c + t1 * s], axis=-1)


def diff_attention(q, k, v, lam):
    B, S, H, _, dk = q.shape
    nb = S // Q_BLOCK
    qb = (q * (dk ** -0.5)).reshape(B, nb, Q_BLOCK, H, 2, dk).transpose(1, 0, 2, 3, 4, 5)
    kpos = jnp.arange(S)

    def one_block(args):
        qblk, i = args
        s = jnp.einsum('bqhmd,bkhmd->bhmqk', qblk, k).astype(jnp.float32)
        qpos = i * Q_BLOCK + jnp.arange(Q_BLOCK)
        s = jnp.where(kpos[None, :] <= qpos[:, None], s, -jnp.inf)
        p = jax.nn.softmax(s, axis=-1)
        a = p[:, :, 0] - lam * p[:, :, 1]
        return jnp.einsum('bhqk,bkhd->bqhd', a.astype(v.dtype), v)

    out = lax.map(one_block, (qb, jnp.arange(nb)))
    return out.transpose(1, 0, 2, 3, 4).reshape(B, S, H, v.shape[-1])


def chunk_spatial_gate(zu, zv, ln_g, ln_b, w_s, b_s):
    B, S, G, C = zv.shape
    zv = layer_norm(zv, ln_g, ln_b)
    causal = jnp.tril(jnp.ones((CHUNK, CHUNK), dtype=bool))
    w = jnp.where(causal[None], w_s, jnp.zeros_like(w_s))
    zc = zv.reshape(B, S // CHUNK, CHUNK, G, C)
    mixed = jnp.einsum('gts,bnsgc->bntgc', w, zc) + b_s.T[None, None, :, :, None]
    return zu * mixed.reshape(B, S, G, C)


def hier_moe(h, w_grp, b_grp, w_rt, b_rt, w_gate, w_up, w_down):
    B, S, D = h.shape
    t = h.reshape(-1, D)
    T = t.shape[0]
    grp_logits = (t @ w_grp + b_grp).astype(jnp.float32)
    grp_prob = jax.nn.softmax(grp_logits, axis=-1)
    g_idx = jnp.argmax(grp_logits, axis=-1)
    g_p = jnp.take_along_axis(grp_prob, g_idx[:, None], axis=-1)[:, 0]
    exp_logits = (jnp.einsum('td,gde->tge', t, w_rt) + b_rt).astype(jnp.float32)
    sel = jnp.take_along_axis(exp_logits, g_idx[:, None, None], axis=1)[:, 0]
    top_v, top_i = lax.top_k(sel, TOP_K_IN_GROUP)
    top_w = jax.nn.softmax(top_v, axis=-1)
    within = jnp.sum(jax.nn.one_hot(top_i, EXPERTS_PER_GROUP, dtype=jnp.float32) * top_w[..., None], axis=1)
    grp_w = jax.nn.one_hot(g_idx, N_EXPERT_GROUPS, dtype=jnp.float32) * g_p[:, None]
    combine = (grp_w[:, :, None] * within[:, None, :]).reshape(T, N_EXPERTS).astype(t.dtype)
    out = jnp.zeros_like(t)
    for e in range(N_EXPERTS):
        a = jax.nn.silu(t @ w_gate[e]) * (t @ w_up[e])
        out = out + combine[:, e:e + 1] * (a @ w_down[e])
    return out.reshape(B, S, D)


def setup_inputs(seed: int = 0) -> dict:
    key = jax.random.key(seed)
    ks = jax.random.split(key, 32)
    n = jax.random.normal
    f32 = jnp.float32
    D, L = D_MODEL, DEPTH
    w_in = n(ks[2], (L, D, IN_COLS), f32) * D ** -0.5
    v0, v1 = 2 * QK_COLS, 2 * QK_COLS + DIFF_WIDTH + SG_WIDTH
    col_scale = jnp.ones((IN_COLS,), f32).at[v0:v1].set(DEEPNORM_BETA)
    w_in = w_in * col_scale
    return {
        "x": n(ks[0], (BATCH, SEQ, D), f32),
        "c": n(ks[1], (BATCH, D), f32),
        "w_ada": n(ks[3], (L, D, 6 * D), f32) * (D ** -0.5) * ADA_SCALE,
        "b_ada": n(ks[4], (L, 6 * D), f32) * 0.02,
        "w_in": w_in,
        "lambda_q1": n(ks[5], (L, DIFF_QK_DIM), f32) * 0.1,
        "lambda_k1": n(ks[6], (L, DIFF_QK_DIM), f32) * 0.1,
        "lambda_q2": n(ks[7], (L, DIFF_QK_DIM), f32) * 0.1,
        "lambda_k2": n(ks[8], (L, DIFF_QK_DIM), f32) * 0.1,
        "subln_g": 1.0 + 0.02 * n(ks[9], (L, DIFF_V_DIM), f32),
        "sg_ln_g": 1.0 + 0.02 * n(ks[10], (L, N_SG_GROUPS, SG_GROUP_DIM), f32),
        "sg_ln_b": 0.02 * n(ks[11], (L, N_SG_GROUPS, SG_GROUP_DIM), f32),
        "w_spatial": n(ks[12], (L, N_SG_GROUPS, CHUNK, CHUNK), f32) * CHUNK ** -0.5,
        "b_spatial": 1.0 + 0.02 * n(ks[13], (L, N_SG_GROUPS, CHUNK), f32),
        "w_out": n(ks[14], (L, MIX_WIDTH, D), f32) * (MIX_WIDTH ** -0.5) * DEEPNORM_BETA,
        "ln1_g": 1.0 + 0.02 * n(ks[15], (L, D), f32),
        "ln1_b": 0.02 * n(ks[16], (L, D), f32),
        "w_group": n(ks[17], (L, D, N_EXPERT_GROUPS), f32) * D ** -0.5,
        "b_group": 0.01 * n(ks[18], (L, N_EXPERT_GROUPS), f32),
        "w_router": n(ks[19], (L, N_EXPERT_GROUPS, D, EXPERTS_PER_GROUP), f32) * D ** -0.5,
        "b_router": 0.01 * n(ks[20], (L, N_EXPERT_GROUPS, EXPERTS_PER_GROUP), f32),
        "w_gate": n(ks[21], (L, N_EXPERTS, D, D_EXPERT), f32) * D ** -0.5,
        "w_up": n(ks[22], (L, N_EXPERTS, D, D_EXPERT), f32) * (D ** -0.5) * DEEPNORM_BETA,
        "w_down": n(ks[23], (L, N_EXPERTS, D_EXPERT, D), f32) * (D_EXPERT ** -0.5) * DEEPNORM_BETA,
        "ln2_g": 1.0 + 0.02 * n(ks[24], (L, D), f32),
        "ln2_b": 0.02 * n(ks[25], (L, D), f32),
    }


def reference(x, c, w_ada, b_ada, w_in, lambda_q1, lambda_k1, lambda_q2, lambda_k2, subln_g,
              sg_ln_g, sg_ln_b, w_spatial, b_spatial, w_out, ln1_g, ln1_b, w_group, b_group,
              w_router, b_router, w_gate, w_up, w_down, ln2_g, ln2_b):
    B, S, D = x.shape
    cos, sin = rope_tables(S, DIFF_QK_DIM)
    split_at = [QK_COLS, 2 * QK_COLS, 2 * QK_COLS + DIFF_WIDTH, 2 * QK_COLS + DIFF_WIDTH + SG_WIDTH]
    for l in range(DEPTH):
        mod = (jax.nn.silu(c) @ w_ada[l] + b_ada[l])[:, None, :]
        sh1, sc1, gt1, sh2, sc2, gt2 = jnp.split(mod, 6, axis=-1)

        h = x * (1 + sc1) + sh1
        proj = h @ w_in[l]
        q, k, v, u, vs = jnp.split(proj, split_at, axis=-1)
        q = apply_rope(q.reshape(B, S, N_DIFF_HEADS, 2, DIFF_QK_DIM), cos, sin)
        k = apply_rope(k.reshape(B, S, N_DIFF_HEADS, 2, DIFF_QK_DIM), cos, sin)
        v = v.reshape(B, S, N_DIFF_HEADS, DIFF_V_DIM)
        lam_init = 0.8 - 0.6 * math.exp(-0.3 * l)
        lam = (jnp.exp(jnp.sum((lambda_q1[l] * lambda_k1[l]).astype(jnp.float32)))
               - jnp.exp(jnp.sum((lambda_q2[l] * lambda_k2[l]).astype(jnp.float32))) + lam_init)
        att = diff_attention(q, k, v, lam)
        att = rms_norm(att, subln_g[l]) * (1.0 - lam_init)
        zu = jax.nn.gelu(u, approximate=False).reshape(B, S, N_SG_GROUPS, SG_GROUP_DIM)
        zv = jax.nn.gelu(vs, approximate=False).reshape(B, S, N_SG_GROUPS, SG_GROUP_DIM)
        sg = chunk_spatial_gate(zu, zv, sg_ln_g[l], sg_ln_b[l], w_spatial[l], b_spatial[l])
        mix = jnp.concatenate([att.reshape(B, S, DIFF_WIDTH), sg.reshape(B, S, SG_WIDTH)], axis=-1) @ w_out[l]
        x = layer_norm(DEEPNORM_ALPHA * x + (1 + gt1) * mix, ln1_g[l], ln1_b[l])

        h = x * (1 + sc2) + sh2
        ffn = hier_moe(h, w_group[l], b_group[l], w_router[l], b_router[l], w_gate[l], w_up[l], w_down[l])
        x = layer_norm(DEEPNORM_ALPHA * x + (1 + gt2) * ffn, ln2_g[l], ln2_b[l])
    return x
```

```python
import contextlib
import math
import numpy as np
import concourse.bass as bass
import concourse.mybir as mybir
from concourse.bass_utils import run_bass_kernel_spmd

F32 = mybir.dt.float32
BF16 = mybir.dt.bfloat16
AF = mybir.ActivationFunctionType
ALU = mybir.AluOpType
AX = mybir.AxisListType

D = 1024
NKC = 8
NE = 32
DFF = 512
INX = 3584
ALPHA = (2.0 * 2) ** 0.25
LN_EPS = 1e-5
NEG_BIG = -1.0e30


class Tracker:
    def __init__(self, nc, es):
        self.nc = nc
        self.es = es
        self.eng = {"pe": nc.tensor, "act": nc.scalar, "dve": nc.vector, "pool": nc.gpsimd, "sp": nc.sync}
        self.sem = {}
        self.cnt = {}
        self.pool = [es.enter_context(nc.semaphore(f"sm{i}")) for i in range(88)]
        for h in self.pool:
            nc.gpsimd.sem_clear(h)
        nc.all_engine_barrier()
        for k in self.eng:
            self.sem[k] = self.pool.pop()
            self.cnt[k] = 0
        self.seen = {k: {} for k in self.eng}
        self.writers = {}
        self.readers = {}
        self.n_ops = 0
        self.n_waits = 0

    def _dsem(self, name):
        if name not in self.sem:
            self.sem[name] = self.pool.pop()
            self.cnt[name] = 0
        return name

    def op(self, engname, fn, reads=(), writes=(), dsem=None, nowaw=False):
        deps = {}
        for k in reads:
            for s, v in self.writers.get(k, {}).items():
                if deps.get(s, 0) < v:
                    deps[s] = v
        for k in writes:
            for s, v in self.writers.get(k, {}).items():
                if nowaw and s == dsem:
                    continue
                if deps.get(s, 0) < v:
                    deps[s] = v
            for s, v in self.readers.get(k, {}).items():
                if deps.get(s, 0) < v:
                    deps[s] = v
        seen = self.seen[engname]
        e = self.eng[engname]
        for s, v in deps.items():
            if s == engname and engname == "pe" and dsem is None:
                continue
            if seen.get(s, 0) >= v:
                continue
            e.wait_ge(self.sem[s], v)
            seen[s] = v
            self.n_waits += 1
        ins = fn()
        self.n_ops += 1
        if dsem is not None:
            self._dsem(dsem)
            self.cnt[dsem] += 16
            ins.then_inc(self.sem[dsem], 16)
            ev = (dsem, self.cnt[dsem])
        else:
            self.cnt[engname] += 1
            ins.then_inc(self.sem[engname], 1)
            ev = (engname, self.cnt[engname])
        for k in writes:
            self.writers[k] = {ev[0]: ev[1]}
            self.readers[k] = {}
        for k in reads:
            r = self.readers.setdefault(k, {})
            if r.get(ev[0], 0) < ev[1]:
                r[ev[0]] = ev[1]

    def final_wait(self, engname, keys):
        deps = {}
        for k in keys:
            for s, v in self.writers.get(k, {}).items():
                if deps.get(s, 0) < v:
                    deps[s] = v
        for s, v in deps.items():
            self.eng[engname].wait_ge(self.sem[s], v)


def build_program(S, L, dbg=False):
    assert S % 512 == 0
    NG = S // 512
    NT = S // 128
    TB = min(S, 1024)
    NBLK = S // TB
    nc = bass.Bass("TRN2", target_bir_lowering=False)

    def din(name, shape, dt=F32):
        return nc.dram_tensor(name, list(shape), dt, kind="ExternalInput").ap()

    def dscr(name, shape, dt):
        return nc.dram_tensor(name, list(shape), dt, kind=("ExternalOutput" if dbg else "Internal")).ap()

    x_in = din("x", [S, D])
    cT_in = din("cT", [128, 8])
    w_ada = din("w_ada", [L, D, 6 * D])
    b_ada = din("b_ada", [L, 6 * D])
    w_in = din("w_in", [L, D, INX])
    lam_in = din("lam4", [L, 4, 64])
    subln_g = din("subln_g", [L, 128])
    sg_ln_g = din("sg_ln_g", [L, 512])
    sg_ln_b = din("sg_ln_b", [L, 512])
    w_spT = din("w_spT", [L, 4, 128, 128])
    b_sp = din("b_sp", [L, 512])
    w_out = din("w_out", [L, D, D])
    ln1_g = din("ln1_g", [L, D])
    ln1_b = din("ln1_b", [L, D])
    w_r = din("w_r", [L, D, 36])
    b_r = din("b_r", [L, 36])
    wall_in = din("wall", [L, NE * 128, 12288])
    triu1_in = din("triu1", [128, 128])
    pcol_in = din("pcol", [128, 2])
    iota_in = din("iota32", [128, 32])
    kt_in = din("kt512", [128, 64])
    ln2_g = din("ln2_g", [L, D])
    ln2_b = din("ln2_b", [L, D])
    ident_in = din("ident", [128, 128])
    tri_in = din("tri", [128, 128])
    cs_in = din("csT", [128, 2, S])
    out = nc.dram_tensor("out", [S, D], F32, kind="ExternalOutput").ap()

    QT = dscr("QT", [4, 128, S], BF16)
    KT = dscr("KT", [4, 128, S], BF16)
    VS = dscr("VS", [S, 512], BF16)
    SGT = dscr("SGT", [4, 128, S], BF16)
    ATT = dscr("ATT", [4, 128, S], BF16)
    X1 = dscr("X1", [S, D], F32)
    X2 = dscr("X2", [S, D], F32)
    CAP = S
    NTILE = (2 * S + NE * 511) // 512
    HS = dscr("HS", [NTILE * 512, D], BF16)
    H2 = dscr("H2", [S, D], BF16)
    WB = dscr("WB", [NE * 128, 12288], BF16)
    YS = dscr("YS", [NTILE * 512, D], F32)

    es = contextlib.ExitStack()
    with es:
        T = Tracker(nc, es)
        DQ, DQE = "pool", nc.gpsimd
        reg_hs = nc.gpsimd.to_reg(NTILE * 512 - 1)
        reg_w = nc.gpsimd.to_reg(NE * 128 - 1)
        reg_ys = nc.gpsimd.to_reg(NTILE * 512 - 1)

        uniq = [0]

        def sb(name, shape, dt, stack=es):
            uniq[0] += 1
            return stack.enter_context(nc.sbuf_tensor(f"sb{uniq[0]}_{name}", list(shape), dt))

        ps = [es.enter_context(nc.psum_tensor(f"ps{i}", [128, 512], F32)) for i in range(8)]

        def PK(i):
            return ("ps", i)

        ident = sb("ident", [128, 128], F32)
        tri_bf = sb("tri_bf", [128, 128], BF16)
        ones_bf = sb("ones_bf", [128, 128], BF16)
        onesdiv = sb("onesdiv", [128, 128], F32)
        ones_f32 = sb("ones_f32", [128, 128], F32)
        epscol = sb("epscol", [128, 1], F32)
        ones_row = sb("ones_row", [1, 128], F32)
        cTs = sb("cTs", [128, 8], F32)
        siluc = sb("siluc", [128, 8], F32)
        modcol = sb("modcol", [128, 32], F32)
        gt1p = sb("gt1p", [128, D], F32)
        gt2p = sb("gt2p", [128, D], F32)
        lamt = sb("lamt", [128, 4, 64], F32)
        lamw = sb("lamw", [128, 8], F32)
        neglam = sb("neglam", [128, 1], F32)
        gcol = sb("gcol", [128, 1], F32)
        sc2b = sb("sc2b", [128, D], F32)
        sh2b = sb("sh2b", [128, D], F32)
        tokmeta = sb("tokmeta", [128, NT, 8], F32)
        ident_bf = sb("ident_bf", [128, 128], BF16)
        triu1_bf = sb("triu1_bf", [128, 128], BF16)
        pcol = sb("pcol", [128, 2], F32)
        iota32 = sb("iota32", [128, 32], F32)
        kt512 = sb("kt512", [128, 64], F32)
        cum = sb("cum", [128, 32], F32)
        cumb = [sb(f"cumb{i}", [128, 32], BF16) for i in range(2)]
        ohb = [sb(f"ohb{i}", [128, 32], BF16) for i in range(2)]
        md = sb("md", [128, 192], F32)
        tl = sb("tl", [128, 5, NTILE], F32)
        widxf = sb("widxf", [128, NTILE], F32)
        widx_i = sb("widx_i", [128, NTILE], mybir.dt.int32)
        poscf = sb("poscf", [128, NT, 2], F32)
        posc_i = sb("posc_i", [128, NT * 2], mybir.dt.int32)

        T.op(DQ, lambda: DQE.dma_start(out=ident[:], in_=ident_in), writes=["ident"], dsem="c0")
        T.op("pool", lambda: nc.gpsimd.dma_start(out=tri_bf[:], in_=tri_in), writes=["tri"], dsem="c1")
        T.op(DQ, lambda: DQE.dma_start(out=cTs[:], in_=cT_in), writes=["cT"], dsem="c0")
        T.op("pool", lambda: nc.gpsimd.dma_start(out=ident_bf[:], in_=ident_in), writes=["ident_bf"], dsem="c2")
        T.op("pool", lambda: nc.gpsimd.dma_start(out=triu1_bf[:], in_=triu1_in), writes=["triu1"], dsem="c3")
        T.op("pool", lambda: nc.gpsimd.dma_start(out=pcol[:], in_=pcol_in), writes=["pcol"], dsem="c4")
        T.op("pool", lambda: nc.gpsimd.dma_start(out=iota32[:], in_=iota_in), writes=["iota32"], dsem="c5")
        T.op("pool", lambda: nc.gpsimd.dma_start(out=kt512[:], in_=kt_in), writes=["kt512"], dsem="c6")
        T.op("dve", lambda: nc.vector.memset(ones_bf[:], 1.0), writes=["ones_bf"])
        T.op("dve", lambda: nc.vector.memset(onesdiv[:], 1.0 / 128.0), writes=["onesdiv"])
        T.op("dve", lambda: nc.vector.memset(ones_f32[:], 1.0), writes=["ones_f32"])
        T.op("dve", lambda: nc.vector.memset(epscol[:], LN_EPS), writes=["epscol"])
        T.op("dve", lambda: nc.vector.memset(ones_row[:], 1.0), writes=["ones_row"])
        T.op("act", lambda: nc.scalar.activation(out=siluc[:], in_=cTs[:], func=AF.Silu),
             reads=["cT"], writes=["siluc"])

        def dump(name, ap, shape, keys, dt=F32):
            if not dbg:
                return
            dt_ = nc.dram_tensor("dbg_" + name, list(shape), dt, kind="ExternalOutput").ap()
            T.op(DQ, lambda: DQE.dma_start(out=dt_, in_=ap), reads=keys, writes=[("dbg", name)], dsem="dbg")

        def rsqrt_small(stack, name, dst, src_ap, n, rd_keys, wr_key):
            T.op("dve", lambda: nc.vector.tensor_scalar(out=dst, in0=src_ap, scalar1=LN_EPS, scalar2=None,
                                                        op0=ALU.add), reads=rd_keys, writes=[wr_key])
            T.op("act", lambda: nc.scalar.activation(out=dst, in_=dst, func=AF.Sqrt),
                 reads=[wr_key], writes=[wr_key])
            T.op("dve", lambda: nc.vector.reciprocal(out=dst, in_=dst), reads=[wr_key], writes=[wr_key])

        for l in range(L):
            lam_init = 0.8 - 0.6 * math.exp(-0.3 * l)
            xsrc = x_in if l == 0 else X2
            xdst = out if l == L - 1 else X2

            with contextlib.ExitStack() as st:
                wada = [sb(f"wada{i}", [128, 8, 1024], F32, st) for i in range(2)]
                modrow = sb("modrow", [1, 6 * D], F32, st)
                badarow = sb("badarow", [1, 6 * D], F32, st)
                T.op(DQ, lambda: DQE.dma_start(out=badarow[:], in_=b_ada[l:l + 1, :]),
                     writes=["badarow"], dsem="badarow")
                for bi, blk in enumerate(range(6)):
                    b = bi % 2
                    T.op("pool", lambda: nc.gpsimd.dma_start(
                        out=wada[b][:], in_=w_ada[l, :, blk * 1024:(blk + 1) * 1024].rearrange("(k p) n -> p k n", p=128)),
                        reads=[("wada", 1 - b)], writes=[("wada", b)], dsem=f"wada{b}")
                    if l == 0 and blk == 0:
                        dump("wada0", wada[0][:], [128, 8, 1024], [("wada", 0)])
                        dump("badarow", badarow[:], [1, 6 * D], ["badarow"])
                    for half in range(2):
                        bank = (blk * 2 + half) % 8
                        c0 = blk * 1024 + half * 512
                        for kc in range(8):
                            T.op("pe", lambda: nc.tensor.matmul(
                                ps[bank][0:1, :], lhsT=siluc[:, kc:kc + 1], rhs=wada[b][:, kc, half * 512:(half + 1) * 512],
                                start=(kc == 0), stop=(kc == 7)),
                                reads=[("wada", b), "siluc"], writes=[PK(bank)])
                        T.op("dve", lambda: nc.vector.tensor_tensor(
                            out=modrow[0:1, c0:c0 + 512], in0=ps[bank][0:1, :], in1=badarow[0:1, c0:c0 + 512], op=ALU.add),
                            reads=[PK(bank), "badarow"], writes=["modrow"])
                if l == 0:
                    dump("modrow", modrow[:], [1, 6 * D], ["modrow"])
                for i, blk in enumerate((0, 1, 3, 4)):
                    for j in range(8):
                        T.op("pe", lambda: nc.tensor.matmul(
                            ps[4][:, i * 8 + j:i * 8 + j + 1], lhsT=modrow[0:1, blk * 1024 + j * 128: blk * 1024 + (j + 1) * 128],
                            rhs=ones_row[0:1, 0:1], start=True, stop=True),
                            reads=["modrow", "ones_row"], writes=[PK(4)])
                T.op("dve", lambda: nc.vector.tensor_copy(out=modcol[:], in_=ps[4][:, 0:32]),
                     reads=[PK(4)], writes=["modcol"])
                for c0 in (8, 24):
                    T.op("dve", lambda: nc.vector.tensor_scalar(
                        out=modcol[:, c0:c0 + 8], in0=modcol[:, c0:c0 + 8], scalar1=1.0, scalar2=None, op0=ALU.add),
                        reads=["modcol"], writes=["modcol"])
                for dst, key, blk, addc in ((gt1p, "gt1p", 2, 1.0), (gt2p, "gt2p", 5, 1.0), (sh2b, "sh2b", 3, 0.0), (sc2b, "sc2b", 4, 1.0)):
                    for half in range(2):
                        bank = 5 + half
                        T.op("pe", lambda: nc.tensor.matmul(
                            ps[bank][:], lhsT=ones_row[0:1, :], rhs=modrow[0:1, blk * 1024 + half * 512: blk * 1024 + (half + 1) * 512],
                            start=True, stop=True), reads=["modrow", "ones_row"], writes=[PK(bank)])
                        T.op("dve", lambda: nc.vector.tensor_scalar(
                            out=dst[:, half * 512:(half + 1) * 512], in0=ps[bank][:], scalar1=addc, scalar2=None, op0=ALU.add),
                            reads=[PK(bank)], writes=[key])
                T.op(DQ, lambda: DQE.dma_start(
                    out=lamt[:].rearrange("p a b -> p (a b)"),
                    in_=lam_in[l].rearrange("a b -> (a b)").partition_broadcast(128)),
                    writes=["lamt"], dsem="lamt")
                T.op("dve", lambda: nc.vector.tensor_tensor(out=lamt[:, 0, :], in0=lamt[:, 0, :], in1=lamt[:, 1, :], op=ALU.mult),
                     reads=["lamt"], writes=["lamt"])
                T.op("dve", lambda: nc.vector.tensor_tensor(out=lamt[:, 2, :], in0=lamt[:, 2, :], in1=lamt[:, 3, :], op=ALU.mult),
                     reads=["lamt"], writes=["lamt"])
                T.op("dve", lambda: nc.vector.tensor_reduce(out=lamw[:, 0:1], in_=lamt[:, 0, :], axis=AX.X, op=ALU.add),
                     reads=["lamt"], writes=["lamw"])
                T.op("dve", lambda: nc.vector.tensor_reduce(out=lamw[:, 1:2], in_=lamt[:, 2, :], axis=AX.X, op=ALU.add),
                     reads=["lamt"], writes=["lamw"])
                T.op("act", lambda: nc.scalar.activation(out=lamw[:, 2:4], in_=lamw[:, 0:2], func=AF.Exp),
                     reads=["lamw"], writes=["lamw"])
                T.op("dve", lambda: nc.vector.tensor_tensor(out=lamw[:, 4:5], in0=lamw[:, 3:4], in1=lamw[:, 2:3], op=ALU.subtract),
                     reads=["lamw"], writes=["lamw"])
                T.op("dve", lambda: nc.vector.tensor_scalar(out=neglam[:], in0=lamw[:, 4:5], scalar1=-lam_init, scalar2=None, op0=ALU.add),
                     reads=["lamw"], writes=["neglam"])
                T.op(DQ, lambda: DQE.dma_start(out=gcol[:], in_=subln_g[l:l + 1, :].rearrange("o p -> p o"),
                                                     allow_slow_non_contiguous=True),
                     writes=["gcol"], dsem="gcol")
                T.op("dve", lambda: nc.vector.tensor_scalar(out=gcol[:], in0=gcol[:], scalar1=1.0 - lam_init, scalar2=None, op0=ALU.mult),
                     reads=["gcol"], writes=["gcol"])

            if l == 0:
                dump("modcol", modcol[:], [128, 32], ["modcol"])
                dump("gt1p", gt1p[:], [128, D], ["gt1p"])
                dump("gt2p", gt2p[:], [128, D], ["gt2p"])
                dump("neglam", neglam[:], [128, 1], ["neglam"])
                dump("gcol", gcol[:], [128, 1], ["gcol"])
                dump("siluc", siluc[:], [128, 8], ["siluc"])
            with contextlib.ExitStack() as st:
                win = sb("win", [128, 8, INX], BF16, st)
                xg = [sb(f"xg{i}", [128, 4, D], F32, st) for i in range(2)]
                hT = [sb(f"hT{i}", [128, 8, 512], BF16, st) for i in range(2)]
                cs = [sb(f"cs{i}", [128, 2, 512], F32, st) for i in range(2)]
                qT = [sb(f"qT{i}", [128, 4, 512], BF16, st) for i in range(2)]
                kT = [sb(f"kT{i}", [128, 4, 512], BF16, st) for i in range(2)]
                vt = [sb(f"vt{i}", [128, 4, 512], BF16, st) for i in range(2)]
                sgT = [sb(f"sgT{i}", [128, 4, 512], BF16, st) for i in range(2)]
                zuT2 = [sb(f"zuT{i}", [128, 4, 512], BF16, st) for i in range(2)]
                tmpa = [sb(f"tmpa{i}", [128, 512], F32, st) for i in range(2)]
                tmpb = [sb(f"tmpb{i}", [128, 512], F32, st) for i in range(2)]
                zv = [sb(f"zv{i}", [128, 512], F32, st) for i in range(2)]
                zvn = [sb(f"zvn{i}", [128, 512], BF16, st) for i in range(2)]
                stats2 = [sb(f"stats{i}", [128, 4, 6], F32, st) for i in range(2)]
                mv2 = [sb(f"mv{i}", [128, 4, 2], F32, st) for i in range(2)]
                rstd2 = [sb(f"rstd{i}", [128, 4], F32, st) for i in range(2)]
                wsT = sb("wsT", [128, 4, 128], BF16, st)
                bspb = sb("bspb", [128, 512], F32, st)
                lngb = sb("lngb", [128, 512], F32, st)
                lnbb = sb("lnbb", [128, 512], F32, st)

                for kc in range(8):
                    T.op("pool", lambda: nc.gpsimd.dma_start(out=win[:, kc, :], in_=w_in[l, kc * 128:(kc + 1) * 128, :],
                                                             max_dma_last_dim=7168),
                         writes=[("win", kc)], dsem=f"win{kc}")
                T.op("pool", lambda: nc.gpsimd.dma_start(out=wsT[:], in_=w_spT[l].rearrange("g s t -> s g t")),
                     writes=["wsT"], dsem="wsT")
                for gi in range(4):
                    T.op("dve", lambda: nc.vector.tensor_tensor(out=wsT[:, gi, :], in0=wsT[:, gi, :], in1=tri_bf[:], op=ALU.mult),
                         reads=["wsT", "tri"], writes=["wsT"])
                for dst, key, src in ((bspb, "bspb", b_sp), (lngb, "lngb", sg_ln_g), (lnbb, "lnbb", sg_ln_b)):
                    T.op(DQ, lambda: DQE.dma_start(out=dst[:], in_=src[l].partition_broadcast(128)),
                         writes=[key], dsem=key)

                bank_rr = [0]

                def nxt_bank():
                    b = 2 + bank_rr[0] % 6
                    bank_rr[0] += 1
                    return b

                EPG = (NE + NG - 1) // NG

                def a_loads(g_):
                    b_ = g_ % 2
                    T.op(DQ, lambda: DQE.dma_start(out=xg[b_][:], in_=xsrc[g_ * 512:(g_ + 1) * 512, :].rearrange("(t p) d -> p t d", p=128)),
                         reads=[("X", l, g_)] if l > 0 else [], writes=[("xg", b_)], dsem=f"xg{b_}")
                    T.op(DQ, lambda: DQE.dma_start(out=cs[b_][:], in_=cs_in[:, :, g_ * 512:(g_ + 1) * 512]),
                         writes=[("cs", b_)], dsem=f"cs{b_}")
                def a_transposes(g_, kcs):
                    b_ = g_ % 2
                    for kc in kcs:
                        bank = kc % 2
                        for t in range(4):
                            T.op("pe", lambda: nc.tensor.transpose(ps[bank][:, t * 128:(t + 1) * 128],
                                                                   xg[b_][:, t, kc * 128:(kc + 1) * 128], ident[:]),
                                 reads=[("xg", b_), "ident"], writes=[PK(bank)])
                        T.op("act", lambda: nc.scalar.activation(out=hT[b_][:, kc, :], in_=ps[bank][:], func=AF.Identity,
                                                                 bias=modcol[:, kc:kc + 1], scale=modcol[:, 8 + kc:9 + kc]),
                             reads=[PK(bank), "modcol"], writes=[("hT", b_, kc)])

                for g in range(NG):
                    b = g % 2
                    t0 = g * 512
                    for e in range(g * EPG, min(NE, (g + 1) * EPG)):
                        T.op("pool", lambda: nc.gpsimd.dma_start(
                            out=WB[e * 128:(e + 1) * 128, :].rearrange("r (a b) -> r a b", b=2048),
                            in_=wall_in[l, e * 128:(e + 1) * 128, :].rearrange("r (a b) -> r a b", b=2048)),
                            writes=[("WB", e)], dsem=f"wb{e % 2}")
                    if g == 0:
                        a_loads(0)
                    if g + 1 < NG:
                        a_loads(g + 1)
                    if g == 0:
                        a_transposes(0, range(8))
                    HT = [("hT", b, kc) for kc in range(8)]
                    for h in range(4):
                        for nm, base, dst in (("q", 0, qT), ("k", 1024, kT)):
                            ba = nxt_bank()
                            bb = nxt_bank()
                            for bank, off in ((ba, base), (bb, base + 512)):
                                for kc in range(8):
                                    T.op("pe", lambda: nc.tensor.matmul(
                                        ps[bank][:], lhsT=win[:, kc, off + h * 128: off + (h + 1) * 128], rhs=hT[b][:, kc, :],
                                        start=(kc == 0), stop=(kc == 7)),
                                        reads=[("win", kc), ("hT", b, kc)], writes=[PK(bank)])
                            i = (h * 2 + (nm == "k")) % 2
                            T.op("dve", lambda: nc.vector.tensor_tensor(out=tmpa[i][:], in0=ps[ba][:], in1=cs[b][:, 0, :], op=ALU.mult),
                                 reads=[PK(ba), ("cs", b)], writes=[("tmpa", i)])
                            T.op("dve", lambda: nc.vector.tensor_tensor(out=tmpb[i][:], in0=ps[bb][:], in1=cs[b][:, 1, :], op=ALU.mult),
                                 reads=[PK(bb), ("cs", b)], writes=[("tmpb", i)])
                            T.op("pool", lambda: nc.gpsimd.tensor_tensor(out=dst[b][:, h, :], in0=tmpa[i][:], in1=tmpb[i][:], op=ALU.add),
                                 reads=[("tmpa", i), ("tmpb", i)], writes=[(nm + "T", b)])
                    if g + 1 < NG:
                        a_transposes(g + 1, range(0, 4))
                    zuT = zuT2[b]
                    for gi in range(4):
                        bank = nxt_bank()
                        for kc in range(8):
                            T.op("pe", lambda: nc.tensor.matmul(
                                ps[bank][:], lhsT=win[:, kc, 2560 + gi * 128: 2560 + (gi + 1) * 128], rhs=hT[b][:, kc, :],
                                start=(kc == 0), stop=(kc == 7)),
                                reads=[("win", kc), ("hT", b, kc)], writes=[PK(bank)])
                        T.op("act", lambda: nc.scalar.activation(out=zuT[:, gi, :], in_=ps[bank][:], func=AF.Gelu),
                             reads=[PK(bank)], writes=[("zuT", b, gi)])
                    if g + 1 < NG:
                        a_transposes(g + 1, range(4, 8))

                    def a_spatial(tt):
                        bank = nxt_bank()
                        for gi in range(4):
                            T.op("pe", lambda: nc.tensor.matmul(
                                ps[bank][:, gi * 128:(gi + 1) * 128], lhsT=zvn[tt % 2][:, gi * 128:(gi + 1) * 128], rhs=wsT[:, gi, :],
                                start=True, stop=True),
                                reads=[("zvn", tt % 2), "wsT"], writes=[PK(bank)])
                        ti = tt % 2
                        T.op("dve", lambda: nc.vector.tensor_tensor(out=tmpa[ti][:], in0=ps[bank][:], in1=bspb[:], op=ALU.add),
                             reads=[PK(bank), "bspb"], writes=[("tmpa", ti)])
                        T.op("dve", lambda: nc.vector.tensor_tensor(
                            out=sgT[b][:, :, tt * 128:(tt + 1) * 128], in0=tmpa[ti][:].rearrange("p (g t) -> p g t", g=4),
                            in1=zuT[:, :, tt * 128:(tt + 1) * 128], op=ALU.mult),
                            reads=[("tmpa", ti)] + [("zuT", b, gi) for gi in range(4)], writes=[("sgT", b)])
                    for t in range(4):
                        bank = nxt_bank()
                        for kc in range(8):
                            T.op("pe", lambda: nc.tensor.matmul(
                                ps[bank][:], lhsT=hT[b][:, kc, t * 128:(t + 1) * 128], rhs=win[:, kc, 2048:2560],
                                start=(kc == 0), stop=(kc == 7)),
                                reads=[("win", kc), ("hT", b, kc)], writes=[PK(bank)])
                        T.op("act", lambda: nc.scalar.copy(out=vt[b][:, t, :], in_=ps[bank][:]),
                             reads=[PK(bank)], writes=[("vt", b)])
                        bank = nxt_bank()
                        for kc in range(8):
                            T.op("pe", lambda: nc.tensor.matmul(
                                ps[bank][:], lhsT=hT[b][:, kc, t * 128:(t + 1) * 128], rhs=win[:, kc, 3072:3584],
                                start=(kc == 0), stop=(kc == 7)),
                                reads=[("win", kc), ("hT", b, kc)], writes=[PK(bank)])
                        zi = t % 2
                        stats, mv, rstd = stats2[zi], mv2[zi], rstd2[zi]
                        SK, MK, RK = ("stats", zi), ("mv", zi), ("rstd", zi)
                        T.op("act", lambda: nc.scalar.activation(out=zv[zi][:], in_=ps[bank][:], func=AF.Gelu),
                             reads=[PK(bank)], writes=[("zv", zi)])
                        for gi in range(4):
                            T.op("dve", lambda: nc.vector.bn_stats(out=stats[:, gi, :], in_=zv[zi][:, gi * 128:(gi + 1) * 128]),
                                 reads=[("zv", zi)], writes=[SK])
                        for gi in range(4):
                            T.op("dve", lambda: nc.vector.bn_aggr(out=mv[:, gi, :], in_=stats[:, gi, :]),
                                 reads=[SK], writes=[MK])
                        rsqrt_small(st, "rstd", rstd[:], mv[:, :, 1], 4, [MK], RK)
                        for gi in range(4):
                            T.op("dve", lambda: nc.vector.tensor_scalar(
                                out=zv[zi][:, gi * 128:(gi + 1) * 128], in0=zv[zi][:, gi * 128:(gi + 1) * 128],
                                scalar1=mv[:, gi, 0:1], scalar2=rstd[:, gi:gi + 1], op0=ALU.subtract, op1=ALU.mult),
                                reads=[("zv", zi), MK, RK], writes=[("zv", zi)])
                        T.op("dve", lambda: nc.vector.tensor_tensor(out=zv[zi][:], in0=zv[zi][:], in1=lngb[:], op=ALU.mult),
                             reads=[("zv", zi), "lngb"], writes=[("zv", zi)])
                        T.op("dve", lambda: nc.vector.tensor_tensor(out=zvn[zi][:], in0=zv[zi][:], in1=lnbb[:], op=ALU.add),
                             reads=[("zv", zi), "lnbb"], writes=[("zvn", zi)])
                        if t > 0:
                            a_spatial(t - 1)
                    a_spatial(3)
                    T.op("pool", lambda: nc.gpsimd.dma_start(out=QT[:, :, t0:t0 + 512].rearrange("h p t -> p h t"), in_=qT[b][:]),
                         reads=[("qT", b)], writes=[("QT", g)], dsem=f"stq{b}")
                    T.op("pool", lambda: nc.gpsimd.dma_start(out=KT[:, :, t0:t0 + 512].rearrange("h p t -> p h t"), in_=kT[b][:]),
                         reads=[("kT", b)], writes=[("KT", g)], dsem=f"stk{b}")
                    T.op("pool", lambda: nc.gpsimd.dma_start(out=VS[t0:t0 + 512, :].rearrange("(t p) c -> p t c", p=128), in_=vt[b][:]),
                         reads=[("vt", b)], writes=[("VS", g)], dsem=f"stv{b}")
                    T.op("pool", lambda: nc.gpsimd.dma_start(out=SGT[:, :, t0:t0 + 512].rearrange("h p t -> p h t"), in_=sgT[b][:]),
                         reads=[("sgT", b)], writes=[("SGT", g)], dsem=f"sts{b}")

            with contextlib.ExitStack() as st:
                kTh = [sb(f"kTh{i}", [128, S], BF16, st) for i in range(2)]
                vh = [sb(f"vh{i}", [128, NT, 128], BF16, st) for i in range(2)]
                qc_sb = [sb(f"qc{i}", [128, 512], BF16, st) for i in range(2)]
                pb = [sb(f"pb{i}", [128, 2, 512], BF16, st) for i in range(4)]
                SB = ((0, 1), (2, 3))
                racc = [[[sb(f"racc{i}{m}{p_}", [128, 512], F32, st) for p_ in range(2)] for m in range(2)] for i in range(2)]
                r1 = sb("r1", [128, 512], F32, st)
                r2 = sb("r2", [128, 512], F32, st)
                fa = sb("fa", [128, 512], F32, st)
                fb = sb("fb", [128, 512], F32, st)
                fsq = sb("fsq", [128, 512], F32, st)
                frs = sb("frs", [128, 512], F32, st)
                ao = [sb(f"ao{i}", [128, 512], BF16, st) for i in range(2)]
                OB = (4, 5, 6, 7)
                def b_load_head(h_):
                    hb_ = h_ % 2
                    T.op(DQ, lambda: DQE.dma_start(out=kTh[hb_][:], in_=KT[h_]),
                         reads=[("KT", g) for g in range(NG)], writes=[("kTh", hb_)], dsem=f"kTh{hb_}")
                    T.op(DQ, lambda: DQE.dma_start(out=vh[hb_][:], in_=VS[:, h_ * 128:(h_ + 1) * 128].rearrange("(n p) c -> p n c", p=128)),
                         reads=[("VS", g) for g in range(NG)], writes=[("vh", hb_)], dsem=f"vh{hb_}")

                def b_load_q(it_):
                    h_, qc_ = divmod(it_, NG)
                    T.op(DQ, lambda: DQE.dma_start(out=qc_sb[it_ % 2][:], in_=QT[h_, :, qc_ * 512:(qc_ + 1) * 512]),
                         reads=[("QT", qc_)], writes=[("qc", it_ % 2)], dsem=f"qc{it_ % 2}")

                b_load_head(0)
                b_load_q(0)
                for h in range(4):
                    hb = h % 2
                    for qc in range(NG):
                        it = h * NG + qc
                        qb = it % 2
                        q0 = qc * 512
                        if qc == 0 and h + 1 < 4:
                            b_load_head(h + 1)
                        if it + 1 < 4 * NG:
                            b_load_q(it + 1)
                        nkb = 4 * (qc + 1)

                        def emit_S(kb):
                            j = kb - 4 * qc
                            c0 = max(0, j) * 128
                            for m in range(2):
                                bank = SB[kb % 2][m]
                                T.op("pe", lambda: nc.tensor.matmul(
                                    ps[bank][:, c0:512], lhsT=kTh[hb][m * 64:(m + 1) * 64, kb * 128:(kb + 1) * 128],
                                    rhs=qc_sb[qb][m * 64:(m + 1) * 64, c0:512], start=True, stop=True),
                                    reads=[("kTh", hb), ("qc", qb)], writes=[PK(bank)])

                        def emit_P(kb):
                            j = kb - 4 * qc
                            c0 = max(0, j) * 128
                            pi = kb % 4
                            for m in range(2):
                                bank = SB[kb % 2][m]
                                T.op("act", lambda: nc.scalar.activation(out=pb[pi][:, m, c0:512], in_=ps[bank][:, c0:512],
                                                                         func=AF.Exp, scale=0.125),
                                     reads=[PK(bank)], writes=[("pb", pi, m)])
                                if j >= 0:
                                    en, ee = ("dve", nc.vector) if m == 0 else ("pool", nc.gpsimd)
                                    T.op(en, lambda: ee.tensor_tensor(
                                        out=pb[pi][:, m, c0:c0 + 128], in0=pb[pi][:, m, c0:c0 + 128], in1=tri_bf[:], op=ALU.mult),
                                        reads=[("pb", pi, m), "tri"], writes=[("pb", pi, m)])

                        def emit_PV(kb):
                            j = kb - 4 * qc
                            c0 = max(0, j) * 128
                            pi = kb % 4
                            for m in range(2):
                                T.op("pe", lambda: nc.tensor.matmul(
                                    ps[OB[2 * m]][:, c0:512], lhsT=vh[hb][:, kb, :], rhs=pb[pi][:, m, c0:512],
                                    start=(kb == 0), stop=(kb == nkb - 1)),
                                    reads=[("vh", hb), ("pb", pi, m)], writes=[PK(OB[2 * m])])
                                if m == 1 and kb % 3 != 0:
                                    T.op("pe", lambda: nc.tensor.matmul(
                                        ps[7][:, c0:512], lhsT=ones_bf[:], rhs=pb[pi][:, m, c0:512],
                                        start=(kb == 1), stop=False),
                                        reads=["ones_bf", ("pb", pi, m)], writes=[PK(7)])
                                    continue
                                ai_ = (kb % 2) if m == 0 else ((kb // 3) % 2)
                                ra = racc[qc % 2][m][ai_]
                                rk = ("racc", qc % 2, m, ai_)
                                if (m, ai_) not in acc_used:
                                    acc_used.add((m, ai_))
                                    T.op("dve", lambda: nc.vector.tensor_copy(out=ra[:, c0:512], in_=pb[pi][:, m, c0:512]),
                                         reads=[("pb", pi, m)], writes=[rk])
                                    if c0 > 0:
                                        T.op("dve", lambda: nc.vector.memset(ra[:, 0:c0], 0.0), reads=[rk], writes=[rk])
                                else:
                                    T.op("dve", lambda: nc.vector.tensor_tensor(out=ra[:, c0:512], in0=ra[:, c0:512], in1=pb[pi][:, m, c0:512], op=ALU.add),
                                         reads=[("pb", pi, m), rk], writes=[rk])

                        acc_used = set()
                        emit_S(0)
                        for kb in range(nkb):
                            if kb + 1 < nkb:
                                emit_S(kb + 1)
                            emit_P(kb)
                            emit_PV(kb)
                        for m in range(2):
                            used = [p_ for p_ in range(2) if (m, p_) in acc_used]
                            for ui, p_ in enumerate(used):
                                T.op("pe", lambda: nc.tensor.matmul(ps[5 + 2 * m][:], lhsT=ones_f32[:], rhs=racc[qc % 2][m][p_][:],
                                                                    start=(m == 0 and ui == 0), stop=(ui == len(used) - 1)),
                                     reads=["ones_f32", ("racc", qc % 2, m, p_)], writes=[PK(5 + 2 * m)])
                        T.op("act", lambda: nc.scalar.activation(out=r1[:], in_=ps[5][:], func=AF.Ln), reads=[PK(5)], writes=["r1"])
                        T.op("act", lambda: nc.scalar.activation(out=r1[:], in_=r1[:], func=AF.Exp, scale=-1.0), reads=["r1"], writes=["r1"])
                        T.op("act", lambda: nc.scalar.activation(out=r2[:], in_=ps[7][:], func=AF.Ln), reads=[PK(7)], writes=["r2"])
                        T.op("act", lambda: nc.scalar.activation(out=r2[:], in_=r2[:], func=AF.Exp, scale=-1.0), reads=["r2"], writes=["r2"])
                        T.op("dve", lambda: nc.vector.tensor_tensor(out=fa[:], in0=ps[4][:], in1=r1[:], op=ALU.mult),
                             reads=[PK(4), "r1"], writes=["fa"])
                        T.op("dve", lambda: nc.vector.tensor_tensor(out=fb[:], in0=ps[6][:], in1=r2[:], op=ALU.mult),
                             reads=[PK(6), "r2"], writes=["fb"])
                        T.op("dve", lambda: nc.vector.scalar_tensor_tensor(out=fa[:], in0=fb[:], scalar=neglam[:, 0:1], in1=fa[:],
                                                                           op0=ALU.mult, op1=ALU.add),
                             reads=["fa", "fb", "neglam"], writes=["fa"])
                        T.op("act", lambda: nc.scalar.activation(out=fsq[:], in_=fa[:], func=AF.Square),
                             reads=["fa"], writes=["fsq"])
                        T.op("pe", lambda: nc.tensor.matmul(ps[0][:], lhsT=onesdiv[:], rhs=fsq[:], start=True, stop=True),
                             reads=["onesdiv", "fsq"], writes=[PK(0)])
                        T.op("act", lambda: nc.scalar.activation(out=frs[:], in_=ps[0][:], func=AF.Ln, bias=epscol[:, 0:1]),
                             reads=[PK(0), "epscol"], writes=["frs"])
                        T.op("act", lambda: nc.scalar.activation(out=frs[:], in_=frs[:], func=AF.Exp, scale=-0.5), reads=["frs"], writes=["frs"])
                        ai = qc % 2
                        T.op("dve", lambda: nc.vector.scalar_tensor_tensor(out=ao[ai][:], in0=fa[:], scalar=gcol[:, 0:1], in1=frs[:],
                                                                           op0=ALU.mult, op1=ALU.mult),
                             reads=["fa", "frs", "gcol"], writes=[("ao", ai)])
                        T.op("pool", lambda: nc.gpsimd.dma_start(out=ATT[h, :, q0:q0 + 512], in_=ao[ai][:]),
                             reads=[("ao", ai)], writes=[("ATT", h, qc)], dsem=f"sta{ai}")

            with contextlib.ExitStack() as st:
                wout = sb("wout", [128, 8, D], BF16, st)
                wr = sb("wr", [128, 8, 36], BF16, st)
                brb = sb("brb", [128, 36], F32, st)
                l1g = sb("l1g", [128, D], F32, st)
                l1b = sb("l1b", [128, D], F32, st)
                mixT = [sb(f"mixT{i}", [128, 8, 512], BF16, st) for i in range(2)]
                xg = [sb(f"xgc{i}", [128, 4, D], F32, st) for i in range(2)]
                x1g = [sb(f"x1g{i}", [128, 4, D], F32, st) for i in range(2)]
                h2T = [sb(f"h2T{i}", [128, 8, 512], BF16, st) for i in range(2)]
                ytmp2 = [sb(f"ytmp{i}", [128, D], F32, st) for i in range(2)]
                stats2 = [sb(f"statsc{i}", [128, 2, 6], F32, st) for i in range(2)]
                mv2 = [sb(f"mvc{i}", [128, 2], F32, st) for i in range(2)]
                rstd2 = [sb(f"rstdc{i}", [128, 1], F32, st) for i in range(2)]
                rg_s = []
                for i in range(2):
                    rg_s.append((sb(f"rlg{i}", [128, 4, 36], F32, st), sb(f"rs4{i}", [128, 4, 16], F32, st), sb(f"rohg{i}", [128, 4, 4], F32, st),
                                 sb(f"rgexp{i}", [128, 4, 4], F32, st), sb(f"rsel{i}", [128, 4, 8], F32, st), sb(f"roh1{i}", [128, 4, 8], F32, st),
                                 sb(f"roh2{i}", [128, 4, 8], F32, st), sb(f"rsel2{i}", [128, 4, 8], F32, st), sb(f"rohs{i}", [128, 4, 8], F32, st),
                                 sb(f"rRg{i}", [128, 4, 8], F32, st), sb(f"rt8{i}", [128, 4, 8], F32, st), sb(f"rt4{i}", [128, 4, 4], F32, st),
                                 sb(f"rprod{i}", [128, 4, 8, 4], F32, st), sb(f"rtmp32{i}", [128, 32], F32, st)))
                ohb4 = [sb(f"ohb4{i}", [128, 4, 32], BF16, st) for i in range(2)]
                h2f = sb("h2f", [128, D], F32, st)
                mdK = sb("mdK", [128, NTILE, 32], F32, st)
                mdP = sb("mdP", [128, NT, 2, 32], F32, st)
                h2tok = [sb(f"h2tok{i}", [128, 4, D], BF16, st) for i in range(2)]
                T.op("dve", lambda: nc.vector.memset(cum[:], 0.0), writes=["cum"])
                T.op("dve", lambda: nc.vector.memset(cumb[0][:], 0.0), writes=[("cumb", 0)])
                T.op("pool", lambda: nc.gpsimd.dma_start(out=wout[:], in_=w_out[l].rearrange("(k p) n -> p k n", p=128),
                                                         max_dma_last_dim=4096),
                     writes=["wout"], dsem="wout")
                T.op("pool", lambda: nc.gpsimd.dma_start(out=wr[:], in_=w_r[l].rearrange("(k p) n -> p k n", p=128)),
                     writes=["wr"], dsem="wr")
                for dst, key, src in ((brb, "brb", b_r), (l1g, "l1g", ln1_g), (l1b, "l1b", ln1_b)):
                    T.op(DQ, lambda: DQE.dma_start(out=dst[:], in_=src[l].partition_broadcast(128)),
                         writes=[key], dsem=key)
                def c_loads(g_):
                    b_ = g_ % 2
                    T.op(DQ, lambda: DQE.dma_start(out=mixT[b_][:, 0:4, :], in_=ATT[:, :, g_ * 512:(g_ + 1) * 512].rearrange("h p t -> p h t")),
                         reads=[("ATT", h, g_) for h in range(4)], writes=[("mixTa", b_)], dsem=f"mixTa{b_}")
                    T.op(DQ, lambda: DQE.dma_start(out=mixT[b_][:, 4:8, :], in_=SGT[:, :, g_ * 512:(g_ + 1) * 512].rearrange("h p t -> p h t")),
                         reads=[("SGT", g_)], writes=[("mixTb", b_)], dsem=f"mixTb{b_}")
                    T.op(DQ, lambda: DQE.dma_start(out=xg[b_][:], in_=xsrc[g_ * 512:(g_ + 1) * 512, :].rearrange("(t p) d -> p t d", p=128)),
                         reads=[("X", l, g_)] if l > 0 else [], writes=[("xgc", b_)], dsem=f"xgc{b_}")

                for g in range(NG):
                    b = g % 2
                    t0 = g * 512
                    if g == 0:
                        c_loads(0)
                    if g + 1 < NG:
                        c_loads(g + 1)
                    for t in range(4):
                        yp = t % 2
                        ytmp, stats, mv, rstd = ytmp2[yp], stats2[yp], mv2[yp], rstd2[yp]
                        YK, SK, MK, RK = ("ytmp", yp), ("statsc", yp), ("mvc", yp), ("rstdc", yp)
                        for half in range(2):
                            bank = 2 + (t * 2 + half) % 6
                            for kc in range(8):
                                T.op("pe", lambda: nc.tensor.matmul(
                                    ps[bank][:], lhsT=mixT[b][:, kc, t * 128:(t + 1) * 128], rhs=wout[:, kc, half * 512:(half + 1) * 512],
                                    start=(kc == 0), stop=(kc == 7)),
                                    reads=[("mixTa", b), ("mixTb", b), "wout"], writes=[PK(bank)])
                            T.op("dve", lambda: nc.vector.tensor_tensor(
                                out=ytmp[:, half * 512:(half + 1) * 512], in0=ps[bank][:], in1=gt1p[:, half * 512:(half + 1) * 512], op=ALU.mult),
                                reads=[PK(bank), "gt1p"], writes=[YK])
                        T.op("dve", lambda: nc.vector.scalar_tensor_tensor(out=ytmp[:], in0=xg[b][:, t, :], scalar=ALPHA, in1=ytmp[:],
                                                                           op0=ALU.mult, op1=ALU.add),
                             reads=[("xgc", b), YK], writes=[YK])
                        for half in range(2):
                            T.op("dve", lambda: nc.vector.bn_stats(out=stats[:, half, :], in_=ytmp[:, half * 512:(half + 1) * 512]),
                                 reads=[YK], writes=[SK])
                        T.op("dve", lambda: nc.vector.bn_aggr(out=mv[:], in_=stats[:].rearrange("p a b -> p (a b)")),
                             reads=[SK], writes=[MK])
                        rsqrt_small(st, "r", rstd[:], mv[:, 1:2], 1, [MK], RK)
                        T.op("dve", lambda: nc.vector.tensor_scalar(out=ytmp[:], in0=ytmp[:], scalar1=mv[:, 0:1], scalar2=rstd[:, 0:1],
                                                                    op0=ALU.subtract, op1=ALU.mult),
                             reads=[YK, MK, RK], writes=[YK])
                        T.op("pool", lambda: nc.gpsimd.tensor_tensor(out=ytmp[:], in0=ytmp[:], in1=l1g[:], op=ALU.mult),
                             reads=[YK, "l1g"], writes=[YK])
                        T.op("pool", lambda: nc.gpsimd.tensor_tensor(out=x1g[b][:, t, :], in0=ytmp[:], in1=l1b[:], op=ALU.add),
                             reads=[YK, "l1b"], writes=[("x1g", b, t)])
                    X1G = [("x1g", b, t) for t in range(4)]
                    for kc in range(8):
                        bank = kc % 2
                        for t in range(4):
                            T.op("pe", lambda: nc.tensor.transpose(ps[bank][:, t * 128:(t + 1) * 128],
                                                                   x1g[b][:, t, kc * 128:(kc + 1) * 128], ident[:]),
                                 reads=[("x1g", b, t), "ident"], writes=[PK(bank)])
                        T.op("act", lambda: nc.scalar.activation(out=h2T[b][:, kc, :], in_=ps[bank][:], func=AF.Identity,
                                                                 bias=modcol[:, 16 + kc:17 + kc], scale=modcol[:, 24 + kc:25 + kc]),
                             reads=[PK(bank), "modcol"], writes=[("h2T", b)])
                    gq = g % 2
                    RK_ = [("rg", gq)]
                    lg, s4, ohg, gexp, sel, oh1, oh2, sel2, ohs8, Rg, t8, t4, prod, tmp32 = rg_s[gq]
                    LB = 2 + gq
                    RB = 4 + gq

                    def dv(fn, extra_r=(), w=None):
                        T.op("dve", fn, reads=RK_ + list(extra_r), writes=(RK_ if w is None else w))

                    def bc(ap, shape):
                        return ap.to_broadcast(shape)
                    for t in range(4):
                        for kc in range(8):
                            T.op("pe", lambda: nc.tensor.matmul(
                                ps[LB][:, t * 64:t * 64 + 36], lhsT=h2T[b][:, kc, t * 128:(t + 1) * 128], rhs=wr[:, kc, :],
                                start=(kc == 0), stop=(kc == 7)),
                                reads=[("h2T", b), "wr"], writes=[PK(LB)])
                    dv(lambda: nc.vector.tensor_tensor(
                        out=lg[:], in0=ps[LB][:, 0:256].rearrange("p (t c) -> p t c", c=64)[:, :, 0:36],
                        in1=bc(brb[:].unsqueeze(1), [128, 4, 36]), op=ALU.add), [PK(LB), "brb"])
                    dv(lambda: nc.vector.tensor_reduce(out=s4[:, :, 0], in_=lg[:, :, 0:4], axis=AX.X, op=ALU.max))
                    dv(lambda: nc.vector.tensor_tensor(out=ohg[:], in0=lg[:, :, 0:4], in1=bc(s4[:, :, 0:1], [128, 4, 4]), op=ALU.is_equal))
                    dv(lambda: nc.vector.tensor_tensor(out=gexp[:], in0=lg[:, :, 0:4], in1=bc(s4[:, :, 0:1], [128, 4, 4]), op=ALU.subtract))
                    T.op("act", lambda: nc.scalar.activation(out=gexp[:], in_=gexp[:], func=AF.Exp), reads=RK_, writes=RK_)
                    dv(lambda: nc.vector.tensor_reduce(out=s4[:, :, 1], in_=gexp[:], axis=AX.X, op=ALU.add))
                    dv(lambda: nc.vector.reciprocal(out=s4[:, :, 2], in_=s4[:, :, 1]))
                    dv(lambda: nc.vector.tensor_tensor(
                        out=prod[:], in0=lg[:, :, 4:36].rearrange("p t (g e) -> p t e g", g=4),
                        in1=bc(ohg[:].unsqueeze(2), [128, 4, 8, 4]), op=ALU.mult))
                    dv(lambda: nc.vector.tensor_reduce(out=sel[:], in_=prod[:], axis=AX.X, op=ALU.add))
                    dv(lambda: nc.vector.tensor_reduce(out=s4[:, :, 3], in_=sel[:], axis=AX.X, op=ALU.max))
                    dv(lambda: nc.vector.tensor_tensor(out=oh1[:], in0=sel[:], in1=bc(s4[:, :, 3:4], [128, 4, 8]), op=ALU.is_equal))
                    dv(lambda: nc.vector.scalar_tensor_tensor(
                        out=sel2[:].rearrange("p t e -> p (t e)"), in0=oh1[:].rearrange("p t e -> p (t e)"), scalar=NEG_BIG,
                        in1=sel[:].rearrange("p t e -> p (t e)"), op0=ALU.mult, op1=ALU.add))
                    dv(lambda: nc.vector.tensor_reduce(out=s4[:, :, 4], in_=sel2[:], axis=AX.X, op=ALU.max))
                    dv(lambda: nc.vector.tensor_tensor(out=oh2[:], in0=sel2[:], in1=bc(s4[:, :, 4:5], [128, 4, 8]), op=ALU.is_equal))
                    dv(lambda: nc.vector.tensor_tensor(out=s4[:, :, 5], in0=s4[:, :, 4], in1=s4[:, :, 3], op=ALU.subtract))
                    T.op("act", lambda: nc.scalar.activation(out=s4[:, :, 5], in_=s4[:, :, 5], func=AF.Exp), reads=RK_, writes=RK_)
                    dv(lambda: nc.vector.tensor_scalar(out=s4[:, :, 5], in0=s4[:, :, 5], scalar1=1.0, scalar2=None, op0=ALU.add))
                    dv(lambda: nc.vector.reciprocal(out=s4[:, :, 6], in_=s4[:, :, 5]))
                    dv(lambda: nc.vector.tensor_scalar(out=s4[:, :, 7], in0=s4[:, :, 6], scalar1=-1.0, scalar2=1.0, op0=ALU.mult, op1=ALU.add))
                    dv(lambda: nc.vector.tensor_tensor(out=s4[:, :, 6:8], in0=s4[:, :, 6:8], in1=bc(s4[:, :, 2:3], [128, 4, 2]), op=ALU.mult))
                    dv(lambda: nc.vector.tensor_tensor(out=ohs8[:], in0=oh1[:], in1=oh2[:], op=ALU.add))
                    dv(lambda: nc.vector.tensor_tensor(
                        out=ohb4[gq][:].rearrange("p t (g e) -> p t g e", g=4), in0=bc(ohs8[:].unsqueeze(2), [128, 4, 4, 8]),
                        in1=bc(ohg[:].unsqueeze(3), [128, 4, 4, 8]), op=ALU.mult), w=RK_ + [("ohb4", gq)])
                    for t in range(4):
                        mms = [(triu1_bf, ohb4[gq][:, t, :], "triu1")] + [(ones_bf, ohb4[gq][:, t2, :], "ones_bf") for t2 in range(t)] \
                            + [(ones_bf, cumb[gq][:], "ones_bf")]
                        for mi, (lh, rh, lk) in enumerate(mms):
                            T.op("pe", lambda: nc.tensor.matmul(ps[RB][:, t * 32:(t + 1) * 32], lhsT=lh[:], rhs=rh,
                                                                start=(mi == 0), stop=(mi == len(mms) - 1)),
                                 reads=[("ohb4", gq), ("cumb", gq), lk], writes=[PK(RB)])
                    T.op("dve", lambda: nc.vector.tensor_reduce(out=tmp32[:], in_=ohb4[gq][:].rearrange("p t e -> p e t"), axis=AX.X, op=ALU.add),
                         reads=[("ohb4", gq)] + RK_, writes=RK_)
                    T.op("dve", lambda: nc.vector.tensor_tensor(out=cum[:], in0=cum[:], in1=tmp32[:], op=ALU.add),
                         reads=["cum"] + RK_, writes=["cum"])
                    T.op("dve", lambda: nc.vector.tensor_copy(out=cumb[1 - gq][:], in_=cum[:]),
                         reads=["cum"], writes=[("cumb", 1 - gq)])
                    dv(lambda: nc.vector.tensor_tensor(
                        out=prod[:], in0=ps[RB][:, 0:128].rearrange("p (t g e) -> p t e g", t=4, g=4),
                        in1=bc(ohg[:].unsqueeze(2), [128, 4, 8, 4]), op=ALU.mult), [PK(RB)])
                    dv(lambda: nc.vector.tensor_reduce(out=Rg[:], in_=prod[:], axis=AX.X, op=ALU.add))
                    dv(lambda: nc.vector.tensor_tensor(out=t8[:], in0=oh1[:], in1=Rg[:], op=ALU.mult))
                    dv(lambda: nc.vector.tensor_reduce(out=s4[:, :, 13], in_=t8[:], axis=AX.X, op=ALU.add))
                    dv(lambda: nc.vector.tensor_tensor(out=t8[:], in0=oh2[:], in1=Rg[:], op=ALU.mult))
                    dv(lambda: nc.vector.tensor_reduce(out=s4[:, :, 14], in_=t8[:], axis=AX.X, op=ALU.add))
                    dv(lambda: nc.vector.tensor_tensor(out=t4[:], in0=ohg[:], in1=bc(iota32[:, 0:4].unsqueeze(1), [128, 4, 4]), op=ALU.mult), ["iota32"])
                    dv(lambda: nc.vector.tensor_reduce(out=s4[:, :, 8], in_=t4[:], axis=AX.X, op=ALU.add))
                    dv(lambda: nc.vector.tensor_tensor(out=t8[:], in0=oh1[:], in1=bc(iota32[:, 0:8].unsqueeze(1), [128, 4, 8]), op=ALU.mult), ["iota32"])
                    dv(lambda: nc.vector.tensor_reduce(out=s4[:, :, 9], in_=t8[:], axis=AX.X, op=ALU.add))
                    dv(lambda: nc.vector.tensor_tensor(out=t8[:], in0=oh2[:], in1=bc(iota32[:, 0:8].unsqueeze(1), [128, 4, 8]), op=ALU.mult), ["iota32"])
                    dv(lambda: nc.vector.tensor_reduce(out=s4[:, :, 10], in_=t8[:], axis=AX.X, op=ALU.add))
                    dv(lambda: nc.vector.tensor_scalar(out=s4[:, :, 8], in0=s4[:, :, 8], scalar1=8.0, scalar2=None, op0=ALU.mult))
                    dv(lambda: nc.vector.tensor_tensor(out=s4[:, :, 11:13], in0=s4[:, :, 9:11], in1=bc(s4[:, :, 8:9], [128, 4, 2]), op=ALU.add))
                    TMK = [("tokmeta", g * 4 + t) for t in range(4)]
                    dv(lambda: nc.vector.tensor_copy(out=tokmeta[:, g * 4:(g + 1) * 4, 0:2], in_=s4[:, :, 11:13]), w=RK_ + TMK)
                    dv(lambda: nc.vector.tensor_copy(out=tokmeta[:, g * 4:(g + 1) * 4, 2:4], in_=s4[:, :, 13:15]), w=RK_ + TMK)
                    dv(lambda: nc.vector.tensor_copy(out=tokmeta[:, g * 4:(g + 1) * 4, 4:6], in_=s4[:, :, 6:8]), w=RK_ + TMK)
                    for t in range(4):
                        T.op("pool", lambda: nc.gpsimd.tensor_tensor(out=h2f[:], in0=x1g[b][:, t, :], in1=sc2b[:], op=ALU.mult),
                             reads=[("x1g", b, t), "sc2b"], writes=["h2f"])
                        T.op("pool", lambda: nc.gpsimd.tensor_tensor(out=h2tok[b][:, t, :], in0=h2f[:], in1=sh2b[:], op=ALU.add),
                             reads=["h2f", "sh2b"], writes=[("h2tok", b, t)])
                    T.op("pool", lambda: nc.gpsimd.dma_start(out=H2[t0:t0 + 512, :].rearrange("(t p) d -> p t d", p=128), in_=h2tok[b][:]),
                         reads=[("h2tok", b, t) for t in range(4)], writes=[("H2", g)], dsem=f"sth2{b}")
                    T.op("pool", lambda: nc.gpsimd.dma_start(out=X1[t0:t0 + 512, :].rearrange("(t p) d -> p t d", p=128), in_=x1g[b][:]),
                         reads=X1G, writes=[("X1", g)], dsem=f"stx1{b}")

                MB = 5
                fin = NG % 2
                T.op("pe", lambda: nc.tensor.matmul(ps[MB][:, 0:32], lhsT=ones_bf[:], rhs=cumb[fin][:], start=True, stop=True),
                     reads=[("cumb", fin), "ones_bf"], writes=[PK(MB)])
                M_ = ["md"]

                def dm(fn, extra_r=(), w=M_):
                    T.op("dve", fn, reads=M_ + list(extra_r), writes=w)
                cnt_, ntl_, off_, end_, tmp_, tmp2_ = (md[:, i * 32:(i + 1) * 32] for i in range(6))
                dm(lambda: nc.vector.tensor_copy(out=cnt_, in_=ps[MB][:, 0:32]), [PK(MB)])
                dm(lambda: nc.vector.tensor_scalar(out=ntl_, in0=cnt_, scalar1=0.0, scalar2=None, op0=ALU.is_gt))
                for j in range(1, NG):
                    dm(lambda: nc.vector.scalar_tensor_tensor(out=ntl_, in0=cnt_, scalar=j * 512.0, in1=ntl_, op0=ALU.is_gt, op1=ALU.add))
                dm(lambda: nc.vector.tensor_scalar(out=ntl_, in0=ntl_, scalar1=512.0, scalar2=None, op0=ALU.mult))
                dm(lambda: nc.vector.memset(md[:, 64:65], 0.0))
                for e in range(1, NE):
                    dm(lambda: nc.vector.tensor_tensor(out=md[:, 64 + e:65 + e], in0=md[:, 63 + e:64 + e], in1=md[:, 31 + e:32 + e], op=ALU.add))
                dm(lambda: nc.vector.tensor_tensor(out=end_, in0=off_, in1=ntl_, op=ALU.add))
                dm(lambda: nc.vector.tensor_tensor(out=mdK[:], in0=end_.unsqueeze(1).to_broadcast([128, NTILE, 32]),
                                                   in1=kt512[:, 0:NTILE].unsqueeze(2).to_broadcast([128, NTILE, 32]), op=ALU.is_le),
                   ["kt512"], w=M_ + ["mdK"])
                dm(lambda: nc.vector.tensor_reduce(out=tl[:, 0, :], in_=mdK[:], axis=AX.X, op=ALU.add), ["mdK"], w=M_ + ["tl"])
                TL = ["tl"]

                def dt_(fn, extra_r=(), w=TL):
                    T.op("dve", fn, reads=TL + list(extra_r), writes=w)
                dt_(lambda: nc.vector.tensor_scalar(out=tl[:, 2, :], in0=tl[:, 0, :], scalar1=31.5, scalar2=None, op0=ALU.is_le))
                dt_(lambda: nc.vector.tensor_scalar(out=tl[:, 3, :], in0=tl[:, 0, :], scalar1=31.0, scalar2=None, op0=ALU.min))
                dt_(lambda: nc.vector.tensor_scalar(out=widxf[:], in0=tl[:, 3, :], scalar1=128.0, scalar2=pcol[:, 0:1], op0=ALU.mult, op1=ALU.add),
                    ["pcol"], w=TL + ["widxf"])
                T.op("dve", lambda: nc.vector.tensor_copy(out=widx_i[:], in_=widxf[:]), reads=["widxf"], writes=["widx"])
                TMALL = [("tokmeta", i_) for i_ in range(NT)]
                dm(lambda: nc.vector.tensor_tensor(out=mdP[:], in0=iota32[:].unsqueeze(1).unsqueeze(1).to_broadcast([128, NT, 2, 32]),
                                                   in1=tokmeta[:, :, 0:2].unsqueeze(3).to_broadcast([128, NT, 2, 32]), op=ALU.is_equal),
                   ["iota32"] + TMALL, w=M_ + ["mdP"])
                dm(lambda: nc.vector.tensor_tensor(out=mdP[:], in0=mdP[:], in1=off_.unsqueeze(1).unsqueeze(1).to_broadcast([128, NT, 2, 32]), op=ALU.mult),
                   ["mdP"], w=M_ + ["mdP"])
                dm(lambda: nc.vector.tensor_reduce(out=poscf[:], in_=mdP[:], axis=AX.X, op=ALU.add), ["mdP"], w=M_ + ["poscf"])
                T.op("dve", lambda: nc.vector.tensor_tensor(out=poscf[:], in0=poscf[:], in1=tokmeta[:, :, 2:4], op=ALU.add),
                     reads=["poscf"] + [("tokmeta", i_) for i_ in range(NT)], writes=["poscf"])
                T.op("dve", lambda: nc.vector.tensor_copy(out=posc_i[:], in_=poscf[:].rearrange("p n j -> p (n j)")),
                     reads=["poscf"], writes=["posc"])

                for g in range(NG):
                    b = g % 2
                    t0 = g * 512
                    T.op("pool", lambda: nc.gpsimd.dma_start(out=h2tok[b][:], in_=H2[t0:t0 + 512, :].rearrange("(t p) d -> p t d", p=128)),
                         reads=[("H2", g)], writes=[("h2tok", b, t) for t in range(4)], dsem=f"ldh2{b}")
                    for t in range(4):
                        tile_i = g * 4 + t
                        for j in range(2):
                            T.op("pool", lambda: nc.gpsimd.indirect_dma_start(
                                out=HS, out_offset=bass.IndirectOffsetOnAxis(ap=posc_i[:, tile_i * 2 + j:tile_i * 2 + j + 1], axis=0),
                                in_=h2tok[b][:, t, :], in_offset=None, bounds_check=reg_hs, oob_is_err=False),
                                reads=[("h2tok", b, t), "posc"], writes=[("HS", tile_i, j)], dsem=f"sc{b}{j}")

            HSKEYS = [("HS", i_, j) for i_ in range(NT) for j in range(2)]
            with contextlib.ExitStack() as st:
                hs = [sb(f"hs{i}", [128, 4, D], BF16, st) for i in range(2)]
                wall = [sb(f"wall{i}", [128, 12288], BF16, st) for i in range(2)]
                hTs = [sb(f"hTs{i}", [128, 8, 512], BF16, st) for i in range(2)]
                aT = [sb(f"aT{i}", [128, 4, 512], BF16, st) for i in range(2)]
                sgt = [sb(f"sgt{i}", [128, 512], F32, st) for i in range(2)]
                yout = [sb(f"yout{i}", [128, 4, D], F32, st) for i in range(2)]
                psb = [ps[i][:].bitcast(BF16) for i in range(2)]
                WBKEYS = [("WB", e) for e in range(NE)]
                ev = 0

                def d_loads(k_):
                    hb_ = k_ % 2
                    T.op("pool", lambda: nc.gpsimd.dma_start(out=hs[hb_][:], in_=HS[k_ * 512:(k_ + 1) * 512, :].rearrange("(s p) d -> p s d", p=128)),
                         reads=HSKEYS, writes=[("hs", hb_)], dsem=f"hs{hb_}")
                    T.op("pool", lambda: nc.gpsimd.indirect_dma_start(
                        out=wall[hb_][:], out_offset=None, in_=WB,
                        in_offset=bass.IndirectOffsetOnAxis(ap=widx_i[:, k_:k_ + 1], axis=0),
                        bounds_check=reg_w, oob_is_err=False),
                        reads=["widx"] + WBKEYS, writes=[("wall", hb_)], dsem=f"wall{hb_}")

                def d_transposes(k_, kcs):
                    hb_ = k_ % 2
                    for kc in kcs:
                        bank = kc % 2
                        for s_ in range(4):
                            T.op("pe", lambda: nc.tensor.transpose(psb[bank][:, s_ * 128:(s_ + 1) * 128],
                                                                   hs[hb_][:, s_, kc * 128:(kc + 1) * 128], ident_bf[:]),
                                 reads=[("hs", hb_), "ident_bf"], writes=[PK(bank)])
                        if kc % 2 == 0:
                            T.op("act", lambda: nc.scalar.copy(out=hTs[hb_][:, kc, :], in_=psb[bank][:, 0:512]),
                                 reads=[PK(bank)], writes=[("hTs", hb_, kc)])
                        else:
                            T.op("dve", lambda: nc.vector.tensor_copy(out=hTs[hb_][:, kc, :], in_=psb[bank][:, 0:512]),
                                 reads=[PK(bank)], writes=[("hTs", hb_, kc)])

                for k in range(NTILE):
                    hb = k % 2
                    if k == 0:
                        d_loads(0)
                    if k + 1 < NTILE:
                        d_loads(k + 1)
                    if k == 0:
                        d_transposes(0, range(8))
                    for fc in range(4):
                        bg = 2 + (fc % 2) * 2
                        bu = 3 + (fc % 2) * 2
                        for kc in range(8):
                            T.op("pe", lambda: nc.tensor.matmul(
                                ps[bg][:], lhsT=wall[hb][:, kc * 512 + fc * 128: kc * 512 + (fc + 1) * 128], rhs=hTs[hb][:, kc, :],
                                start=(kc == 0), stop=(kc == 7)), reads=[("wall", hb), ("hTs", hb, kc)], writes=[PK(bg)])
                        for kc in range(8):
                            T.op("pe", lambda: nc.tensor.matmul(
                                ps[bu][:], lhsT=wall[hb][:, 4096 + kc * 512 + fc * 128: 4096 + kc * 512 + (fc + 1) * 128], rhs=hTs[hb][:, kc, :],
                                start=(kc == 0), stop=(kc == 7)), reads=[("wall", hb), ("hTs", hb, kc)], writes=[PK(bu)])
                        si = fc % 2
                        T.op("act", lambda: nc.scalar.activation(out=sgt[si][:], in_=ps[bg][:], func=AF.Silu),
                             reads=[PK(bg)], writes=[("sgt", si)])
                        T.op("dve", lambda: nc.vector.tensor_tensor(out=aT[hb][:, fc, :], in0=ps[bu][:], in1=sgt[si][:], op=ALU.mult),
                             reads=[PK(bu), ("sgt", si)], writes=[("aT", hb)])
                        if k + 1 < NTILE and fc == 1:
                            d_transposes(k + 1, range(0, 4))
                        if k + 1 < NTILE and fc == 3:
                            d_transposes(k + 1, range(4, 8))
                    for t in range(4):
                        for half in range(2):
                            bank = 6 + ev % 2
                            for fc in range(4):
                                T.op("pe", lambda: nc.tensor.matmul(
                                    ps[bank][:], lhsT=aT[hb][:, fc, t * 128:(t + 1) * 128],
                                    rhs=wall[hb][:, 8192 + fc * 1024 + half * 512: 8192 + fc * 1024 + (half + 1) * 512],
                                    start=(fc == 0), stop=(fc == 3)), reads=[("aT", hb), ("wall", hb)], writes=[PK(bank)])
                            if ev % 2 == 0:
                                T.op("act", lambda: nc.scalar.copy(out=yout[hb][:, t, half * 512:(half + 1) * 512], in_=ps[bank][:]),
                                     reads=[PK(bank)], writes=[("yout", hb, t, half)])
                            else:
                                T.op("dve", lambda: nc.vector.tensor_copy(out=yout[hb][:, t, half * 512:(half + 1) * 512], in_=ps[bank][:]),
                                     reads=[PK(bank)], writes=[("yout", hb, t, half)])
                            ev += 1
                    T.op("pool", lambda: nc.gpsimd.dma_start(out=YS[k * 512:(k + 1) * 512, :].rearrange("(s p) d -> p s d", p=128), in_=yout[hb][:]),
                         reads=[("yout", hb, t, half) for t in range(4) for half in range(2)], writes=[("YS", k)], dsem=f"sty{hb}")

            YSKEYS = [("YS", k) for k in range(NTILE)]
            with contextlib.ExitStack() as st:
                l2g = sb("l2g", [128, D], F32, st)
                l2b = sb("l2b", [128, D], F32, st)
                x1t = [sb(f"x1t{i}", [128, D], F32, st) for i in range(2)]
                yg = [sb(f"yg{i}", [128, 2, D], F32, st) for i in range(2)]
                ff = [sb(f"ff{i}", [128, D], F32, st) for i in range(2)]
                yo = [sb(f"yo{i}", [128, D], F32, st) for i in range(2)]
                stats = sb("statsd", [128, 2, 6], F32, st)
                mv = sb("mvd", [128, 2], F32, st)
                rstd = sb("rstdd", [128, 1], F32, st)
                for dst, key, src in ((l2g, "l2g", ln2_g), (l2b, "l2b", ln2_b)):
                    T.op(DQ, lambda: DQE.dma_start(out=dst[:], in_=src[l].partition_broadcast(128)),
                         writes=[key], dsem=key)
                def e_loads(gt_i):
                    r0 = gt_i * 128
                    xb = gt_i % 2
                    T.op(DQ, lambda: DQE.dma_start(out=x1t[xb][:], in_=X1[r0:r0 + 128, :]),
                         reads=[("X1", gt_i // 4)], writes=[("x1t", xb)], dsem=f"x1t{xb}")
                    for j in range(2):
                        T.op("pool", lambda: nc.gpsimd.indirect_dma_start(
                            out=yg[xb][:, j, :], out_offset=None, in_=YS,
                            in_offset=bass.IndirectOffsetOnAxis(ap=posc_i[:, gt_i * 2 + j:gt_i * 2 + j + 1], axis=0),
                            bounds_check=reg_ys, oob_is_err=False),
                            reads=YSKEYS + ["posc"], writes=[("yg", xb)], dsem=f"yg{xb}", nowaw=True)

                e_loads(0)
                for gt_i in range(NT):
                    r0 = gt_i * 128
                    xb = gt_i % 2
                    if gt_i + 1 < NT:
                        e_loads(gt_i + 1)
                    A = [("ff", xb)]
                    T.op("act", lambda: nc.scalar.activation(out=ff[xb][:], in_=yg[xb][:, 0, :], func=AF.Identity, scale=tokmeta[:, gt_i, 4:5]),
                         reads=[("yg", xb), ("tokmeta", gt_i)], writes=A)
                    T.op("act", lambda: nc.scalar.activation(out=yg[xb][:, 1, :], in_=yg[xb][:, 1, :], func=AF.Identity, scale=tokmeta[:, gt_i, 5:6]),
                         reads=[("yg", xb), ("tokmeta", gt_i)], writes=[("yg", xb)])
                    T.op("dve", lambda: nc.vector.tensor_tensor(out=ff[xb][:], in0=ff[xb][:], in1=yg[xb][:, 1, :], op=ALU.add),
                         reads=[("yg", xb)] + A, writes=A)
                    T.op("dve", lambda: nc.vector.tensor_tensor(out=ff[xb][:], in0=ff[xb][:], in1=gt2p[:], op=ALU.mult),
                         reads=A + ["gt2p"], writes=A)
                    T.op("dve", lambda: nc.vector.scalar_tensor_tensor(out=ff[xb][:], in0=x1t[xb][:], scalar=ALPHA, in1=ff[xb][:],
                                                                       op0=ALU.mult, op1=ALU.add),
                         reads=A + [("x1t", xb)], writes=A)
                    for half in range(2):
                        T.op("dve", lambda: nc.vector.bn_stats(out=stats[:, half, :], in_=ff[xb][:, half * 512:(half + 1) * 512]),
                             reads=A, writes=["statsd"])
                    T.op("dve", lambda: nc.vector.bn_aggr(out=mv[:], in_=stats[:].rearrange("p a b -> p (a b)")),
                         reads=["statsd"], writes=["mvd"])
                    rsqrt_small(st, "rstdd", rstd[:], mv[:, 1:2], 1, ["mvd"], "rstdd")
                    T.op("dve", lambda: nc.vector.scalar_tensor_tensor(out=mv[:, 1:2], in0=mv[:, 0:1], scalar=-1.0, in1=rstd[:, 0:1],
                                                                       op0=ALU.mult, op1=ALU.mult),
                         reads=["mvd", "rstdd"], writes=["mvd"])
                    T.op("act", lambda: nc.scalar.activation(out=ff[xb][:], in_=ff[xb][:], func=AF.Identity,
                                                             scale=rstd[:, 0:1], bias=mv[:, 1:2]),
                         reads=A + ["mvd", "rstdd"], writes=A)
                    T.op("dve", lambda: nc.vector.tensor_tensor(out=ff[xb][:], in0=ff[xb][:], in1=l2g[:], op=ALU.mult),
                         reads=A + ["l2g"], writes=A)
                    T.op("dve", lambda: nc.vector.tensor_tensor(out=yo[xb][:], in0=ff[xb][:], in1=l2b[:], op=ALU.add),
                         reads=A + ["l2b"], writes=[("yo", xb)])
                    T.op(DQ, lambda: DQE.dma_start(out=xdst[r0:r0 + 128, :], in_=yo[xb][:]),
                         reads=[("yo", xb)], writes=[("Xt", l + 1, gt_i)], dsem=f"sto{xb}")
                for g in range(NG):
                    T.writers[("X", l + 1, g)] = {}
                    for lt4 in range(4):
                        for s_, v in T.writers.get(("Xt", l + 1, g * 4 + lt4), {}).items():
                            if T.writers[("X", l + 1, g)].get(s_, 0) < v:
                                T.writers[("X", l + 1, g)][s_] = v

        T.final_wait(DQ, [("Xt", L, i) for i in range(NT)])
        print(f"[build] ops={T.n_ops} waits={T.n_waits} sems={len(T.sem)}")
    return nc


def rope_tables(S):
    inv = 1.0 / (10000.0 ** (np.arange(0, 64, 2, dtype=np.float32) / 64.0))
    ang = np.arange(S, dtype=np.float32)[:, None] * inv[None, :].astype(np.float32)
    cos = np.cos(ang).astype(np.float32)
    sin = np.sin(ang).astype(np.float32)
    cosT = np.empty((128, S), np.float32)
    sinT = np.empty((128, S), np.float32)
    for p in range(128):
        d = p % 64
        cosT[p] = cos[:, d % 32]
        sinT[p] = -sin[:, d % 32] if d < 32 else sin[:, d % 32]
    return cosT, sinT


def prep_shared(inp, S, L):
    f = lambda a: np.ascontiguousarray(np.asarray(a, dtype=np.float32))
    w_in = np.asarray(inp["w_in"], dtype=np.float32)[:L]
    q = w_in[:, :, 0:512].reshape(L, D, 4, 2, 64)
    k = w_in[:, :, 512:1024].reshape(L, D, 4, 2, 64)
    swap = lambda t: np.concatenate([t[..., 32:], t[..., :32]], axis=-1).reshape(L, D, 512)
    w_in_x = np.concatenate([w_in[:, :, 0:512], swap(q), w_in[:, :, 512:1024], swap(k), w_in[:, :, 1024:2560]], axis=-1)
    lam4 = np.stack([inp["lambda_q1"], inp["lambda_k1"], inp["lambda_q2"], inp["lambda_k2"]], axis=1)[:L]
    w_r = np.concatenate([np.asarray(inp["w_group"])[:L], np.transpose(np.asarray(inp["w_router"])[:L], (0, 2, 1, 3)).reshape(L, D, 32)], axis=-1)
    b_r = np.concatenate([np.asarray(inp["b_group"])[:L], np.asarray(inp["b_router"])[:L].reshape(L, 32)], axis=-1)
    wg = np.asarray(inp["w_gate"], dtype=np.float32)[:L].reshape(L, NE, 8, 128, DFF).transpose(0, 1, 3, 2, 4).reshape(L, NE, 128, 8 * DFF)
    wu = np.asarray(inp["w_up"], dtype=np.float32)[:L].reshape(L, NE, 8, 128, DFF).transpose(0, 1, 3, 2, 4).reshape(L, NE, 128, 8 * DFF)
    wd = np.asarray(inp["w_down"], dtype=np.float32)[:L].reshape(L, NE, 4, 128, D).transpose(0, 1, 3, 2, 4).reshape(L, NE, 128, 4 * D)
    wall = np.ascontiguousarray(np.concatenate([wg, wu, wd], axis=-1).reshape(L, NE * 128, 12288))
    del wg, wu, wd
    cosT, sinT = rope_tables(S)
    tri = np.triu(np.ones((128, 128), np.float32))
    sh = {
        "w_ada": f(inp["w_ada"][:L]), "b_ada": f(inp["b_ada"][:L]), "w_in": f(w_in_x), "lam4": f(lam4),
        "subln_g": f(inp["subln_g"][:L]), "sg_ln_g": f(np.asarray(inp["sg_ln_g"])[:L].reshape(L, 512)),
        "sg_ln_b": f(np.asarray(inp["sg_ln_b"])[:L].reshape(L, 512)),
        "w_spT": f(np.transpose(np.asarray(inp["w_spatial"])[:L], (0, 1, 3, 2))),
        "b_sp": f(np.asarray(inp["b_spatial"])[:L].reshape(L, 512)),
        "w_out": f(inp["w_out"][:L]), "ln1_g": f(inp["ln1_g"][:L]), "ln1_b": f(inp["ln1_b"][:L]),
        "w_r": f(w_r), "b_r": f(b_r),
        "wall": wall, "triu1": np.triu(np.ones((128, 128), np.float32), 1),
        "pcol": np.stack([np.arange(128, dtype=np.float32), 6.0 * np.arange(128, dtype=np.float32)], axis=1),
        "iota32": np.ascontiguousarray(np.broadcast_to(np.arange(32, dtype=np.float32), (128, 32))),
        "kt512": np.ascontiguousarray(np.broadcast_to(512.0 * np.arange(64, dtype=np.float32), (128, 64))),
        "ln2_g": f(inp["ln2_g"][:L]), "ln2_b": f(inp["ln2_b"][:L]),
        "ident": np.eye(128, dtype=np.float32), "tri": tri, "csT": np.ascontiguousarray(np.stack([cosT, sinT], axis=1)),
    }
    return sh


def run(inp, S, L, n_cores, dbg=False, trace=False):
    nc = build_program(S, L, dbg=dbg)
    sh = prep_shared(inp, S, L)
    x = np.asarray(inp["x"], dtype=np.float32)
    c = np.asarray(inp["c"], dtype=np.float32)
    in_maps = []
    for b in range(n_cores):
        m = dict(sh)
        m["x"] = np.ascontiguousarray(x[b])
        m["cT"] = np.ascontiguousarray(c[b].reshape(8, 128).T)
        in_maps.append(m)
    res = run_bass_kernel_spmd(nc, in_maps, core_ids=list(range(n_cores)), trace=trace)
    return res


def kernel(**inputs):
    S = inputs["x"].shape[1]
    res = run(inputs, S, 2, 8)
    return np.stack([np.asarray(r["out"], dtype=np.float32) for r in res.results], axis=0)
```

```python
import contextlib
import math
import numpy as np
import concourse.bass as bass
import concourse.mybir as mybir
from concourse.bass_utils import run_bass_kernel_spmd

F32 = mybir.dt.float32
BF16 = mybir.dt.bfloat16
AF = mybir.ActivationFunctionType
ALU = mybir.AluOpType
AX = mybir.AxisListType

D = 1024
NKC = 8
NE = 32
DFF = 512
INX = 3584
ALPHA = (2.0 * 2) ** 0.25
LN_EPS = 1e-5
NEG_BIG = -1.0e30


class Tracker:
    def __init__(self, nc, es):
        self.nc = nc
        self.es = es
        self.eng = {"pe": nc.tensor, "act": nc.scalar, "dve": nc.vector, "pool": nc.gpsimd, "sp": nc.sync}
        self.sem = {}
        self.cnt = {}
        self.pool = [es.enter_context(nc.semaphore(f"sm{i}")) for i in range(88)]
        for h in self.pool:
            nc.gpsimd.sem_clear(h)
        nc.all_engine_barrier()
        for k in self.eng:
            self.sem[k] = self.pool.pop()
            self.cnt[k] = 0
        self.seen = {k: {} for k in self.eng}
        self.writers = {}
        self.readers = {}
        self.n_ops = 0
        self.n_waits = 0

    def _dsem(self, name):
        if name not in self.sem:
            self.sem[name] = self.pool.pop()
            self.cnt[name] = 0
        return name

    def op(self, engname, fn, reads=(), writes=(), dsem=None, nowaw=False):
        deps = {}
        for k in reads:
            for s, v in self.writers.get(k, {}).items():
                if deps.get(s, 0) < v:
                    deps[s] = v
        for k in writes:
            for s, v in self.writers.get(k, {}).items():
                if nowaw and s == dsem:
                    continue
                if deps.get(s, 0) < v:
                    deps[s] = v
            for s, v in self.readers.get(k, {}).items():
                if deps.get(s, 0) < v:
                    deps[s] = v
        seen = self.seen[engname]
        e = self.eng[engname]
        for s, v in deps.items():
            if s == engname and engname == "pe" and dsem is None:
                continue
            if seen.get(s, 0) >= v:
                continue
            e.wait_ge(self.sem[s], v)
            seen[s] = v
            self.n_waits += 1
        ins = fn()
        self.n_ops += 1
        if dsem is not None:
            self._dsem(dsem)
            self.cnt[dsem] += 16
            ins.then_inc(self.sem[dsem], 16)
            ev = (dsem, self.cnt[dsem])
        else:
            self.cnt[engname] += 1
            ins.then_inc(self.sem[engname], 1)
            ev = (engname, self.cnt[engname])
        for k in writes:
            self.writers[k] = {ev[0]: ev[1]}
            self.readers[k] = {}
        for k in reads:
            r = self.readers.setdefault(k, {})
            if r.get(ev[0], 0) < ev[1]:
                r[ev[0]] = ev[1]

    def final_wait(self, engname, keys):
        deps = {}
        for k in keys:
            for s, v in self.writers.get(k, {}).items():
                if deps.get(s, 0) < v:
                    deps[s] = v
        for s, v in deps.items():
            self.eng[engname].wait_ge(self.sem[s], v)


def build_program(S, L, dbg=False):
    assert S % 512 == 0
    NG = S // 512
    NT = S // 128
    TB = min(S, 1024)
    NBLK = S // TB
    nc = bass.Bass("TRN2", target_bir_lowering=False)

    def din(name, shape, dt=F32):
        return nc.dram_tensor(name, list(shape), dt, kind="ExternalInput").ap()

    def dscr(name, shape, dt):
        return nc.dram_tensor(name, list(shape), dt, kind=("ExternalOutput" if dbg else "Internal")).ap()

    x_in = din("x", [S, D])
    cT_in = din("cT", [128, 8])
    w_ada = din("w_ada", [L, D, 6 * D])
    b_ada = din("b_ada", [L, 6 * D])
    w_in = din("w_in", [L, D, INX])
    lam_in = din("lam4", [L, 4, 64])
    subln_g = din("subln_g", [L, 128])
    sg_ln_g = din("sg_ln_g", [L, 512])
    sg_ln_b = din("sg_ln_b", [L, 512])
    w_spT = din("w_spT", [L, 4, 128, 128])
    b_sp = din("b_sp", [L, 512])
    w_out = din("w_out", [L, D, D])
    ln1_g = din("ln1_g", [L, D])
    ln1_b = din("ln1_b", [L, D])
    w_r = din("w_r", [L, D, 36])
    b_r = din("b_r", [L, 36])
    wall_in = din("wall", [L, NE * 128, 12288])
    triu1_in = din("triu1", [128, 128])
    pcol_in = din("pcol", [128, 2])
    iota_in = din("iota32", [128, 32])
    kt_in = din("kt512", [128, 64])
    ln2_g = din("ln2_g", [L, D])
    ln2_b = din("ln2_b", [L, D])
    ident_in = din("ident", [128, 128])
    tri_in = din("tri", [128, 128])
    cs_in = din("csT", [128, 2, S])
    out = nc.dram_tensor("out", [S, D], F32, kind="ExternalOutput").ap()

    QT = dscr("QT", [4, 128, S], BF16)
    KT = dscr("KT", [4, 128, S], BF16)
    VS = dscr("VS", [S, 512], BF16)
    SGT = dscr("SGT", [4, 128, S], BF16)
    ATT = dscr("ATT", [4, 128, S], BF16)
    X1 = dscr("X1", [S, D], F32)
    X2 = dscr("X2", [S, D], F32)
    CAP = S
    NTILE = (2 * S + NE * 511) // 512
    HS = dscr("HS", [NTILE * 512, D], BF16)
    H2 = dscr("H2", [S, D], BF16)
    WB = dscr("WB", [NE * 128, 12288], BF16)
    YS = dscr("YS", [NTILE * 512, D], F32)

    es = contextlib.ExitStack()
    with es:
        T = Tracker(nc, es)
        DQ, DQE = "pool", nc.gpsimd
        reg_hs = nc.gpsimd.to_reg(NTILE * 512 - 1)
        reg_w = nc.gpsimd.to_reg(NE * 128 - 1)
        reg_ys = nc.gpsimd.to_reg(NTILE * 512 - 1)

        uniq = [0]

        def sb(name, shape, dt, stack=es):
            uniq[0] += 1
            return stack.enter_context(nc.sbuf_tensor(f"sb{uniq[0]}_{name}", list(shape), dt))

        ps = [es.enter_context(nc.psum_tensor(f"ps{i}", [128, 512], F32)) for i in range(8)]

        def PK(i):
            return ("ps", i)

        ident = sb("ident", [128, 128], F32)
        tri_bf = sb("tri_bf", [128, 128], BF16)
        ones_bf = sb("ones_bf", [128, 128], BF16)
        onesdiv = sb("onesdiv", [128, 128], F32)
        ones_f32 = sb("ones_f32", [128, 128], F32)
        epscol = sb("epscol", [128, 1], F32)
        ones_row = sb("ones_row", [1, 128], F32)
        cTs = sb("cTs", [128, 8], F32)
        siluc = sb("siluc", [128, 8], F32)
        modcol = sb("modcol", [128, 32], F32)
        gt1p = sb("gt1p", [128, D], F32)
        gt2p = sb("gt2p", [128, D], F32)
        lamt = sb("lamt", [128, 4, 64], F32)
        lamw = sb("lamw", [128, 8], F32)
        neglam = sb("neglam", [128, 1], F32)
        gcol = sb("gcol", [128, 1], F32)
        sc2b = sb("sc2b", [128, D], F32)
        sh2b = sb("sh2b", [128, D], F32)
        tokmeta = sb("tokmeta", [128, NT, 8], F32)
        ident_bf = sb("ident_bf", [128, 128], BF16)
        triu1_bf = sb("triu1_bf", [128, 128], BF16)
        pcol = sb("pcol", [128, 2], F32)
        iota32 = sb("iota32", [128, 32], F32)
        kt512 = sb("kt512", [128, 64], F32)
        cum = sb("cum", [128, 32], F32)
        cumb = [sb(f"cumb{i}", [128, 32], BF16) for i in range(2)]
        ohb = [sb(f"ohb{i}", [128, 32], BF16) for i in range(2)]
        md = sb("md", [128, 192], F32)
        tl = sb("tl", [128, 5, NTILE], F32)
        widxf = sb("widxf", [128, NTILE], F32)
        widx_i = sb("widx_i", [128, NTILE], mybir.dt.int32)
        poscf = sb("poscf", [128, NT, 2], F32)
        posc_i = sb("posc_i", [128, NT * 2], mybir.dt.int32)

        T.op(DQ, lambda: DQE.dma_start(out=ident[:], in_=ident_in), writes=["ident"], dsem="c0")
        T.op("pool", lambda: nc.gpsimd.dma_start(out=tri_bf[:], in_=tri_in), writes=["tri"], dsem="c1")
        T.op(DQ, lambda: DQE.dma_start(out=cTs[:], in_=cT_in), writes=["cT"], dsem="c0")
        T.op("pool", lambda: nc.gpsimd.dma_start(out=ident_bf[:], in_=ident_in), writes=["ident_bf"], dsem="c2")
        T.op("pool", lambda: nc.gpsimd.dma_start(out=triu1_bf[:], in_=triu1_in), writes=["triu1"], dsem="c3")
        T.op("pool", lambda: nc.gpsimd.dma_start(out=pcol[:], in_=pcol_in), writes=["pcol"], dsem="c4")
        T.op("pool", lambda: nc.gpsimd.dma_start(out=iota32[:], in_=iota_in), writes=["iota32"], dsem="c5")
        T.op("pool", lambda: nc.gpsimd.dma_start(out=kt512[:], in_=kt_in), writes=["kt512"], dsem="c6")
        T.op("dve", lambda: nc.vector.memset(ones_bf[:], 1.0), writes=["ones_bf"])
        T.op("dve", lambda: nc.vector.memset(onesdiv[:], 1.0 / 128.0), writes=["onesdiv"])
        T.op("dve", lambda: nc.vector.memset(ones_f32[:], 1.0), writes=["ones_f32"])
        T.op("dve", lambda: nc.vector.memset(epscol[:], LN_EPS), writes=["epscol"])
        T.op("dve", lambda: nc.vector.memset(ones_row[:], 1.0), writes=["ones_row"])
        T.op("act", lambda: nc.scalar.activation(out=siluc[:], in_=cTs[:], func=AF.Silu),
             reads=["cT"], writes=["siluc"])

        def dump(name, ap, shape, keys, dt=F32):
            if not dbg:
                return
            dt_ = nc.dram_tensor("dbg_" + name, list(shape), dt, kind="ExternalOutput").ap()
            T.op(DQ, lambda: DQE.dma_start(out=dt_, in_=ap), reads=keys, writes=[("dbg", name)], dsem="dbg")

        def rsqrt_small(stack, name, dst, src_ap, n, rd_keys, wr_key):
            T.op("dve", lambda: nc.vector.tensor_scalar(out=dst, in0=src_ap, scalar1=LN_EPS, scalar2=None,
                                                        op0=ALU.add), reads=rd_keys, writes=[wr_key])
            T.op("act", lambda: nc.scalar.activation(out=dst, in_=dst, func=AF.Sqrt),
                 reads=[wr_key], writes=[wr_key])
            T.op("dve", lambda: nc.vector.reciprocal(out=dst, in_=dst), reads=[wr_key], writes=[wr_key])

        for l in range(L):
            lam_init = 0.8 - 0.6 * math.exp(-0.3 * l)
            xsrc = x_in if l == 0 else X2
            xdst = out if l == L - 1 else X2

            with contextlib.ExitStack() as st:
                wada = [sb(f"wada{i}", [128, 8, 1024], F32, st) for i in range(2)]
                modrow = sb("modrow", [1, 6 * D], F32, st)
                badarow = sb("badarow", [1, 6 * D], F32, st)
                T.op(DQ, lambda: DQE.dma_start(out=badarow[:], in_=b_ada[l:l + 1, :]),
                     writes=["badarow"], dsem="badarow")
                for bi, blk in enumerate(range(6)):
                    b = bi % 2
                    T.op("pool", lambda: nc.gpsimd.dma_start(
                        out=wada[b][:], in_=w_ada[l, :, blk * 1024:(blk + 1) * 1024].rearrange("(k p) n -> p k n", p=128)),
                        reads=[("wada", 1 - b)], writes=[("wada", b)], dsem=f"wada{b}")
                    if l == 0 and blk == 0:
                        dump("wada0", wada[0][:], [128, 8, 1024], [("wada", 0)])
                        dump("badarow", badarow[:], [1, 6 * D], ["badarow"])
                    for half in range(2):
                        bank = (blk * 2 + half) % 8
                        c0 = blk * 1024 + half * 512
                        for kc in range(8):
                            T.op("pe", lambda: nc.tensor.matmul(
                                ps[bank][0:1, :], lhsT=siluc[:, kc:kc + 1], rhs=wada[b][:, kc, half * 512:(half + 1) * 512],
                                start=(kc == 0), stop=(kc == 7)),
                                reads=[("wada", b), "siluc"], writes=[PK(bank)])
                        T.op("dve", lambda: nc.vector.tensor_tensor(
                            out=modrow[0:1, c0:c0 + 512], in0=ps[bank][0:1, :], in1=badarow[0:1, c0:c0 + 512], op=ALU.add),
                            reads=[PK(bank), "badarow"], writes=["modrow"])
                if l == 0:
                    dump("modrow", modrow[:], [1, 6 * D], ["modrow"])
                for i, blk in enumerate((0, 1, 3, 4)):
                    for j in range(8):
                        T.op("pe", lambda: nc.tensor.matmul(
                            ps[4][:, i * 8 + j:i * 8 + j + 1], lhsT=modrow[0:1, blk * 1024 + j * 128: blk * 1024 + (j + 1) * 128],
                            rhs=ones_row[0:1, 0:1], start=True, stop=True),
                            reads=["modrow", "ones_row"], writes=[PK(4)])
                T.op("dve", lambda: nc.vector.tensor_copy(out=modcol[:], in_=ps[4][:, 0:32]),
                     reads=[PK(4)], writes=["modcol"])
                for c0 in (8, 24):
                    T.op("dve", lambda: nc.vector.tensor_scalar(
                        out=modcol[:, c0:c0 + 8], in0=modcol[:, c0:c0 + 8], scalar1=1.0, scalar2=None, op0=ALU.add),
                        reads=["modcol"], writes=["modcol"])
                for dst, key, blk, addc in ((gt1p, "gt1p", 2, 1.0), (gt2p, "gt2p", 5, 1.0), (sh2b, "sh2b", 3, 0.0), (sc2b, "sc2b", 4, 1.0)):
                    for half in range(2):
                        bank = 5 + half
                        T.op("pe", lambda: nc.tensor.matmul(
                            ps[bank][:], lhsT=ones_row[0:1, :], rhs=modrow[0:1, blk * 1024 + half * 512: blk * 1024 + (half + 1) * 512],
                            start=True, stop=True), reads=["modrow", "ones_row"], writes=[PK(bank)])
                        T.op("dve", lambda: nc.vector.tensor_scalar(
                            out=dst[:, half * 512:(half + 1) * 512], in0=ps[bank][:], scalar1=addc, scalar2=None, op0=ALU.add),
                            reads=[PK(bank)], writes=[key])
                T.op(DQ, lambda: DQE.dma_start(
                    out=lamt[:].rearrange("p a b -> p (a b)"),
                    in_=lam_in[l].rearrange("a b -> (a b)").partition_broadcast(128)),
                    writes=["lamt"], dsem="lamt")
                T.op("dve", lambda: nc.vector.tensor_tensor(out=lamt[:, 0, :], in0=lamt[:, 0, :], in1=lamt[:, 1, :], op=ALU.mult),
                     reads=["lamt"], writes=["lamt"])
                T.op("dve", lambda: nc.vector.tensor_tensor(out=lamt[:, 2, :], in0=lamt[:, 2, :], in1=lamt[:, 3, :], op=ALU.mult),
                     reads=["lamt"], writes=["lamt"])
                T.op("dve", lambda: nc.vector.tensor_reduce(out=lamw[:, 0:1], in_=lamt[:, 0, :], axis=AX.X, op=ALU.add),
                     reads=["lamt"], writes=["lamw"])
                T.op("dve", lambda: nc.vector.tensor_reduce(out=lamw[:, 1:2], in_=lamt[:, 2, :], axis=AX.X, op=ALU.add),
                     reads=["lamt"], writes=["lamw"])
                T.op("act", lambda: nc.scalar.activation(out=lamw[:, 2:4], in_=lamw[:, 0:2], func=AF.Exp),
                     reads=["lamw"], writes=["lamw"])
                T.op("dve", lambda: nc.vector.tensor_tensor(out=lamw[:, 4:5], in0=lamw[:, 3:4], in1=lamw[:, 2:3], op=ALU.subtract),
                     reads=["lamw"], writes=["lamw"])
                T.op("dve", lambda: nc.vector.tensor_scalar(out=neglam[:], in0=lamw[:, 4:5], scalar1=-lam_init, scalar2=None, op0=ALU.add),
                     reads=["lamw"], writes=["neglam"])
                T.op(DQ, lambda: DQE.dma_start(out=gcol[:], in_=subln_g[l:l + 1, :].rearrange("o p -> p o"),
                                                     allow_slow_non_contiguous=True),
                     writes=["gcol"], dsem="gcol")
                T.op("dve", lambda: nc.vector.tensor_scalar(out=gcol[:], in0=gcol[:], scalar1=1.0 - lam_init, scalar2=None, op0=ALU.mult),
                     reads=["gcol"], writes=["gcol"])

            if l == 0:
                dump("modcol", modcol[:], [128, 32], ["modcol"])
                dump("gt1p", gt1p[:], [128, D], ["gt1p"])
                dump("gt2p", gt2p[:], [128, D], ["gt2p"])
                dump("neglam", neglam[:], [128, 1], ["neglam"])
                dump("gcol", gcol[:], [128, 1], ["gcol"])
                dump("siluc", siluc[:], [128, 8], ["siluc"])
            with contextlib.ExitStack() as st:
                win = sb("win", [128, 8, INX], BF16, st)
                xg = [sb(f"xg{i}", [128, 4, D], F32, st) for i in range(2)]
                hT = [sb(f"hT{i}", [128, 8, 512], BF16, st) for i in range(2)]
                cs = [sb(f"cs{i}", [128, 2, 512], F32, st) for i in range(2)]
                qT = [sb(f"qT{i}", [128, 4, 512], BF16, st) for i in range(2)]
                kT = [sb(f"kT{i}", [128, 4, 512], BF16, st) for i in range(2)]
                vt = [sb(f"vt{i}", [128, 4, 512], BF16, st) for i in range(2)]
                sgT = [sb(f"sgT{i}", [128, 4, 512], BF16, st) for i in range(2)]
                zuT2 = [sb(f"zuT{i}", [128, 4, 512], BF16, st) for i in range(2)]
                tmpa = [sb(f"tmpa{i}", [128, 512], F32, st) for i in range(2)]
                tmpb = [sb(f"tmpb{i}", [128, 512], F32, st) for i in range(2)]
                zv = [sb(f"zv{i}", [128, 512], F32, st) for i in range(2)]
                zvn = [sb(f"zvn{i}", [128, 512], BF16, st) for i in range(2)]
                stats2 = [sb(f"stats{i}", [128, 4, 6], F32, st) for i in range(2)]
                mv2 = [sb(f"mv{i}", [128, 4, 2], F32, st) for i in range(2)]
                rstd2 = [sb(f"rstd{i}", [128, 4], F32, st) for i in range(2)]
                wsT = sb("wsT", [128, 4, 128], BF16, st)
                bspb = sb("bspb", [128, 512], F32, st)
                lngb = sb("lngb", [128, 512], F32, st)
                lnbb = sb("lnbb", [128, 512], F32, st)

                for kc in range(8):
                    T.op("pool", lambda: nc.gpsimd.dma_start(out=win[:, kc, :], in_=w_in[l, kc * 128:(kc + 1) * 128, :],
                                                             max_dma_last_dim=7168),
                         writes=[("win", kc)], dsem=f"win{kc}")
                T.op("pool", lambda: nc.gpsimd.dma_start(out=wsT[:], in_=w_spT[l].rearrange("g s t -> s g t")),
                     writes=["wsT"], dsem="wsT")
                for gi in range(4):
                    T.op("dve", lambda: nc.vector.tensor_tensor(out=wsT[:, gi, :], in0=wsT[:, gi, :], in1=tri_bf[:], op=ALU.mult),
                         reads=["wsT", "tri"], writes=["wsT"])
                for dst, key, src in ((bspb, "bspb", b_sp), (lngb, "lngb", sg_ln_g), (lnbb, "lnbb", sg_ln_b)):
                    T.op(DQ, lambda: DQE.dma_start(out=dst[:], in_=src[l].partition_broadcast(128)),
                         writes=[key], dsem=key)

                bank_rr = [0]

                def nxt_bank():
                    b = 2 + bank_rr[0] % 6
                    bank_rr[0] += 1
                    return b

                EPG = (NE + NG - 1) // NG

                def a_loads(g_):
                    b_ = g_ % 2
                    T.op(DQ, lambda: DQE.dma_start(out=xg[b_][:], in_=xsrc[g_ * 512:(g_ + 1) * 512, :].rearrange("(t p) d -> p t d", p=128)),
                         reads=[("X", l, g_)] if l > 0 else [], writes=[("xg", b_)], dsem=f"xg{b_}")
                    T.op(DQ, lambda: DQE.dma_start(out=cs[b_][:], in_=cs_in[:, :, g_ * 512:(g_ + 1) * 512]),
                         writes=[("cs", b_)], dsem=f"cs{b_}")
                for g in range(NG):
                    b = g % 2
                    t0 = g * 512
                    for e in range(g * EPG, min(NE, (g + 1) * EPG)):
                        T.op("pool", lambda: nc.gpsimd.dma_start(
                            out=WB[e * 128:(e + 1) * 128, :].rearrange("r (a b) -> r a b", b=2048),
                            in_=wall_in[l, e * 128:(e + 1) * 128, :].rearrange("r (a b) -> r a b", b=2048)),
                            writes=[("WB", e)], dsem=f"wb{e % 2}")
                    if g == 0:
                        a_loads(0)
                    if g + 1 < NG:
                        a_loads(g + 1)
                    for kc in range(8):
                        bank = kc % 2
                        for t in range(4):
                            T.op("pe", lambda: nc.tensor.transpose(ps[bank][:, t * 128:(t + 1) * 128],
                                                                   xg[b][:, t, kc * 128:(kc + 1) * 128], ident[:]),
                                 reads=[("xg", b), "ident"], writes=[PK(bank)])
                        T.op("act", lambda: nc.scalar.activation(out=hT[b][:, kc, :], in_=ps[bank][:], func=AF.Identity,
                                                                 bias=modcol[:, kc:kc + 1], scale=modcol[:, 8 + kc:9 + kc]),
                             reads=[PK(bank), "modcol"], writes=[("hT", b, kc)])
                    HT = [("hT", b, kc) for kc in range(8)]
                    for h in range(4):
                        for nm, base, dst in (("q", 0, qT), ("k", 1024, kT)):
                            ba = nxt_bank()
                            bb = nxt_bank()
                            for bank, off in ((ba, base), (bb, base + 512)):
                                for kc in range(8):
                                    T.op("pe", lambda: nc.tensor.matmul(
                                        ps[bank][:], lhsT=win[:, kc, off + h * 128: off + (h + 1) * 128], rhs=hT[b][:, kc, :],
                                        start=(kc == 0), stop=(kc == 7)),
                                        reads=[("win", kc), ("hT", b, kc)], writes=[PK(bank)])
                            i = (h * 2 + (nm == "k")) % 2
                            T.op("dve", lambda: nc.vector.tensor_tensor(out=tmpa[i][:], in0=ps[ba][:], in1=cs[b][:, 0, :], op=ALU.mult),
                                 reads=[PK(ba), ("cs", b)], writes=[("tmpa", i)])
                            T.op("dve", lambda: nc.vector.tensor_tensor(out=tmpb[i][:], in0=ps[bb][:], in1=cs[b][:, 1, :], op=ALU.mult),
                                 reads=[PK(bb), ("cs", b)], writes=[("tmpb", i)])
                            T.op("pool", lambda: nc.gpsimd.tensor_tensor(out=dst[b][:, h, :], in0=tmpa[i][:], in1=tmpb[i][:], op=ALU.add),
                                 reads=[("tmpa", i), ("tmpb", i)], writes=[(nm + "T", b)])
                    zuT = zuT2[b]
                    for gi in range(4):
                        bank = nxt_bank()
                        for kc in range(8):
                            T.op("pe", lambda: nc.tensor.matmul(
                                ps[bank][:], lhsT=win[:, kc, 2560 + gi * 128: 2560 + (gi + 1) * 128], rhs=hT[b][:, kc, :],
                                start=(kc == 0), stop=(kc == 7)),
                                reads=[("win", kc), ("hT", b, kc)], writes=[PK(bank)])
                        T.op("act", lambda: nc.scalar.activation(out=zuT[:, gi, :], in_=ps[bank][:], func=AF.Gelu),
                             reads=[PK(bank)], writes=[("zuT", b, gi)])
                    def a_spatial(tt):
                        bank = nxt_bank()
                        for gi in range(4):
                            T.op("pe", lambda: nc.tensor.matmul(
                                ps[bank][:, gi * 128:(gi + 1) * 128], lhsT=zvn[tt % 2][:, gi * 128:(gi + 1) * 128], rhs=wsT[:, gi, :],
                                start=True, stop=True),
                                reads=[("zvn", tt % 2), "wsT"], writes=[PK(bank)])
                        ti = tt % 2
                        T.op("dve", lambda: nc.vector.tensor_tensor(out=tmpa[ti][:], in0=ps[bank][:], in1=bspb[:], op=ALU.add),
                             reads=[PK(bank), "bspb"], writes=[("tmpa", ti)])
                        T.op("dve", lambda: nc.vector.tensor_tensor(
                            out=sgT[b][:, :, tt * 128:(tt + 1) * 128], in0=tmpa[ti][:].rearrange("p (g t) -> p g t", g=4),
                            in1=zuT[:, :, tt * 128:(tt + 1) * 128], op=ALU.mult),
                            reads=[("tmpa", ti)] + [("zuT", b, gi) for gi in range(4)], writes=[("sgT", b)])
                    for t in range(4):
                        bank = nxt_bank()
                        for kc in range(8):
                            T.op("pe", lambda: nc.tensor.matmul(
                                ps[bank][:], lhsT=hT[b][:, kc, t * 128:(t + 1) * 128], rhs=win[:, kc, 2048:2560],
                                start=(kc == 0), stop=(kc == 7)),
                                reads=[("win", kc), ("hT", b, kc)], writes=[PK(bank)])
                        T.op("act", lambda: nc.scalar.copy(out=vt[b][:, t, :], in_=ps[bank][:]),
                             reads=[PK(bank)], writes=[("vt", b)])
                        bank = nxt_bank()
                        for kc in range(8):
                            T.op("pe", lambda: nc.tensor.matmul(
                                ps[bank][:], lhsT=hT[b][:, kc, t * 128:(t + 1) * 128], rhs=win[:, kc, 3072:3584],
                                start=(kc == 0), stop=(kc == 7)),
                                reads=[("win", kc), ("hT", b, kc)], writes=[PK(bank)])
                        zi = t % 2
                        stats, mv, rstd = stats2[zi], mv2[zi], rstd2[zi]
                        SK, MK, RK = ("stats", zi), ("mv", zi), ("rstd", zi)
                        T.op("act", lambda: nc.scalar.activation(out=zv[zi][:], in_=ps[bank][:], func=AF.Gelu),
                             reads=[PK(bank)], writes=[("zv", zi)])
                        for gi in range(4):
                            T.op("dve", lambda: nc.vector.bn_stats(out=stats[:, gi, :], in_=zv[zi][:, gi * 128:(gi + 1) * 128]),
                                 reads=[("zv", zi)], writes=[SK])
                        for gi in range(4):
                            T.op("dve", lambda: nc.vector.bn_aggr(out=mv[:, gi, :], in_=stats[:, gi, :]),
                                 reads=[SK], writes=[MK])
                        rsqrt_small(st, "rstd", rstd[:], mv[:, :, 1], 4, [MK], RK)
                        for gi in range(4):
                            T.op("dve", lambda: nc.vector.tensor_scalar(
                                out=zv[zi][:, gi * 128:(gi + 1) * 128], in0=zv[zi][:, gi * 128:(gi + 1) * 128],
                                scalar1=mv[:, gi, 0:1], scalar2=rstd[:, gi:gi + 1], op0=ALU.subtract, op1=ALU.mult),
                                reads=[("zv", zi), MK, RK], writes=[("zv", zi)])
                        T.op("dve", lambda: nc.vector.tensor_tensor(out=zv[zi][:], in0=zv[zi][:], in1=lngb[:], op=ALU.mult),
                             reads=[("zv", zi), "lngb"], writes=[("zv", zi)])
                        T.op("dve", lambda: nc.vector.tensor_tensor(out=zvn[zi][:], in0=zv[zi][:], in1=lnbb[:], op=ALU.add),
                             reads=[("zv", zi), "lnbb"], writes=[("zvn", zi)])
                        if t > 0:
                            a_spatial(t - 1)
                    a_spatial(3)
                    T.op("pool", lambda: nc.gpsimd.dma_start(out=QT[:, :, t0:t0 + 512].rearrange("h p t -> p h t"), in_=qT[b][:]),
                         reads=[("qT", b)], writes=[("QT", g)], dsem=f"stq{b}")
                    T.op("pool", lambda: nc.gpsimd.dma_start(out=KT[:, :, t0:t0 + 512].rearrange("h p t -> p h t"), in_=kT[b][:]),
                         reads=[("kT", b)], writes=[("KT", g)], dsem=f"stk{b}")
                    T.op("pool", lambda: nc.gpsimd.dma_start(out=VS[t0:t0 + 512, :].rearrange("(t p) c -> p t c", p=128), in_=vt[b][:]),
                         reads=[("vt", b)], writes=[("VS", g)], dsem=f"stv{b}")
                    T.op("pool", lambda: nc.gpsimd.dma_start(out=SGT[:, :, t0:t0 + 512].rearrange("h p t -> p h t"), in_=sgT[b][:]),
                         reads=[("sgT", b)], writes=[("SGT", g)], dsem=f"sts{b}")

            with contextlib.ExitStack() as st:
                kTh = [sb(f"kTh{i}", [128, S], BF16, st) for i in range(2)]
                vh = [sb(f"vh{i}", [128, NT, 128], BF16, st) for i in range(2)]
                qc_sb = [sb(f"qc{i}", [128, 512], BF16, st) for i in range(2)]
                pb = [sb(f"pb{i}", [128, 2, 512], BF16, st) for i in range(4)]
                SB = ((0, 1), (2, 3))
                racc = [[[sb(f"racc{i}{m}{p_}", [128, 512], F32, st) for p_ in range(2)] for m in range(2)] for i in range(2)]
                r1 = sb("r1", [128, 512], F32, st)
                r2 = sb("r2", [128, 512], F32, st)
                fa = sb("fa", [128, 512], F32, st)
                fb = sb("fb", [128, 512], F32, st)
                fsq = sb("fsq", [128, 512], F32, st)
                frs = sb("frs", [128, 512], F32, st)
                ao = [sb(f"ao{i}", [128, 512], BF16, st) for i in range(2)]
                OB = (4, 5, 6, 7)
                def b_load_head(h_):
                    hb_ = h_ % 2
                    T.op(DQ, lambda: DQE.dma_start(out=kTh[hb_][:], in_=KT[h_]),
                         reads=[("KT", g) for g in range(NG)], writes=[("kTh", hb_)], dsem=f"kTh{hb_}")
                    T.op(DQ, lambda: DQE.dma_start(out=vh[hb_][:], in_=VS[:, h_ * 128:(h_ + 1) * 128].rearrange("(n p) c -> p n c", p=128)),
                         reads=[("VS", g) for g in range(NG)], writes=[("vh", hb_)], dsem=f"vh{hb_}")

                def b_load_q(it_):
                    h_, qc_ = divmod(it_, NG)
                    T.op(DQ, lambda: DQE.dma_start(out=qc_sb[it_ % 2][:], in_=QT[h_, :, qc_ * 512:(qc_ + 1) * 512]),
                         reads=[("QT", qc_)], writes=[("qc", it_ % 2)], dsem=f"qc{it_ % 2}")

                b_load_head(0)
                b_load_q(0)
                for h in range(4):
                    hb = h % 2
                    for qc in range(NG):
                        it = h * NG + qc
                        qb = it % 2
                        q0 = qc * 512
                        if qc == 0 and h + 1 < 4:
                            b_load_head(h + 1)
                        if it + 1 < 4 * NG:
                            b_load_q(it + 1)
                        nkb = 4 * (qc + 1)

                        def emit_S(kb):
                            j = kb - 4 * qc
                            c0 = max(0, j) * 128
                            for m in range(2):
                                bank = SB[kb % 2][m]
                                T.op("pe", lambda: nc.tensor.matmul(
                                    ps[bank][:, c0:512], lhsT=kTh[hb][m * 64:(m + 1) * 64, kb * 128:(kb + 1) * 128],
                                    rhs=qc_sb[qb][m * 64:(m + 1) * 64, c0:512], start=True, stop=True),
                                    reads=[("kTh", hb), ("qc", qb)], writes=[PK(bank)])

                        def emit_P(kb):
                            j = kb - 4 * qc
                            c0 = max(0, j) * 128
                            pi = kb % 4
                            for m in range(2):
                                bank = SB[kb % 2][m]
                                T.op("act", lambda: nc.scalar.activation(out=pb[pi][:, m, c0:512], in_=ps[bank][:, c0:512],
                                                                         func=AF.Exp, scale=0.125),
                                     reads=[PK(bank)], writes=[("pb", pi, m)])
                                if j >= 0:
                                    en, ee = ("dve", nc.vector) if m == 0 else ("pool", nc.gpsimd)
                                    T.op(en, lambda: ee.tensor_tensor(
                                        out=pb[pi][:, m, c0:c0 + 128], in0=pb[pi][:, m, c0:c0 + 128], in1=tri_bf[:], op=ALU.mult),
                                        reads=[("pb", pi, m), "tri"], writes=[("pb", pi, m)])

                        def emit_PV(kb):
                            j = kb - 4 * qc
                            c0 = max(0, j) * 128
                            pi = kb % 4
                            for m in range(2):
                                T.op("pe", lambda: nc.tensor.matmul(
                                    ps[OB[2 * m]][:, c0:512], lhsT=vh[hb][:, kb, :], rhs=pb[pi][:, m, c0:512],
                                    start=(kb == 0), stop=(kb == nkb - 1)),
                                    reads=[("vh", hb), ("pb", pi, m)], writes=[PK(OB[2 * m])])
                                if m == 1 and kb % 3 != 0:
                                    T.op("pe", lambda: nc.tensor.matmul(
                                        ps[7][:, c0:512], lhsT=ones_bf[:], rhs=pb[pi][:, m, c0:512],
                                        start=(kb == 1), stop=False),
                                        reads=["ones_bf", ("pb", pi, m)], writes=[PK(7)])
                                    continue
                                ai_ = (kb % 2) if m == 0 else ((kb // 3) % 2)
                                ra = racc[qc % 2][m][ai_]
                                rk = ("racc", qc % 2, m, ai_)
                                if (m, ai_) not in acc_used:
                                    acc_used.add((m, ai_))
                                    T.op("dve", lambda: nc.vector.tensor_copy(out=ra[:, c0:512], in_=pb[pi][:, m, c0:512]),
                                         reads=[("pb", pi, m)], writes=[rk])
                                    if c0 > 0:
                                        T.op("dve", lambda: nc.vector.memset(ra[:, 0:c0], 0.0), reads=[rk], writes=[rk])
                                else:
                                    T.op("dve", lambda: nc.vector.tensor_tensor(out=ra[:, c0:512], in0=ra[:, c0:512], in1=pb[pi][:, m, c0:512], op=ALU.add),
                                         reads=[("pb", pi, m), rk], writes=[rk])

                        acc_used = set()
                        emit_S(0)
                        for kb in range(nkb):
                            if kb + 1 < nkb:
                                emit_S(kb + 1)
                            emit_P(kb)
                            emit_PV(kb)
                        for m in range(2):
                            used = [p_ for p_ in range(2) if (m, p_) in acc_used]
                            for ui, p_ in enumerate(used):
                                T.op("pe", lambda: nc.tensor.matmul(ps[5 + 2 * m][:], lhsT=ones_f32[:], rhs=racc[qc % 2][m][p_][:],
                                                                    start=(m == 0 and ui == 0), stop=(ui == len(used) - 1)),
                                     reads=["ones_f32", ("racc", qc % 2, m, p_)], writes=[PK(5 + 2 * m)])
                        T.op("act", lambda: nc.scalar.activation(out=r1[:], in_=ps[5][:], func=AF.Ln), reads=[PK(5)], writes=["r1"])
                        T.op("act", lambda: nc.scalar.activation(out=r1[:], in_=r1[:], func=AF.Exp, scale=-1.0), reads=["r1"], writes=["r1"])
                        T.op("act", lambda: nc.scalar.activation(out=r2[:], in_=ps[7][:], func=AF.Ln), reads=[PK(7)], writes=["r2"])
                        T.op("act", lambda: nc.scalar.activation(out=r2[:], in_=r2[:], func=AF.Exp, scale=-1.0), reads=["r2"], writes=["r2"])
                        T.op("dve", lambda: nc.vector.tensor_tensor(out=fa[:], in0=ps[4][:], in1=r1[:], op=ALU.mult),
                             reads=[PK(4), "r1"], writes=["fa"])
                        T.op("dve", lambda: nc.vector.tensor_tensor(out=fb[:], in0=ps[6][:], in1=r2[:], op=ALU.mult),
                             reads=[PK(6), "r2"], writes=["fb"])
                        T.op("dve", lambda: nc.vector.scalar_tensor_tensor(out=fa[:], in0=fb[:], scalar=neglam[:, 0:1], in1=fa[:],
                                                                           op0=ALU.mult, op1=ALU.add),
                             reads=["fa", "fb", "neglam"], writes=["fa"])
                        T.op("act", lambda: nc.scalar.activation(out=fsq[:], in_=fa[:], func=AF.Square),
                             reads=["fa"], writes=["fsq"])
                        T.op("pe", lambda: nc.tensor.matmul(ps[0][:], lhsT=onesdiv[:], rhs=fsq[:], start=True, stop=True),
                             reads=["onesdiv", "fsq"], writes=[PK(0)])
                        T.op("act", lambda: nc.scalar.activation(out=frs[:], in_=ps[0][:], func=AF.Ln, bias=epscol[:, 0:1]),
                             reads=[PK(0), "epscol"], writes=["frs"])
                        T.op("act", lambda: nc.scalar.activation(out=frs[:], in_=frs[:], func=AF.Exp, scale=-0.5), reads=["frs"], writes=["frs"])
                        ai = qc % 2
                        T.op("dve", lambda: nc.vector.scalar_tensor_tensor(out=ao[ai][:], in0=fa[:], scalar=gcol[:, 0:1], in1=frs[:],
                                                                           op0=ALU.mult, op1=ALU.mult),
                             reads=["fa", "frs", "gcol"], writes=[("ao", ai)])
                        T.op("pool", lambda: nc.gpsimd.dma_start(out=ATT[h, :, q0:q0 + 512], in_=ao[ai][:]),
                             reads=[("ao", ai)], writes=[("ATT", h, qc)], dsem=f"sta{ai}")

            with contextlib.ExitStack() as st:
                wout = sb("wout", [128, 8, D], BF16, st)
                wr = sb("wr", [128, 8, 36], BF16, st)
                brb = sb("brb", [128, 36], F32, st)
                l1g = sb("l1g", [128, D], F32, st)
                l1b = sb("l1b", [128, D], F32, st)
                mixT = [sb(f"mixT{i}", [128, 8, 512], BF16, st) for i in range(2)]
                xg = [sb(f"xgc{i}", [128, 4, D], F32, st) for i in range(2)]
                x1g = [sb(f"x1g{i}", [128, 4, D], F32, st) for i in range(2)]
                h2T = [sb(f"h2T{i}", [128, 8, 512], BF16, st) for i in range(2)]
                ytmp2 = [sb(f"ytmp{i}", [128, D], F32, st) for i in range(2)]
                stats2 = [sb(f"statsc{i}", [128, 2, 6], F32, st) for i in range(2)]
                mv2 = [sb(f"mvc{i}", [128, 2], F32, st) for i in range(2)]
                rstd2 = [sb(f"rstdc{i}", [128, 1], F32, st) for i in range(2)]
                rg_s = []
                for i in range(2):
                    rg_s.append((sb(f"rlg{i}", [128, 4, 36], F32, st), sb(f"rs4{i}", [128, 4, 16], F32, st), sb(f"rohg{i}", [128, 4, 4], F32, st),
                                 sb(f"rgexp{i}", [128, 4, 4], F32, st), sb(f"rsel{i}", [128, 4, 8], F32, st), sb(f"roh1{i}", [128, 4, 8], F32, st),
                                 sb(f"roh2{i}", [128, 4, 8], F32, st), sb(f"rsel2{i}", [128, 4, 8], F32, st), sb(f"rohs{i}", [128, 4, 8], F32, st),
                                 sb(f"rRg{i}", [128, 4, 8], F32, st), sb(f"rt8{i}", [128, 4, 8], F32, st), sb(f"rt4{i}", [128, 4, 4], F32, st),
                                 sb(f"rprod{i}", [128, 4, 8, 4], F32, st), sb(f"rtmp32{i}", [128, 32], F32, st)))
                ohb4 = [sb(f"ohb4{i}", [128, 4, 32], BF16, st) for i in range(2)]
                h2f2 = [sb("h2f", [128, D], F32, st)] * 2
                mdK = sb("mdK", [128, NTILE, 32], F32, st)
                mdP = sb("mdP", [128, NT, 2, 32], F32, st)
                h2tok = [sb(f"h2tok{i}", [128, 4, D], BF16, st) for i in range(2)]
                T.op("dve", lambda: nc.vector.memset(cum[:], 0.0), writes=["cum"])
                T.op("dve", lambda: nc.vector.memset(cumb[0][:], 0.0), writes=[("cumb", 0)])
                T.op("pool", lambda: nc.gpsimd.dma_start(out=wout[:], in_=w_out[l].rearrange("(k p) n -> p k n", p=128),
                                                         max_dma_last_dim=4096),
                     writes=["wout"], dsem="wout")
                T.op("pool", lambda: nc.gpsimd.dma_start(out=wr[:], in_=w_r[l].rearrange("(k p) n -> p k n", p=128)),
                     writes=["wr"], dsem="wr")
                for dst, key, src in ((brb, "brb", b_r), (l1g, "l1g", ln1_g), (l1b, "l1b", ln1_b)):
                    T.op(DQ, lambda: DQE.dma_start(out=dst[:], in_=src[l].partition_broadcast(128)),
                         writes=[key], dsem=key)
                def c_loads(g_):
                    b_ = g_ % 2
                    T.op(DQ, lambda: DQE.dma_start(out=mixT[b_][:, 0:4, :], in_=ATT[:, :, g_ * 512:(g_ + 1) * 512].rearrange("h p t -> p h t")),
                         reads=[("ATT", h, g_) for h in range(4)], writes=[("mixTa", b_)], dsem=f"mixTa{b_}")
                    T.op(DQ, lambda: DQE.dma_start(out=mixT[b_][:, 4:8, :], in_=SGT[:, :, g_ * 512:(g_ + 1) * 512].rearrange("h p t -> p h t")),
                         reads=[("SGT", g_)], writes=[("mixTb", b_)], dsem=f"mixTb{b_}")
                    T.op(DQ, lambda: DQE.dma_start(out=xg[b_][:], in_=xsrc[g_ * 512:(g_ + 1) * 512, :].rearrange("(t p) d -> p t d", p=128)),
                         reads=[("X", l, g_)] if l > 0 else [], writes=[("xgc", b_)], dsem=f"xgc{b_}")

                for g in range(NG):
                    b = g % 2
                    t0 = g * 512
                    if g == 0:
                        c_loads(0)
                    if g + 1 < NG:
                        c_loads(g + 1)
                    for t in range(4):
                        yp = t % 2
                        ytmp, stats, mv, rstd = ytmp2[yp], stats2[yp], mv2[yp], rstd2[yp]
                        YK, SK, MK, RK = ("ytmp", yp), ("statsc", yp), ("mvc", yp), ("rstdc", yp)
                        for half in range(2):
                            bank = 2 + (t * 2 + half) % 6
                            for kc in range(8):
                                T.op("pe", lambda: nc.tensor.matmul(
                                    ps[bank][:], lhsT=mixT[b][:, kc, t * 128:(t + 1) * 128], rhs=wout[:, kc, half * 512:(half + 1) * 512],
                                    start=(kc == 0), stop=(kc == 7)),
                                    reads=[("mixTa", b), ("mixTb", b), "wout"], writes=[PK(bank)])
                            T.op("dve", lambda: nc.vector.tensor_tensor(
                                out=ytmp[:, half * 512:(half + 1) * 512], in0=ps[bank][:], in1=gt1p[:, half * 512:(half + 1) * 512], op=ALU.mult),
                                reads=[PK(bank), "gt1p"], writes=[YK])
                        T.op("dve", lambda: nc.vector.scalar_tensor_tensor(out=ytmp[:], in0=xg[b][:, t, :], scalar=ALPHA, in1=ytmp[:],
                                                                           op0=ALU.mult, op1=ALU.add),
                             reads=[("xgc", b), YK], writes=[YK])
                        for half in range(2):
                            T.op("dve", lambda: nc.vector.bn_stats(out=stats[:, half, :], in_=ytmp[:, half * 512:(half + 1) * 512]),
                                 reads=[YK], writes=[SK])
                        T.op("dve", lambda: nc.vector.bn_aggr(out=mv[:], in_=stats[:].rearrange("p a b -> p (a b)")),
                             reads=[SK], writes=[MK])
                        rsqrt_small(st, "r", rstd[:], mv[:, 1:2], 1, [MK], RK)
                        T.op("dve", lambda: nc.vector.tensor_scalar(out=ytmp[:], in0=ytmp[:], scalar1=mv[:, 0:1], scalar2=rstd[:, 0:1],
                                                                    op0=ALU.subtract, op1=ALU.mult),
                             reads=[YK, MK, RK], writes=[YK])
                        T.op("pool", lambda: nc.gpsimd.tensor_tensor(out=ytmp[:], in0=ytmp[:], in1=l1g[:], op=ALU.mult),
                             reads=[YK, "l1g"], writes=[YK])
                        T.op("pool", lambda: nc.gpsimd.tensor_tensor(out=x1g[b][:, t, :], in0=ytmp[:], in1=l1b[:], op=ALU.add),
                             reads=[YK, "l1b"], writes=[("x1g", b, t)])
                    X1G = [("x1g", b, t) for t in range(4)]
                    for kc in range(8):
                        bank = kc % 2
                        for t in range(4):
                            T.op("pe", lambda: nc.tensor.transpose(ps[bank][:, t * 128:(t + 1) * 128],
                                                                   x1g[b][:, t, kc * 128:(kc + 1) * 128], ident[:]),
                                 reads=[("x1g", b, t), "ident"], writes=[PK(bank)])
                        T.op("act", lambda: nc.scalar.activation(out=h2T[b][:, kc, :], in_=ps[bank][:], func=AF.Identity,
                                                                 bias=modcol[:, 16 + kc:17 + kc], scale=modcol[:, 24 + kc:25 + kc]),
                             reads=[PK(bank), "modcol"], writes=[("h2T", b)])
                    gq = g % 2
                    RK_ = [("rg", gq)]
                    lg, s4, ohg, gexp, sel, oh1, oh2, sel2, ohs8, Rg, t8, t4, prod, tmp32 = rg_s[gq]
                    LB = 2 + gq
                    RB = 4 + gq

                    def dv(fn, extra_r=(), w=None):
                        T.op("dve", fn, reads=RK_ + list(extra_r), writes=(RK_ if w is None else w))

                    def bc(ap, shape):
                        return ap.to_broadcast(shape)
                    for t in range(4):
                        for kc in range(8):
                            T.op("pe", lambda: nc.tensor.matmul(
                                ps[LB][:, t * 64:t * 64 + 36], lhsT=h2T[b][:, kc, t * 128:(t + 1) * 128], rhs=wr[:, kc, :],
                                start=(kc == 0), stop=(kc == 7)),
                                reads=[("h2T", b), "wr"], writes=[PK(LB)])
                    dv(lambda: nc.vector.tensor_tensor(
                        out=lg[:], in0=ps[LB][:, 0:256].rearrange("p (t c) -> p t c", c=64)[:, :, 0:36],
                        in1=bc(brb[:].unsqueeze(1), [128, 4, 36]), op=ALU.add), [PK(LB), "brb"])
                    dv(lambda: nc.vector.tensor_reduce(out=s4[:, :, 0], in_=lg[:, :, 0:4], axis=AX.X, op=ALU.max))
                    dv(lambda: nc.vector.tensor_tensor(out=ohg[:], in0=lg[:, :, 0:4], in1=bc(s4[:, :, 0:1], [128, 4, 4]), op=ALU.is_equal))
                    dv(lambda: nc.vector.tensor_tensor(out=gexp[:], in0=lg[:, :, 0:4], in1=bc(s4[:, :, 0:1], [128, 4, 4]), op=ALU.subtract))
                    T.op("act", lambda: nc.scalar.activation(out=gexp[:], in_=gexp[:], func=AF.Exp), reads=RK_, writes=RK_)
                    dv(lambda: nc.vector.tensor_reduce(out=s4[:, :, 1], in_=gexp[:], axis=AX.X, op=ALU.add))
                    dv(lambda: nc.vector.reciprocal(out=s4[:, :, 2], in_=s4[:, :, 1]))
                    dv(lambda: nc.vector.tensor_tensor(
                        out=prod[:], in0=lg[:, :, 4:36].rearrange("p t (g e) -> p t e g", g=4),
                        in1=bc(ohg[:].unsqueeze(2), [128, 4, 8, 4]), op=ALU.mult))
                    dv(lambda: nc.vector.tensor_reduce(out=sel[:], in_=prod[:], axis=AX.X, op=ALU.add))
                    dv(lambda: nc.vector.tensor_reduce(out=s4[:, :, 3], in_=sel[:], axis=AX.X, op=ALU.max))
                    dv(lambda: nc.vector.tensor_tensor(out=oh1[:], in0=sel[:], in1=bc(s4[:, :, 3:4], [128, 4, 8]), op=ALU.is_equal))
                    dv(lambda: nc.vector.scalar_tensor_tensor(
                        out=sel2[:].rearrange("p t e -> p (t e)"), in0=oh1[:].rearrange("p t e -> p (t e)"), scalar=NEG_BIG,
                        in1=sel[:].rearrange("p t e -> p (t e)"), op0=ALU.mult, op1=ALU.add))
                    dv(lambda: nc.vector.tensor_reduce(out=s4[:, :, 4], in_=sel2[:], axis=AX.X, op=ALU.max))
                    dv(lambda: nc.vector.tensor_tensor(out=oh2[:], in0=sel2[:], in1=bc(s4[:, :, 4:5], [128, 4, 8]), op=ALU.is_equal))
                    dv(lambda: nc.vector.tensor_tensor(out=s4[:, :, 5], in0=s4[:, :, 4], in1=s4[:, :, 3], op=ALU.subtract))
                    T.op("act", lambda: nc.scalar.activation(out=s4[:, :, 5], in_=s4[:, :, 5], func=AF.Exp), reads=RK_, writes=RK_)
                    dv(lambda: nc.vector.tensor_scalar(out=s4[:, :, 5], in0=s4[:, :, 5], scalar1=1.0, scalar2=None, op0=ALU.add))
                    dv(lambda: nc.vector.reciprocal(out=s4[:, :, 6], in_=s4[:, :, 5]))
                    dv(lambda: nc.vector.tensor_scalar(out=s4[:, :, 7], in0=s4[:, :, 6], scalar1=-1.0, scalar2=1.0, op0=ALU.mult, op1=ALU.add))
                    dv(lambda: nc.vector.tensor_tensor(out=s4[:, :, 6:8], in0=s4[:, :, 6:8], in1=bc(s4[:, :, 2:3], [128, 4, 2]), op=ALU.mult))
                    dv(lambda: nc.vector.tensor_tensor(out=ohs8[:], in0=oh1[:], in1=oh2[:], op=ALU.add))
                    dv(lambda: nc.vector.tensor_tensor(
                        out=ohb4[gq][:].rearrange("p t (g e) -> p t g e", g=4), in0=bc(ohs8[:].unsqueeze(2), [128, 4, 4, 8]),
                        in1=bc(ohg[:].unsqueeze(3), [128, 4, 4, 8]), op=ALU.mult), w=RK_ + [("ohb4", gq)])
                    for t in range(4):
                        mms = [(triu1_bf, ohb4[gq][:, t, :], "triu1")] + [(ones_bf, ohb4[gq][:, t2, :], "ones_bf") for t2 in range(t)] \
                            + [(ones_bf, cumb[gq][:], "ones_bf")]
                        for mi, (lh, rh, lk) in enumerate(mms):
                            T.op("pe", lambda: nc.tensor.matmul(ps[RB][:, t * 32:(t + 1) * 32], lhsT=lh[:], rhs=rh,
                                                                start=(mi == 0), stop=(mi == len(mms) - 1)),
                                 reads=[("ohb4", gq), ("cumb", gq), lk], writes=[PK(RB)])
                    T.op("dve", lambda: nc.vector.tensor_reduce(out=tmp32[:], in_=ohb4[gq][:].rearrange("p t e -> p e t"), axis=AX.X, op=ALU.add),
                         reads=[("ohb4", gq)] + RK_, writes=RK_)
                    T.op("dve", lambda: nc.vector.tensor_tensor(out=cum[:], in0=cum[:], in1=tmp32[:], op=ALU.add),
                         reads=["cum"] + RK_, writes=["cum"])
                    T.op("dve", lambda: nc.vector.tensor_copy(out=cumb[1 - gq][:], in_=cum[:]),
                         reads=["cum"], writes=[("cumb", 1 - gq)])
                    dv(lambda: nc.vector.tensor_tensor(
                        out=prod[:], in0=ps[RB][:, 0:128].rearrange("p (t g e) -> p t e g", t=4, g=4),
                        in1=bc(ohg[:].unsqueeze(2), [128, 4, 8, 4]), op=ALU.mult), [PK(RB)])
                    dv(lambda: nc.vector.tensor_reduce(out=Rg[:], in_=prod[:], axis=AX.X, op=ALU.add))
                    dv(lambda: nc.vector.tensor_tensor(out=t8[:], in0=oh1[:], in1=Rg[:], op=ALU.mult))
                    dv(lambda: nc.vector.tensor_reduce(out=s4[:, :, 13], in_=t8[:], axis=AX.X, op=ALU.add))
                    dv(lambda: nc.vector.tensor_tensor(out=t8[:], in0=oh2[:], in1=Rg[:], op=ALU.mult))
                    dv(lambda: nc.vector.tensor_reduce(out=s4[:, :, 14], in_=t8[:], axis=AX.X, op=ALU.add))
                    dv(lambda: nc.vector.tensor_tensor(out=t4[:], in0=ohg[:], in1=bc(iota32[:, 0:4].unsqueeze(1), [128, 4, 4]), op=ALU.mult), ["iota32"])
                    dv(lambda: nc.vector.tensor_reduce(out=s4[:, :, 8], in_=t4[:], axis=AX.X, op=ALU.add))
                    dv(lambda: nc.vector.tensor_tensor(out=t8[:], in0=oh1[:], in1=bc(iota32[:, 0:8].unsqueeze(1), [128, 4, 8]), op=ALU.mult), ["iota32"])
                    dv(lambda: nc.vector.tensor_reduce(out=s4[:, :, 9], in_=t8[:], axis=AX.X, op=ALU.add))
                    dv(lambda: nc.vector.tensor_tensor(out=t8[:], in0=oh2[:], in1=bc(iota32[:, 0:8].unsqueeze(1), [128, 4, 8]), op=ALU.mult), ["iota32"])
                    dv(lambda: nc.vector.tensor_reduce(out=s4[:, :, 10], in_=t8[:], axis=AX.X, op=ALU.add))
                    dv(lambda: nc.vector.tensor_scalar(out=s4[:, :, 8], in0=s4[:, :, 8], scalar1=8.0, scalar2=None, op0=ALU.mult))
                    dv(lambda: nc.vector.tensor_tensor(out=s4[:, :, 11:13], in0=s4[:, :, 9:11], in1=bc(s4[:, :, 8:9], [128, 4, 2]), op=ALU.add))
                    TMK = [("tokmeta", g * 4 + t) for t in range(4)]
                    dv(lambda: nc.vector.tensor_copy(out=tokmeta[:, g * 4:(g + 1) * 4, 0:2], in_=s4[:, :, 11:13]), w=RK_ + TMK)
                    dv(lambda: nc.vector.tensor_copy(out=tokmeta[:, g * 4:(g + 1) * 4, 2:4], in_=s4[:, :, 13:15]), w=RK_ + TMK)
                    dv(lambda: nc.vector.tensor_copy(out=tokmeta[:, g * 4:(g + 1) * 4, 4:6], in_=s4[:, :, 6:8]), w=RK_ + TMK)
                    for t in range(4):
                        T.op("dve", lambda: nc.vector.tensor_tensor(out=h2f2[t % 2][:], in0=x1g[b][:, t, :], in1=sc2b[:], op=ALU.mult),
                             reads=[("x1g", b, t), "sc2b"], writes=["h2f"])
                        T.op("dve", lambda: nc.vector.tensor_tensor(out=h2tok[b][:, t, :], in0=h2f2[t % 2][:], in1=sh2b[:], op=ALU.add),
                             reads=["h2f", "sh2b"], writes=[("h2tok", b, t)])
                    T.op("pool", lambda: nc.gpsimd.dma_start(out=H2[t0:t0 + 512, :].rearrange("(t p) d -> p t d", p=128), in_=h2tok[b][:]),
                         reads=[("h2tok", b, t) for t in range(4)], writes=[("H2", g)], dsem=f"sth2{b}")
                    T.op("pool", lambda: nc.gpsimd.dma_start(out=X1[t0:t0 + 512, :].rearrange("(t p) d -> p t d", p=128), in_=x1g[b][:]),
                         reads=X1G, writes=[("X1", g)], dsem=f"stx1{b}")

                MB = 5
                fin = NG % 2
                T.op("pe", lambda: nc.tensor.matmul(ps[MB][:, 0:32], lhsT=ones_bf[:], rhs=cumb[fin][:], start=True, stop=True),
                     reads=[("cumb", fin), "ones_bf"], writes=[PK(MB)])
                M_ = ["md"]

                def dm(fn, extra_r=(), w=M_):
                    T.op("dve", fn, reads=M_ + list(extra_r), writes=w)
                cnt_, ntl_, off_, end_, tmp_, tmp2_ = (md[:, i * 32:(i + 1) * 32] for i in range(6))
                dm(lambda: nc.vector.tensor_copy(out=cnt_, in_=ps[MB][:, 0:32]), [PK(MB)])
                dm(lambda: nc.vector.tensor_scalar(out=ntl_, in0=cnt_, scalar1=0.0, scalar2=None, op0=ALU.is_gt))
                for j in range(1, NG):
                    dm(lambda: nc.vector.scalar_tensor_tensor(out=ntl_, in0=cnt_, scalar=j * 512.0, in1=ntl_, op0=ALU.is_gt, op1=ALU.add))
                dm(lambda: nc.vector.tensor_scalar(out=ntl_, in0=ntl_, scalar1=512.0, scalar2=None, op0=ALU.mult))
                dm(lambda: nc.vector.memset(md[:, 64:65], 0.0))
                for e in range(1, NE):
                    dm(lambda: nc.vector.tensor_tensor(out=md[:, 64 + e:65 + e], in0=md[:, 63 + e:64 + e], in1=md[:, 31 + e:32 + e], op=ALU.add))
                dm(lambda: nc.vector.tensor_tensor(out=end_, in0=off_, in1=ntl_, op=ALU.add))
                dm(lambda: nc.vector.tensor_tensor(out=mdK[:], in0=end_.unsqueeze(1).to_broadcast([128, NTILE, 32]),
                                                   in1=kt512[:, 0:NTILE].unsqueeze(2).to_broadcast([128, NTILE, 32]), op=ALU.is_le),
                   ["kt512"], w=M_ + ["mdK"])
                dm(lambda: nc.vector.tensor_reduce(out=tl[:, 0, :], in_=mdK[:], axis=AX.X, op=ALU.add), ["mdK"], w=M_ + ["tl"])
                TL = ["tl"]

                def dt_(fn, extra_r=(), w=TL):
                    T.op("dve", fn, reads=TL + list(extra_r), writes=w)
                dt_(lambda: nc.vector.tensor_scalar(out=tl[:, 2, :], in0=tl[:, 0, :], scalar1=31.5, scalar2=None, op0=ALU.is_le))
                dt_(lambda: nc.vector.tensor_scalar(out=tl[:, 3, :], in0=tl[:, 0, :], scalar1=31.0, scalar2=None, op0=ALU.min))
                dt_(lambda: nc.vector.tensor_scalar(out=widxf[:], in0=tl[:, 3, :], scalar1=128.0, scalar2=pcol[:, 0:1], op0=ALU.mult, op1=ALU.add),
                    ["pcol"], w=TL + ["widxf"])
                T.op("dve", lambda: nc.vector.tensor_copy(out=widx_i[:], in_=widxf[:]), reads=["widxf"], writes=["widx"])
                TMALL = [("tokmeta", i_) for i_ in range(NT)]
                dm(lambda: nc.vector.tensor_tensor(out=mdP[:], in0=iota32[:].unsqueeze(1).unsqueeze(1).to_broadcast([128, NT, 2, 32]),
                                                   in1=tokmeta[:, :, 0:2].unsqueeze(3).to_broadcast([128, NT, 2, 32]), op=ALU.is_equal),
                   ["iota32"] + TMALL, w=M_ + ["mdP"])
                dm(lambda: nc.vector.tensor_tensor(out=mdP[:], in0=mdP[:], in1=off_.unsqueeze(1).unsqueeze(1).to_broadcast([128, NT, 2, 32]), op=ALU.mult),
                   ["mdP"], w=M_ + ["mdP"])
                dm(lambda: nc.vector.tensor_reduce(out=poscf[:], in_=mdP[:], axis=AX.X, op=ALU.add), ["mdP"], w=M_ + ["poscf"])
                T.op("dve", lambda: nc.vector.tensor_tensor(out=poscf[:], in0=poscf[:], in1=tokmeta[:, :, 2:4], op=ALU.add),
                     reads=["poscf"] + [("tokmeta", i_) for i_ in range(NT)], writes=["poscf"])
                T.op("dve", lambda: nc.vector.tensor_copy(out=posc_i[:], in_=poscf[:].rearrange("p n j -> p (n j)")),
                     reads=["poscf"], writes=["posc"])

                for g in range(NG):
                    b = g % 2
                    t0 = g * 512
                    T.op("pool", lambda: nc.gpsimd.dma_start(out=h2tok[b][:], in_=H2[t0:t0 + 512, :].rearrange("(t p) d -> p t d", p=128)),
                         reads=[("H2", g)], writes=[("h2tok", b, t) for t in range(4)], dsem=f"ldh2{b}")
                    for t in range(4):
                        tile_i = g * 4 + t
                        for j in range(2):
                            T.op("pool", lambda: nc.gpsimd.indirect_dma_start(
                                out=HS, out_offset=bass.IndirectOffsetOnAxis(ap=posc_i[:, tile_i * 2 + j:tile_i * 2 + j + 1], axis=0),
                                in_=h2tok[b][:, t, :], in_offset=None, bounds_check=reg_hs, oob_is_err=False),
                                reads=[("h2tok", b, t), "posc"], writes=[("HS", tile_i, j)], dsem=f"sc{b}{j}")

            HSKEYS = [("HS", i_, j) for i_ in range(NT) for j in range(2)]
            with contextlib.ExitStack() as st:
                hs = [sb(f"hs{i}", [128, 4, D], BF16, st) for i in range(2)]
                wall = [sb(f"wall{i}", [128, 12288], BF16, st) for i in range(2)]
                hTs = [sb(f"hTs{i}", [128, 8, 512], BF16, st) for i in range(2)]
                aT = [sb(f"aT{i}", [128, 4, 512], BF16, st) for i in range(2)]
                sgt = [sb(f"sgt{i}", [128, 512], F32, st) for i in range(2)]
                yout = [sb(f"yout{i}", [128, 4, D], F32, st) for i in range(2)]
                psb = [ps[i][:].bitcast(BF16) for i in range(2)]
                WBKEYS = [("WB", e) for e in range(NE)]
                ev = 0

                def d_loads(k_):
                    hb_ = k_ % 2
                    T.op("pool", lambda: nc.gpsimd.dma_start(out=hs[hb_][:], in_=HS[k_ * 512:(k_ + 1) * 512, :].rearrange("(s p) d -> p s d", p=128)),
                         reads=HSKEYS, writes=[("hs", hb_)], dsem=f"hs{hb_}")
                    T.op("pool", lambda: nc.gpsimd.indirect_dma_start(
                        out=wall[hb_][:], out_offset=None, in_=WB,
                        in_offset=bass.IndirectOffsetOnAxis(ap=widx_i[:, k_:k_ + 1], axis=0),
                        bounds_check=reg_w, oob_is_err=False),
                        reads=["widx"] + WBKEYS, writes=[("wall", hb_)], dsem=f"wall{hb_}")

                for k in range(NTILE):
                    hb = k % 2
                    if k == 0:
                        d_loads(0)
                    if k + 1 < NTILE:
                        d_loads(k + 1)
                    for kc in range(8):
                        bank = kc % 2
                        for s_ in range(4):
                            T.op("pe", lambda: nc.tensor.transpose(psb[bank][:, s_ * 128:(s_ + 1) * 128],
                                                                   hs[hb][:, s_, kc * 128:(kc + 1) * 128], ident_bf[:]),
                                 reads=[("hs", hb), "ident_bf"], writes=[PK(bank)])
                        if kc % 2 == 0:
                            T.op("act", lambda: nc.scalar.copy(out=hTs[hb][:, kc, :], in_=psb[bank][:, 0:512]),
                                 reads=[PK(bank)], writes=[("hTs", hb, kc)])
                        else:
                            T.op("dve", lambda: nc.vector.tensor_copy(out=hTs[hb][:, kc, :], in_=psb[bank][:, 0:512]),
                                 reads=[PK(bank)], writes=[("hTs", hb, kc)])
                    for fc in range(4):
                        bg = 2 + (fc % 2) * 2
                        bu = 3 + (fc % 2) * 2
                        for kc in range(8):
                            T.op("pe", lambda: nc.tensor.matmul(
                                ps[bg][:], lhsT=wall[hb][:, kc * 512 + fc * 128: kc * 512 + (fc + 1) * 128], rhs=hTs[hb][:, kc, :],
                                start=(kc == 0), stop=(kc == 7)), reads=[("wall", hb), ("hTs", hb, kc)], writes=[PK(bg)])
                        for kc in range(8):
                            T.op("pe", lambda: nc.tensor.matmul(
                                ps[bu][:], lhsT=wall[hb][:, 4096 + kc * 512 + fc * 128: 4096 + kc * 512 + (fc + 1) * 128], rhs=hTs[hb][:, kc, :],
                                start=(kc == 0), stop=(kc == 7)), reads=[("wall", hb), ("hTs", hb, kc)], writes=[PK(bu)])
                        si = fc % 2
                        T.op("act", lambda: nc.scalar.activation(out=sgt[si][:], in_=ps[bg][:], func=AF.Silu),
                             reads=[PK(bg)], writes=[("sgt", si)])
                        T.op("dve", lambda: nc.vector.tensor_tensor(out=aT[hb][:, fc, :], in0=ps[bu][:], in1=sgt[si][:], op=ALU.mult),
                             reads=[PK(bu), ("sgt", si)], writes=[("aT", hb)])
                    for t in range(4):
                        for half in range(2):
                            bank = 6 + ev % 2
                            for fc in range(4):
                                T.op("pe", lambda: nc.tensor.matmul(
                                    ps[bank][:], lhsT=aT[hb][:, fc, t * 128:(t + 1) * 128],
                                    rhs=wall[hb][:, 8192 + fc * 1024 + half * 512: 8192 + fc * 1024 + (half + 1) * 512],
                                    start=(fc == 0), stop=(fc == 3)), reads=[("aT", hb), ("wall", hb)], writes=[PK(bank)])
                            if ev % 2 == 0:
                                T.op("act", lambda: nc.scalar.copy(out=yout[hb][:, t, half * 512:(half + 1) * 512], in_=ps[bank][:]),
                                     reads=[PK(bank)], writes=[("yout", hb, t, half)])
                            else:
                                T.op("dve", lambda: nc.vector.tensor_copy(out=yout[hb][:, t, half * 512:(half + 1) * 512], in_=ps[bank][:]),
                                     reads=[PK(bank)], writes=[("yout", hb, t, half)])
                            ev += 1
                    T.op("pool", lambda: nc.gpsimd.dma_start(out=YS[k * 512:(k + 1) * 512, :].rearrange("(s p) d -> p s d", p=128), in_=yout[hb][:]),
                         reads=[("yout", hb, t, half) for t in range(4) for half in range(2)], writes=[("YS", k)], dsem=f"sty{hb}")

            YSKEYS = [("YS", k) for k in range(NTILE)]
            with contextlib.ExitStack() as st:
                l2g = sb("l2g", [128, D], F32, st)
                l2b = sb("l2b", [128, D], F32, st)
                x1t = [sb(f"x1t{i}", [128, D], F32, st) for i in range(2)]
                yg = [sb(f"yg{i}", [128, 2, D], F32, st) for i in range(2)]
                ff = [sb(f"ff{i}", [128, D], F32, st) for i in range(2)]
                yo = [sb(f"yo{i}", [128, D], F32, st) for i in range(2)]
                stats = sb("statsd", [128, 2, 6], F32, st)
                mv = sb("mvd", [128, 2], F32, st)
                rstd = sb("rstdd", [128, 1], F32, st)
                for dst, key, src in ((l2g, "l2g", ln2_g), (l2b, "l2b", ln2_b)):
                    T.op(DQ, lambda: DQE.dma_start(out=dst[:], in_=src[l].partition_broadcast(128)),
                         writes=[key], dsem=key)
                def e_loads(gt_i):
                    r0 = gt_i * 128
                    xb = gt_i % 2
                    T.op(DQ, lambda: DQE.dma_start(out=x1t[xb][:], in_=X1[r0:r0 + 128, :]),
                         reads=[("X1", gt_i // 4)], writes=[("x1t", xb)], dsem=f"x1t{xb}")
                    for j in range(2):
                        T.op("pool", lambda: nc.gpsimd.indirect_dma_start(
                            out=yg[xb][:, j, :], out_offset=None, in_=YS,
                            in_offset=bass.IndirectOffsetOnAxis(ap=posc_i[:, gt_i * 2 + j:gt_i * 2 + j + 1], axis=0),
                            bounds_check=reg_ys, oob_is_err=False),
                            reads=YSKEYS + ["posc"], writes=[("yg", xb)], dsem=f"yg{xb}", nowaw=True)

                e_loads(0)
                for gt_i in range(NT):
                    r0 = gt_i * 128
                    xb = gt_i % 2
                    if gt_i + 1 < NT:
                        e_loads(gt_i + 1)
                    A = [("ff", xb)]
                    T.op("act", lambda: nc.scalar.activation(out=ff[xb][:], in_=yg[xb][:, 0, :], func=AF.Identity, scale=tokmeta[:, gt_i, 4:5]),
                         reads=[("yg", xb), ("tokmeta", gt_i)], writes=A)
                    T.op("act", lambda: nc.scalar.activation(out=yg[xb][:, 1, :], in_=yg[xb][:, 1, :], func=AF.Identity, scale=tokmeta[:, gt_i, 5:6]),
                         reads=[("yg", xb), ("tokmeta", gt_i)], writes=[("yg", xb)])
                    T.op("dve", lambda: nc.vector.tensor_tensor(out=ff[xb][:], in0=ff[xb][:], in1=yg[xb][:, 1, :], op=ALU.add),
                         reads=[("yg", xb)] + A, writes=A)
                    T.op("dve", lambda: nc.vector.tensor_tensor(out=ff[xb][:], in0=ff[xb][:], in1=gt2p[:], op=ALU.mult),
                         reads=A + ["gt2p"], writes=A)
                    T.op("dve", lambda: nc.vector.scalar_tensor_tensor(out=ff[xb][:], in0=x1t[xb][:], scalar=ALPHA, in1=ff[xb][:],
                                                                       op0=ALU.mult, op1=ALU.add),
                         reads=A + [("x1t", xb)], writes=A)
                    for half in range(2):
                        T.op("dve", lambda: nc.vector.bn_stats(out=stats[:, half, :], in_=ff[xb][:, half * 512:(half + 1) * 512]),
                             reads=A, writes=["statsd"])
                    T.op("dve", lambda: nc.vector.bn_aggr(out=mv[:], in_=stats[:].rearrange("p a b -> p (a b)")),
                         reads=["statsd"], writes=["mvd"])
                    rsqrt_small(st, "rstdd", rstd[:], mv[:, 1:2], 1, ["mvd"], "rstdd")
                    T.op("dve", lambda: nc.vector.scalar_tensor_tensor(out=mv[:, 1:2], in0=mv[:, 0:1], scalar=-1.0, in1=rstd[:, 0:1],
                                                                       op0=ALU.mult, op1=ALU.mult),
                         reads=["mvd", "rstdd"], writes=["mvd"])
                    T.op("act", lambda: nc.scalar.activation(out=ff[xb][:], in_=ff[xb][:], func=AF.Identity,
                                                             scale=rstd[:, 0:1], bias=mv[:, 1:2]),
                         reads=A + ["mvd", "rstdd"], writes=A)
                    T.op("pool", lambda: nc.gpsimd.tensor_tensor(out=ff[xb][:], in0=ff[xb][:], in1=l2g[:], op=ALU.mult),
                         reads=A + ["l2g"], writes=A)
                    T.op("pool", lambda: nc.gpsimd.tensor_tensor(out=yo[xb][:], in0=ff[xb][:], in1=l2b[:], op=ALU.add),
                         reads=A + ["l2b"], writes=[("yo", xb)])
                    T.op(DQ, lambda: DQE.dma_start(out=xdst[r0:r0 + 128, :], in_=yo[xb][:]),
                         reads=[("yo", xb)], writes=[("Xt", l + 1, gt_i)], dsem=f"sto{xb}")
                for g in range(NG):
                    T.writers[("X", l + 1, g)] = {}
                    for lt4 in range(4):
                        for s_, v in T.writers.get(("Xt", l + 1, g * 4 + lt4), {}).items():
                            if T.writers[("X", l + 1, g)].get(s_, 0) < v:
                                T.writers[("X", l + 1, g)][s_] = v

        T.final_wait(DQ, [("Xt", L, i) for i in range(NT)])
        print(f"[build] ops={T.n_ops} waits={T.n_waits} sems={len(T.sem)}")
    return nc


def rope_tables(S):
    inv = 1.0 / (10000.0 ** (np.arange(0, 64, 2, dtype=np.float32) / 64.0))
    ang = np.arange(S, dtype=np.float32)[:, None] * inv[None, :].astype(np.float32)
    cos = np.cos(ang).astype(np.float32)
    sin = np.sin(ang).astype(np.float32)
    cosT = np.empty((128, S), np.float32)
    sinT = np.empty((128, S), np.float32)
    for p in range(128):
        d = p % 64
        cosT[p] = cos[:, d % 32]
        sinT[p] = -sin[:, d % 32] if d < 32 else sin[:, d % 32]
    return cosT, sinT


def prep_shared(inp, S, L):
    f = lambda a: np.ascontiguousarray(np.asarray(a, dtype=np.float32))
    w_in = np.asarray(inp["w_in"], dtype=np.float32)[:L]
    q = w_in[:, :, 0:512].reshape(L, D, 4, 2, 64)
    k = w_in[:, :, 512:1024].reshape(L, D, 4, 2, 64)
    swap = lambda t: np.concatenate([t[..., 32:], t[..., :32]], axis=-1).reshape(L, D, 512)
    w_in_x = np.concatenate([w_in[:, :, 0:512], swap(q), w_in[:, :, 512:1024], swap(k), w_in[:, :, 1024:2560]], axis=-1)
    lam4 = np.stack([inp["lambda_q1"], inp["lambda_k1"], inp["lambda_q2"], inp["lambda_k2"]], axis=1)[:L]
    w_r = np.concatenate([np.asarray(inp["w_group"])[:L], np.transpose(np.asarray(inp["w_router"])[:L], (0, 2, 1, 3)).reshape(L, D, 32)], axis=-1)
    b_r = np.concatenate([np.asarray(inp["b_group"])[:L], np.asarray(inp["b_router"])[:L].reshape(L, 32)], axis=-1)
    wg = np.asarray(inp["w_gate"], dtype=np.float32)[:L].reshape(L, NE, 8, 128, DFF).transpose(0, 1, 3, 2, 4).reshape(L, NE, 128, 8 * DFF)
    wu = np.asarray(inp["w_up"], dtype=np.float32)[:L].reshape(L, NE, 8, 128, DFF).transpose(0, 1, 3, 2, 4).reshape(L, NE, 128, 8 * DFF)
    wd = np.asarray(inp["w_down"], dtype=np.float32)[:L].reshape(L, NE, 4, 128, D).transpose(0, 1, 3, 2, 4).reshape(L, NE, 128, 4 * D)
    wall = np.ascontiguousarray(np.concatenate([wg, wu, wd], axis=-1).reshape(L, NE * 128, 12288))
    del wg, wu, wd
    cosT, sinT = rope_tables(S)
    tri = np.triu(np.ones((128, 128), np.float32))
    sh = {
        "w_ada": f(inp["w_ada"][:L]), "b_ada": f(inp["b_ada"][:L]), "w_in": f(w_in_x), "lam4": f(lam4),
        "subln_g": f(inp["subln_g"][:L]), "sg_ln_g": f(np.asarray(inp["sg_ln_g"])[:L].reshape(L, 512)),
        "sg_ln_b": f(np.asarray(inp["sg_ln_b"])[:L].reshape(L, 512)),
        "w_spT": f(np.transpose(np.asarray(inp["w_spatial"])[:L], (0, 1, 3, 2))),
        "b_sp": f(np.asarray(inp["b_spatial"])[:L].reshape(L, 512)),
        "w_out": f(inp["w_out"][:L]), "ln1_g": f(inp["ln1_g"][:L]), "ln1_b": f(inp["ln1_b"][:L]),
        "w_r": f(w_r), "b_r": f(b_r),
        "wall": wall, "triu1": np.triu(np.ones((128, 128), np.float32), 1),
        "pcol": np.stack([np.arange(128, dtype=np.float32), 6.0 * np.arange(128, dtype=np.float32)], axis=1),
        "iota32": np.ascontiguousarray(np.broadcast_to(np.arange(32, dtype=np.float32), (128, 32))),
        "kt512": np.ascontiguousarray(np.broadcast_to(512.0 * np.arange(64, dtype=np.float32), (128, 64))),
        "ln2_g": f(inp["ln2_g"][:L]), "ln2_b": f(inp["ln2_b"][:L]),
        "ident": np.eye(128, dtype=np.float32), "tri": tri, "csT": np.ascontiguousarray(np.stack([cosT, sinT], axis=1)),
    }
    return sh


def run(inp, S, L, n_cores, dbg=False, trace=False):
    nc = build_program(S, L, dbg=dbg)
    sh = prep_shared(inp, S, L)
    x = np.asarray(inp["x"], dtype=np.float32)
    c = np.asarray(inp["c"], dtype=np.float32)
    in_maps = []
    for b in range(n_cores):
        m = dict(sh)
        m["x"] = np.ascontiguousarray(x[b])
        m["cT"] = np.ascontiguousarray(c[b].reshape(8, 128).T)
        in_maps.append(m)
    res = run_bass_kernel_spmd(nc, in_maps, core_ids=list(range(n_cores)), trace=trace)
    return res


def kernel(**inputs):
    S = inputs["x"].shape[1]
    res = run(inputs, S, 2, 8)
    return np.stack([np.asarray(r["out"], dtype=np.float32) for r in res.results], axis=0)
```

```python
import contextlib
import math
import numpy as np
import concourse.bass as bass
import concourse.mybir as mybir
from concourse.bass_utils import run_bass_kernel_spmd

F32 = mybir.dt.float32
BF16 = mybir.dt.bfloat16
F32R = mybir.dt.float32r
AF = mybir.ActivationFunctionType
ALU = mybir.AluOpType
AX = mybir.AxisListType

D = 1024
NKC = 8
NE = 32
DFF = 512
INX = 3584
ALPHA = (2.0 * 2) ** 0.25
LN_EPS = 1e-5
NEG_BIG = -1.0e30


class Tracker:
    def __init__(self, nc, es):
        self.nc = nc
        self.es = es
        self.eng = {"pe": nc.tensor, "act": nc.scalar, "dve": nc.vector, "pool": nc.gpsimd, "sp": nc.sync}
        self.sem = {}
        self.cnt = {}
        self.pool = [es.enter_context(nc.semaphore(f"sm{i}")) for i in range(88)]
        for h in self.pool:
            nc.gpsimd.sem_clear(h)
        nc.all_engine_barrier()
        for k in self.eng:
            self.sem[k] = self.pool.pop()
            self.cnt[k] = 0
        self.seen = {k: {} for k in self.eng}
        self.writers = {}
        self.readers = {}
        self.n_ops = 0
        self.n_waits = 0

    def _dsem(self, name):
        if name not in self.sem:
            self.sem[name] = self.pool.pop()
            self.cnt[name] = 0
        return name

    def op(self, engname, fn, reads=(), writes=(), dsem=None, nowaw=False):
        deps = {}
        for k in reads:
            for s, v in self.writers.get(k, {}).items():
                if deps.get(s, 0) < v:
                    deps[s] = v
        for k in writes:
            for s, v in self.writers.get(k, {}).items():
                if nowaw and s == dsem:
                    continue
                if deps.get(s, 0) < v:
                    deps[s] = v
            for s, v in self.readers.get(k, {}).items():
                if deps.get(s, 0) < v:
                    deps[s] = v
        seen = self.seen[engname]
        e = self.eng[engname]
        for s, v in deps.items():
            if s == engname and engname == "pe" and dsem is None:
                continue
            if seen.get(s, 0) >= v:
                continue
            e.wait_ge(self.sem[s], v)
            seen[s] = v
            self.n_waits += 1
        ins = fn()
        self.n_ops += 1
        if dsem is not None:
            self._dsem(dsem)
            self.cnt[dsem] += 16
            ins.then_inc(self.sem[dsem], 16)
            ev = (dsem, self.cnt[dsem])
        else:
            self.cnt[engname] += 1
            ins.then_inc(self.sem[engname], 1)
            ev = (engname, self.cnt[engname])
        for k in writes:
            self.writers[k] = {ev[0]: ev[1]}
            self.readers[k] = {}
        for k in reads:
            r = self.readers.setdefault(k, {})
            if r.get(ev[0], 0) < ev[1]:
                r[ev[0]] = ev[1]

    def final_wait(self, engname, keys):
        deps = {}
        for k in keys:
            for s, v in self.writers.get(k, {}).items():
                if deps.get(s, 0) < v:
                    deps[s] = v
        for s, v in deps.items():
            self.eng[engname].wait_ge(self.sem[s], v)


def build_program(S, L, dbg=False):
    assert S % 512 == 0
    NG = S // 512
    NT = S // 128
    TB = min(S, 1024)
    NBLK = S // TB
    nc = bass.Bass("TRN2", target_bir_lowering=False)

    def din(name, shape, dt=F32):
        return nc.dram_tensor(name, list(shape), dt, kind="ExternalInput").ap()

    def dscr(name, shape, dt):
        return nc.dram_tensor(name, list(shape), dt, kind=("ExternalOutput" if dbg else "Internal")).ap()

    x_in = din("x", [S, D])
    cT_in = din("cT", [128, 8])
    w_ada = din("w_ada", [L, D, 6 * D])
    b_ada = din("b_ada", [L, 6 * D])
    w_in = din("w_in", [L, D, INX])
    lam_in = din("lam4", [L, 4, 64])
    subln_g = din("subln_g", [L, 128])
    sg_ln_g = din("sg_ln_g", [L, 512])
    sg_ln_b = din("sg_ln_b", [L, 512])
    w_spT = din("w_spT", [L, 4, 128, 128])
    b_sp = din("b_sp", [L, 512])
    w_out = din("w_out", [L, D, D])
    ln1_g = din("ln1_g", [L, D])
    ln1_b = din("ln1_b", [L, D])
    w_r = din("w_r", [L, D, 36])
    b_r = din("b_r", [L, 36])
    wall_in = din("wall", [L, NE * 128, 12288])
    triu1_in = din("triu1", [128, 128])
    pcol_in = din("pcol", [128, 2])
    iota_in = din("iota32", [128, 32])
    kt_in = din("kt512", [128, 64])
    ln2_g = din("ln2_g", [L, D])
    ln2_b = din("ln2_b", [L, D])
    ident_in = din("ident", [128, 128])
    tri_in = din("tri", [128, 128])
    cs_in = din("csT", [128, 2, S])
    out = nc.dram_tensor("out", [S, D], F32, kind="ExternalOutput").ap()

    QT = dscr("QT", [4, 128, S], BF16)
    KT = dscr("KT", [4, 128, S], BF16)
    VS = dscr("VS", [S, 512], BF16)
    SGT = dscr("SGT", [4, 128, S], BF16)
    ATT = dscr("ATT", [4, 128, S], BF16)
    X1 = dscr("X1", [S, D], F32)
    X2 = dscr("X2", [S, D], F32)
    CAP = S
    NTILE = (2 * S + NE * 511) // 512
    HS = dscr("HS", [NTILE * 512, D], BF16)
    H2 = dscr("H2", [S, D], BF16)
    WB = dscr("WB", [NE * 128, 12288], BF16)
    YS = dscr("YS", [NTILE * 512, D], F32)

    es = contextlib.ExitStack()
    with es:
        T = Tracker(nc, es)
        DQ, DQE = "pool", nc.gpsimd
        reg_hs = nc.gpsimd.to_reg(NTILE * 512 - 1)
        reg_w = nc.gpsimd.to_reg(NE * 128 - 1)
        reg_ys = nc.gpsimd.to_reg(NTILE * 512 - 1)

        uniq = [0]

        def sb(name, shape, dt, stack=es):
            uniq[0] += 1
            return stack.enter_context(nc.sbuf_tensor(f"sb{uniq[0]}_{name}", list(shape), dt))

        ps = [es.enter_context(nc.psum_tensor(f"ps{i}", [128, 512], F32)) for i in range(8)]

        def PK(i):
            return ("ps", i)

        ident = sb("ident", [128, 128], F32)
        tri_bf = sb("tri_bf", [128, 128], BF16)
        ones_bf = sb("ones_bf", [128, 128], BF16)
        onesdiv = sb("onesdiv", [128, 128], F32)
        ones_f32 = sb("ones_f32", [128, 128], F32)
        epscol = sb("epscol", [128, 1], F32)
        ones_tmp = sb("ones_tmp", [128, 128], F32)
        ones_row = sb("ones_row", [1, 128], F32)
        cTs = sb("cTs", [128, 8], F32)
        siluc = sb("siluc", [128, 8], F32)
        modcol = sb("modcol", [128, 32], F32)
        gt1p = sb("gt1p", [128, D], F32)
        gt2p = sb("gt2p", [128, D], F32)
        lamt = sb("lamt", [128, 4, 64], F32)
        lamw = sb("lamw", [128, 8], F32)
        neglam = sb("neglam", [128, 1], F32)
        gcol = sb("gcol", [128, 1], F32)
        sc2b = sb("sc2b", [128, D], F32)
        sh2b = sb("sh2b", [128, D], F32)
        tokmeta = sb("tokmeta", [128, NT, 8], F32)
        ident_bf = sb("ident_bf", [128, 128], BF16)
        triu1_bf = sb("triu1_bf", [128, 128], BF16)
        pcol = sb("pcol", [128, 2], F32)
        iota32 = sb("iota32", [128, 32], F32)
        kt512 = sb("kt512", [128, 64], F32)
        cum = sb("cum", [128, 32], F32)
        cumb = [sb(f"cumb{i}", [128, 32], BF16) for i in range(2)]
        ohb = [sb(f"ohb{i}", [128, 32], BF16) for i in range(2)]
        md = sb("md", [128, 192], F32)
        tl = sb("tl", [128, 5, NTILE], F32)
        widxf = sb("widxf", [128, NTILE], F32)
        widx_i = sb("widx_i", [128, NTILE], mybir.dt.int32)
        poscf = sb("poscf", [128, NT, 2], F32)
        posc_i = sb("posc_i", [128, NT * 2], mybir.dt.int32)

        T.op(DQ, lambda: DQE.dma_start(out=ident[:], in_=ident_in), writes=["ident"], dsem="c0")
        T.op("pool", lambda: nc.gpsimd.dma_start(out=tri_bf[:], in_=tri_in), writes=["tri"], dsem="c1")
        T.op(DQ, lambda: DQE.dma_start(out=cTs[:], in_=cT_in), writes=["cT"], dsem="c0")
        T.op("pool", lambda: nc.gpsimd.dma_start(out=ident_bf[:], in_=ident_in), writes=["ident_bf"], dsem="c2")
        T.op("pool", lambda: nc.gpsimd.dma_start(out=triu1_bf[:], in_=triu1_in), writes=["triu1"], dsem="c3")
        T.op("pool", lambda: nc.gpsimd.dma_start(out=pcol[:], in_=pcol_in), writes=["pcol"], dsem="c4")
        T.op("pool", lambda: nc.gpsimd.dma_start(out=iota32[:], in_=iota_in), writes=["iota32"], dsem="c5")
        T.op("pool", lambda: nc.gpsimd.dma_start(out=kt512[:], in_=kt_in), writes=["kt512"], dsem="c6")
        T.op("dve", lambda: nc.vector.memset(ones_bf[:], 1.0), writes=["ones_bf"])
        T.op("dve", lambda: nc.vector.memset(ones_tmp[:], 1.0 / 128.0), writes=["ones_tmp"])
        T.op("dve", lambda: nc.vector.tensor_copy(out=onesdiv[:].bitcast(F32R), in_=ones_tmp[:]), reads=["ones_tmp"], writes=["onesdiv"])
        T.op("dve", lambda: nc.vector.memset(ones_tmp[:], 1.0), reads=["onesdiv"], writes=["ones_tmp"])
        T.op("dve", lambda: nc.vector.tensor_copy(out=ones_f32[:].bitcast(F32R), in_=ones_tmp[:]), reads=["ones_tmp"], writes=["ones_f32"])
        T.op("dve", lambda: nc.vector.memset(epscol[:], LN_EPS), writes=["epscol"])
        T.op("dve", lambda: nc.vector.memset(ones_row[:], 1.0), writes=["ones_row"])
        T.op("act", lambda: nc.scalar.activation(out=siluc[:], in_=cTs[:], func=AF.Silu),
             reads=["cT"], writes=["siluc"])

        def dump(name, ap, shape, keys, dt=F32):
            if not dbg:
                return
            dt_ = nc.dram_tensor("dbg_" + name, list(shape), dt, kind="ExternalOutput").ap()
            T.op(DQ, lambda: DQE.dma_start(out=dt_, in_=ap), reads=keys, writes=[("dbg", name)], dsem="dbg")

        def rsqrt_small(stack, name, dst, src_ap, n, rd_keys, wr_key):
            T.op("dve", lambda: nc.vector.tensor_scalar(out=dst, in0=src_ap, scalar1=LN_EPS, scalar2=None,
                                                        op0=ALU.add), reads=rd_keys, writes=[wr_key])
            T.op("act", lambda: nc.scalar.activation(out=dst, in_=dst, func=AF.Sqrt),
                 reads=[wr_key], writes=[wr_key])
            T.op("dve", lambda: nc.vector.reciprocal(out=dst, in_=dst), reads=[wr_key], writes=[wr_key])

        for l in range(L):
            lam_init = 0.8 - 0.6 * math.exp(-0.3 * l)
            xsrc = x_in if l == 0 else X2
            xdst = out if l == L - 1 else X2

            with contextlib.ExitStack() as st:
                wada = [sb(f"wada{i}", [128, 8, 1024], F32, st) for i in range(2)]
                modrow = sb("modrow", [1, 6 * D], F32, st)
                badarow = sb("badarow", [1, 6 * D], F32, st)
                T.op(DQ, lambda: DQE.dma_start(out=badarow[:], in_=b_ada[l:l + 1, :]),
                     writes=["badarow"], dsem="badarow")
                for bi, blk in enumerate(range(6)):
                    b = bi % 2
                    T.op("pool", lambda: nc.gpsimd.dma_start(
                        out=wada[b][:], in_=w_ada[l, :, blk * 1024:(blk + 1) * 1024].rearrange("(k p) n -> p k n", p=128)),
                        reads=[("wada", 1 - b)], writes=[("wada", b)], dsem=f"wada{b}")
                    if l == 0 and blk == 0:
                        dump("wada0", wada[0][:], [128, 8, 1024], [("wada", 0)])
                        dump("badarow", badarow[:], [1, 6 * D], ["badarow"])
                    for half in range(2):
                        bank = (blk * 2 + half) % 8
                        c0 = blk * 1024 + half * 512
                        for kc in range(8):
                            T.op("pe", lambda: nc.tensor.matmul(
                                ps[bank][0:1, :], lhsT=siluc[:, kc:kc + 1], rhs=wada[b][:, kc, half * 512:(half + 1) * 512],
                                start=(kc == 0), stop=(kc == 7)),
                                reads=[("wada", b), "siluc"], writes=[PK(bank)])
                        T.op("dve", lambda: nc.vector.tensor_tensor(
                            out=modrow[0:1, c0:c0 + 512], in0=ps[bank][0:1, :], in1=badarow[0:1, c0:c0 + 512], op=ALU.add),
                            reads=[PK(bank), "badarow"], writes=["modrow"])
                if l == 0:
                    dump("modrow", modrow[:], [1, 6 * D], ["modrow"])
                for i, blk in enumerate((0, 1, 3, 4)):
                    for j in range(8):
                        T.op("pe", lambda: nc.tensor.matmul(
                            ps[4][:, i * 8 + j:i * 8 + j + 1], lhsT=modrow[0:1, blk * 1024 + j * 128: blk * 1024 + (j + 1) * 128],
                            rhs=ones_row[0:1, 0:1], start=True, stop=True),
                            reads=["modrow", "ones_row"], writes=[PK(4)])
                T.op("dve", lambda: nc.vector.tensor_copy(out=modcol[:], in_=ps[4][:, 0:32]),
                     reads=[PK(4)], writes=["modcol"])
                for c0 in (8, 24):
                    T.op("dve", lambda: nc.vector.tensor_scalar(
                        out=modcol[:, c0:c0 + 8], in0=modcol[:, c0:c0 + 8], scalar1=1.0, scalar2=None, op0=ALU.add),
                        reads=["modcol"], writes=["modcol"])
                for dst, key, blk, addc in ((gt1p, "gt1p", 2, 1.0), (gt2p, "gt2p", 5, 1.0), (sh2b, "sh2b", 3, 0.0), (sc2b, "sc2b", 4, 1.0)):
                    for half in range(2):
                        bank = 5 + half
                        T.op("pe", lambda: nc.tensor.matmul(
                            ps[bank][:], lhsT=ones_row[0:1, :], rhs=modrow[0:1, blk * 1024 + half * 512: blk * 1024 + (half + 1) * 512],
                            start=True, stop=True), reads=["modrow", "ones_row"], writes=[PK(bank)])
                        T.op("dve", lambda: nc.vector.tensor_scalar(
                            out=dst[:, half * 512:(half + 1) * 512], in0=ps[bank][:], scalar1=addc, scalar2=None, op0=ALU.add),
                            reads=[PK(bank)], writes=[key])
                T.op(DQ, lambda: DQE.dma_start(
                    out=lamt[:].rearrange("p a b -> p (a b)"),
                    in_=lam_in[l].rearrange("a b -> (a b)").partition_broadcast(128)),
                    writes=["lamt"], dsem="lamt")
                T.op("dve", lambda: nc.vector.tensor_tensor(out=lamt[:, 0, :], in0=lamt[:, 0, :], in1=lamt[:, 1, :], op=ALU.mult),
                     reads=["lamt"], writes=["lamt"])
                T.op("dve", lambda: nc.vector.tensor_tensor(out=lamt[:, 2, :], in0=lamt[:, 2, :], in1=lamt[:, 3, :], op=ALU.mult),
                     reads=["lamt"], writes=["lamt"])
                T.op("dve", lambda: nc.vector.tensor_reduce(out=lamw[:, 0:1], in_=lamt[:, 0, :], axis=AX.X, op=ALU.add),
                     reads=["lamt"], writes=["lamw"])
                T.op("dve", lambda: nc.vector.tensor_reduce(out=lamw[:, 1:2], in_=lamt[:, 2, :], axis=AX.X, op=ALU.add),
                     reads=["lamt"], writes=["lamw"])
                T.op("act", lambda: nc.scalar.activation(out=lamw[:, 2:4], in_=lamw[:, 0:2], func=AF.Exp),
                     reads=["lamw"], writes=["lamw"])
                T.op("dve", lambda: nc.vector.tensor_tensor(out=lamw[:, 4:5], in0=lamw[:, 3:4], in1=lamw[:, 2:3], op=ALU.subtract),
                     reads=["lamw"], writes=["lamw"])
                T.op("dve", lambda: nc.vector.tensor_scalar(out=neglam[:], in0=lamw[:, 4:5], scalar1=-lam_init, scalar2=None, op0=ALU.add),
                     reads=["lamw"], writes=["neglam"])
                T.op(DQ, lambda: DQE.dma_start(out=gcol[:], in_=subln_g[l:l + 1, :].rearrange("o p -> p o"),
                                                     allow_slow_non_contiguous=True),
                     writes=["gcol"], dsem="gcol")
                T.op("dve", lambda: nc.vector.tensor_scalar(out=gcol[:], in0=gcol[:], scalar1=1.0 - lam_init, scalar2=None, op0=ALU.mult),
                     reads=["gcol"], writes=["gcol"])

            if l == 0:
                dump("modcol", modcol[:], [128, 32], ["modcol"])
                dump("gt1p", gt1p[:], [128, D], ["gt1p"])
                dump("gt2p", gt2p[:], [128, D], ["gt2p"])
                dump("neglam", neglam[:], [128, 1], ["neglam"])
                dump("gcol", gcol[:], [128, 1], ["gcol"])
                dump("siluc", siluc[:], [128, 8], ["siluc"])
            with contextlib.ExitStack() as st:
                win = sb("win", [128, 8, INX], BF16, st)
                xg = [sb(f"xg{i}", [128, 4, D], F32, st) for i in range(2)]
                hT = [sb(f"hT{i}", [128, 8, 512], BF16, st) for i in range(2)]
                cs = [sb(f"cs{i}", [128, 2, 512], F32, st) for i in range(2)]
                qT = [sb(f"qT{i}", [128, 4, 512], BF16, st) for i in range(2)]
                kT = [sb(f"kT{i}", [128, 4, 512], BF16, st) for i in range(2)]
                vt = [sb(f"vt{i}", [128, 4, 512], BF16, st) for i in range(2)]
                sgT = [sb(f"sgT{i}", [128, 4, 512], BF16, st) for i in range(2)]
                zuT2 = [sb(f"zuT{i}", [128, 4, 512], BF16, st) for i in range(2)]
                tmpa = [sb(f"tmpa{i}", [128, 512], F32, st) for i in range(2)]
                tmpb = [sb(f"tmpb{i}", [128, 512], F32, st) for i in range(2)]
                zv = [sb(f"zv{i}", [128, 512], F32, st) for i in range(2)]
                zvn = [sb(f"zvn{i}", [128, 512], BF16, st) for i in range(2)]
                stats2 = [sb(f"stats{i}", [128, 4, 6], F32, st) for i in range(2)]
                mv2 = [sb(f"mv{i}", [128, 4, 2], F32, st) for i in range(2)]
                rstd2 = [sb(f"rstd{i}", [128, 4], F32, st) for i in range(2)]
                wsT = sb("wsT", [128, 4, 128], BF16, st)
                bspb = sb("bspb", [128, 512], F32, st)
                lngb = sb("lngb", [128, 512], F32, st)
                lnbb = sb("lnbb", [128, 512], F32, st)

                for kc in range(8):
                    T.op("pool", lambda: nc.gpsimd.dma_start(out=win[:, kc, :], in_=w_in[l, kc * 128:(kc + 1) * 128, :],
                                                             max_dma_last_dim=7168),
                         writes=[("win", kc)], dsem=f"win{kc}")
                T.op("pool", lambda: nc.gpsimd.dma_start(out=wsT[:], in_=w_spT[l].rearrange("g s t -> s g t")),
                     writes=["wsT"], dsem="wsT")
                for gi in range(4):
                    T.op("dve", lambda: nc.vector.tensor_tensor(out=wsT[:, gi, :], in0=wsT[:, gi, :], in1=tri_bf[:], op=ALU.mult),
                         reads=["wsT", "tri"], writes=["wsT"])
                for dst, key, src in ((bspb, "bspb", b_sp), (lngb, "lngb", sg_ln_g), (lnbb, "lnbb", sg_ln_b)):
                    T.op(DQ, lambda: DQE.dma_start(out=dst[:], in_=src[l].partition_broadcast(128)),
                         writes=[key], dsem=key)

                bank_rr = [0]

                def nxt_bank():
                    b = 2 + bank_rr[0] % 6
                    bank_rr[0] += 1
                    return b

                EPG = (NE + NG - 1) // NG

                def a_loads(g_):
                    b_ = g_ % 2
                    T.op(DQ, lambda: DQE.dma_start(out=xg[b_][:], in_=xsrc[g_ * 512:(g_ + 1) * 512, :].rearrange("(t p) d -> p t d", p=128)),
                         reads=[("X", l, g_)] if l > 0 else [], writes=[("xg", b_)], dsem=f"xg{b_}")
                    T.op(DQ, lambda: DQE.dma_start(out=cs[b_][:], in_=cs_in[:, :, g_ * 512:(g_ + 1) * 512]),
                         writes=[("cs", b_)], dsem=f"cs{b_}")
                for g in range(NG):
                    b = g % 2
                    t0 = g * 512
                    for e in range(g * EPG, min(NE, (g + 1) * EPG)):
                        T.op("pool", lambda: nc.gpsimd.dma_start(
                            out=WB[e * 128:(e + 1) * 128, :].rearrange("r (a b) -> r a b", b=2048),
                            in_=wall_in[l, e * 128:(e + 1) * 128, :].rearrange("r (a b) -> r a b", b=2048)),
                            writes=[("WB", e)], dsem=f"wb{e % 2}")
                    if g == 0:
                        a_loads(0)
                    if g + 1 < NG:
                        a_loads(g + 1)
                    for kc in range(8):
                        bank = kc % 2
                        for t in range(4):
                            T.op("pe", lambda: nc.tensor.transpose(ps[bank][:, t * 128:(t + 1) * 128],
                                                                   xg[b][:, t, kc * 128:(kc + 1) * 128], ident[:]),
                                 reads=[("xg", b), "ident"], writes=[PK(bank)])
                        T.op("act", lambda: nc.scalar.activation(out=hT[b][:, kc, :], in_=ps[bank][:], func=AF.Identity,
                                                                 bias=modcol[:, kc:kc + 1], scale=modcol[:, 8 + kc:9 + kc]),
                             reads=[PK(bank), "modcol"], writes=[("hT", b, kc)])
                    HT = [("hT", b, kc) for kc in range(8)]
                    for h in range(4):
                        for nm, base, dst in (("q", 0, qT), ("k", 1024, kT)):
                            ba = nxt_bank()
                            bb = nxt_bank()
                            for bank, off in ((ba, base), (bb, base + 512)):
                                for kc in range(8):
                                    T.op("pe", lambda: nc.tensor.matmul(
                                        ps[bank][:], lhsT=win[:, kc, off + h * 128: off + (h + 1) * 128], rhs=hT[b][:, kc, :],
                                        start=(kc == 0), stop=(kc == 7)),
                                        reads=[("win", kc), ("hT", b, kc)], writes=[PK(bank)])
                            i = (h * 2 + (nm == "k")) % 2
                            T.op("dve", lambda: nc.vector.tensor_tensor(out=tmpa[i][:], in0=ps[ba][:], in1=cs[b][:, 0, :], op=ALU.mult),
                                 reads=[PK(ba), ("cs", b)], writes=[("tmpa", i)])
                            T.op("dve", lambda: nc.vector.tensor_tensor(out=tmpb[i][:], in0=ps[bb][:], in1=cs[b][:, 1, :], op=ALU.mult),
                                 reads=[PK(bb), ("cs", b)], writes=[("tmpb", i)])
                            T.op("pool", lambda: nc.gpsimd.tensor_tensor(out=dst[b][:, h, :], in0=tmpa[i][:], in1=tmpb[i][:], op=ALU.add),
                                 reads=[("tmpa", i), ("tmpb", i)], writes=[(nm + "T", b)])
                    zuT = zuT2[b]
                    for gi in range(4):
                        bank = nxt_bank()
                        for kc in range(8):
                            T.op("pe", lambda: nc.tensor.matmul(
                                ps[bank][:], lhsT=win[:, kc, 2560 + gi * 128: 2560 + (gi + 1) * 128], rhs=hT[b][:, kc, :],
                                start=(kc == 0), stop=(kc == 7)),
                                reads=[("win", kc), ("hT", b, kc)], writes=[PK(bank)])
                        T.op("act", lambda: nc.scalar.activation(out=zuT[:, gi, :], in_=ps[bank][:], func=AF.Gelu),
                             reads=[PK(bank)], writes=[("zuT", b, gi)])
                    def a_spatial(tt):
                        bank = nxt_bank()
                        for gi in range(4):
                            T.op("pe", lambda: nc.tensor.matmul(
                                ps[bank][:, gi * 128:(gi + 1) * 128], lhsT=zvn[tt % 2][:, gi * 128:(gi + 1) * 128], rhs=wsT[:, gi, :],
                                start=True, stop=True),
                                reads=[("zvn", tt % 2), "wsT"], writes=[PK(bank)])
                        ti = tt % 2
                        T.op("dve", lambda: nc.vector.tensor_tensor(out=tmpa[ti][:], in0=ps[bank][:], in1=bspb[:], op=ALU.add),
                             reads=[PK(bank), "bspb"], writes=[("tmpa", ti)])
                        T.op("dve", lambda: nc.vector.tensor_tensor(
                            out=sgT[b][:, :, tt * 128:(tt + 1) * 128], in0=tmpa[ti][:].rearrange("p (g t) -> p g t", g=4),
                            in1=zuT[:, :, tt * 128:(tt + 1) * 128], op=ALU.mult),
                            reads=[("tmpa", ti)] + [("zuT", b, gi) for gi in range(4)], writes=[("sgT", b)])
                    for t in range(4):
                        bank = nxt_bank()
                        for kc in range(8):
                            T.op("pe", lambda: nc.tensor.matmul(
                                ps[bank][:], lhsT=hT[b][:, kc, t * 128:(t + 1) * 128], rhs=win[:, kc, 2048:2560],
                                start=(kc == 0), stop=(kc == 7)),
                                reads=[("win", kc), ("hT", b, kc)], writes=[PK(bank)])
                        T.op("act", lambda: nc.scalar.copy(out=vt[b][:, t, :], in_=ps[bank][:]),
                             reads=[PK(bank)], writes=[("vt", b)])
                        bank = nxt_bank()
                        for kc in range(8):
                            T.op("pe", lambda: nc.tensor.matmul(
                                ps[bank][:], lhsT=hT[b][:, kc, t * 128:(t + 1) * 128], rhs=win[:, kc, 3072:3584],
                                start=(kc == 0), stop=(kc == 7)),
                                reads=[("win", kc), ("hT", b, kc)], writes=[PK(bank)])
                        zi = t % 2
                        stats, mv, rstd = stats2[zi], mv2[zi], rstd2[zi]
                        SK, MK, RK = ("stats", zi), ("mv", zi), ("rstd", zi)
                        T.op("act", lambda: nc.scalar.activation(out=zv[zi][:], in_=ps[bank][:], func=AF.Gelu),
                             reads=[PK(bank)], writes=[("zv", zi)])
                        for gi in range(4):
                            T.op("dve", lambda: nc.vector.bn_stats(out=stats[:, gi, :], in_=zv[zi][:, gi * 128:(gi + 1) * 128]),
                                 reads=[("zv", zi)], writes=[SK])
                        for gi in range(4):
                            T.op("dve", lambda: nc.vector.bn_aggr(out=mv[:, gi, :], in_=stats[:, gi, :]),
                                 reads=[SK], writes=[MK])
                        rsqrt_small(st, "rstd", rstd[:], mv[:, :, 1], 4, [MK], RK)
                        for gi in range(4):
                            T.op("dve", lambda: nc.vector.tensor_scalar(
                                out=zv[zi][:, gi * 128:(gi + 1) * 128], in0=zv[zi][:, gi * 128:(gi + 1) * 128],
                                scalar1=mv[:, gi, 0:1], scalar2=rstd[:, gi:gi + 1], op0=ALU.subtract, op1=ALU.mult),
                                reads=[("zv", zi), MK, RK], writes=[("zv", zi)])
                        T.op("dve", lambda: nc.vector.tensor_tensor(out=zv[zi][:], in0=zv[zi][:], in1=lngb[:], op=ALU.mult),
                             reads=[("zv", zi), "lngb"], writes=[("zv", zi)])
                        T.op("dve", lambda: nc.vector.tensor_tensor(out=zvn[zi][:], in0=zv[zi][:], in1=lnbb[:], op=ALU.add),
                             reads=[("zv", zi), "lnbb"], writes=[("zvn", zi)])
                        if t > 0:
                            a_spatial(t - 1)
                    a_spatial(3)
                    T.op("pool", lambda: nc.gpsimd.dma_start(out=QT[:, :, t0:t0 + 512].rearrange("h p t -> p h t"), in_=qT[b][:]),
                         reads=[("qT", b)], writes=[("QT", g)], dsem=f"stq{b}")
                    T.op("pool", lambda: nc.gpsimd.dma_start(out=KT[:, :, t0:t0 + 512].rearrange("h p t -> p h t"), in_=kT[b][:]),
                         reads=[("kT", b)], writes=[("KT", g)], dsem=f"stk{b}")
                    T.op("pool", lambda: nc.gpsimd.dma_start(out=VS[t0:t0 + 512, :].rearrange("(t p) c -> p t c", p=128), in_=vt[b][:]),
                         reads=[("vt", b)], writes=[("VS", g)], dsem=f"stv{b}")
                    T.op("pool", lambda: nc.gpsimd.dma_start(out=SGT[:, :, t0:t0 + 512].rearrange("h p t -> p h t"), in_=sgT[b][:]),
                         reads=[("sgT", b)], writes=[("SGT", g)], dsem=f"sts{b}")

            with contextlib.ExitStack() as st:
                kTh = [sb(f"kTh{i}", [128, S], BF16, st) for i in range(2)]
                vh = [sb(f"vh{i}", [128, NT, 128], BF16, st) for i in range(2)]
                qc_sb = [sb(f"qc{i}", [128, 512], BF16, st) for i in range(2)]
                pb = [sb(f"pb{i}", [128, 2, 512], BF16, st) for i in range(4)]
                SB = ((0, 1), (2, 3))
                racc = [[[sb(f"racc{i}{m}{p_}", [128, 512], F32, st) for p_ in range(2)] for m in range(2)] for i in range(2)]
                rsum = [sb(f"rsum{m}", [128, 512], F32, st) for m in range(2)]
                r1 = sb("r1", [128, 512], F32, st)
                r2 = sb("r2", [128, 512], F32, st)
                fa = sb("fa", [128, 512], F32, st)
                fb = sb("fb", [128, 512], F32, st)
                fsq = sb("fsq", [128, 512], F32, st)
                frs = sb("frs", [128, 512], F32, st)
                ao = [sb(f"ao{i}", [128, 512], BF16, st) for i in range(2)]
                OB = (4, 5, 6, 7)
                def b_load_head(h_):
                    hb_ = h_ % 2
                    T.op(DQ, lambda: DQE.dma_start(out=kTh[hb_][:], in_=KT[h_]),
                         reads=[("KT", g) for g in range(NG)], writes=[("kTh", hb_)], dsem=f"kTh{hb_}")
                    T.op(DQ, lambda: DQE.dma_start(out=vh[hb_][:], in_=VS[:, h_ * 128:(h_ + 1) * 128].rearrange("(n p) c -> p n c", p=128)),
                         reads=[("VS", g) for g in range(NG)], writes=[("vh", hb_)], dsem=f"vh{hb_}")

                def b_load_q(it_):
                    h_, qc_ = divmod(it_, NG)
                    T.op(DQ, lambda: DQE.dma_start(out=qc_sb[it_ % 2][:], in_=QT[h_, :, qc_ * 512:(qc_ + 1) * 512]),
                         reads=[("QT", qc_)], writes=[("qc", it_ % 2)], dsem=f"qc{it_ % 2}")

                b_load_head(0)
                b_load_q(0)
                for h in range(4):
                    hb = h % 2
                    for qc in range(NG):
                        it = h * NG + qc
                        qb = it % 2
                        q0 = qc * 512
                        if qc == 0 and h + 1 < 4:
                            b_load_head(h + 1)
                        if it + 1 < 4 * NG:
                            b_load_q(it + 1)
                        nkb = 4 * (qc + 1)

                        def emit_S(kb):
                            j = kb - 4 * qc
                            c0 = max(0, j) * 128
                            for m in range(2):
                                bank = SB[kb % 2][m]
                                T.op("pe", lambda: nc.tensor.matmul(
                                    ps[bank][:, c0:512], lhsT=kTh[hb][m * 64:(m + 1) * 64, kb * 128:(kb + 1) * 128],
                                    rhs=qc_sb[qb][m * 64:(m + 1) * 64, c0:512], start=True, stop=True),
                                    reads=[("kTh", hb), ("qc", qb)], writes=[PK(bank)])

                        def emit_P(kb):
                            j = kb - 4 * qc
                            c0 = max(0, j) * 128
                            pi = kb % 4
                            for m in range(2):
                                bank = SB[kb % 2][m]
                                T.op("act", lambda: nc.scalar.activation(out=pb[pi][:, m, c0:512], in_=ps[bank][:, c0:512],
                                                                         func=AF.Exp, scale=0.125),
                                     reads=[PK(bank)], writes=[("pb", pi, m)])
                                if j >= 0:
                                    en, ee = ("dve", nc.vector) if m == 0 else ("pool", nc.gpsimd)
                                    T.op(en, lambda: ee.tensor_tensor(
                                        out=pb[pi][:, m, c0:c0 + 128], in0=pb[pi][:, m, c0:c0 + 128], in1=tri_bf[:], op=ALU.mult),
                                        reads=[("pb", pi, m), "tri"], writes=[("pb", pi, m)])

                        def emit_PV(kb):
                            j = kb - 4 * qc
                            c0 = max(0, j) * 128
                            pi = kb % 4
                            for m in range(2):
                                T.op("pe", lambda: nc.tensor.matmul(
                                    ps[OB[2 * m]][:, c0:512], lhsT=vh[hb][:, kb, :], rhs=pb[pi][:, m, c0:512],
                                    start=(kb == 0), stop=(kb == nkb - 1)),
                                    reads=[("vh", hb), ("pb", pi, m)], writes=[PK(OB[2 * m])])
                                if m == 1 and kb % 3 != 0:
                                    T.op("pe", lambda: nc.tensor.matmul(
                                        ps[7][:, c0:512], lhsT=ones_bf[:], rhs=pb[pi][:, m, c0:512],
                                        start=(kb == 1), stop=False),
                                        reads=["ones_bf", ("pb", pi, m)], writes=[PK(7)])
                                    continue
                                ai_ = (kb % 2) if m == 0 else ((kb // 3) % 2)
                                ra = racc[qc % 2][m][ai_]
                                rk = ("racc", qc % 2, m, ai_)
                                if (m, ai_) not in acc_used:
                                    acc_used.add((m, ai_))
                                    T.op("dve", lambda: nc.vector.tensor_copy(out=ra[:, c0:512], in_=pb[pi][:, m, c0:512]),
                                         reads=[("pb", pi, m)], writes=[rk])
                                    if c0 > 0:
                                        T.op("dve", lambda: nc.vector.memset(ra[:, 0:c0], 0.0), reads=[rk], writes=[rk])
                                else:
                                    T.op("dve", lambda: nc.vector.tensor_tensor(out=ra[:, c0:512], in0=ra[:, c0:512], in1=pb[pi][:, m, c0:512], op=ALU.add),
                                         reads=[("pb", pi, m), rk], writes=[rk])

                        acc_used = set()
                        emit_S(0)
                        for kb in range(nkb):
                            if kb + 1 < nkb:
                                emit_S(kb + 1)
                            emit_P(kb)
                            emit_PV(kb)
                        for m in range(2):
                            used = [p_ for p_ in range(2) if (m, p_) in acc_used]
                            assert len(used) == 2
                            T.op("dve", lambda: nc.vector.tensor_tensor(out=rsum[m][:].bitcast(F32R), in0=racc[qc % 2][m][0][:], in1=racc[qc % 2][m][1][:], op=ALU.add),
                                 reads=[("racc", qc % 2, m, 0), ("racc", qc % 2, m, 1)], writes=[("rsum", m)])
                            T.op("pe", lambda: nc.tensor.matmul(ps[5 + 2 * m][:], lhsT=ones_f32[:].bitcast(F32R), rhs=rsum[m][:].bitcast(F32R),
                                                                start=(m == 0), stop=True),
                                 reads=["ones_f32", ("rsum", m)], writes=[PK(5 + 2 * m)])
                        T.op("act", lambda: nc.scalar.activation(out=r1[:], in_=ps[5][:], func=AF.Ln), reads=[PK(5)], writes=["r1"])
                        T.op("act", lambda: nc.scalar.activation(out=r1[:], in_=r1[:], func=AF.Exp, scale=-1.0), reads=["r1"], writes=["r1"])
                        T.op("act", lambda: nc.scalar.activation(out=r2[:], in_=ps[7][:], func=AF.Ln), reads=[PK(7)], writes=["r2"])
                        T.op("act", lambda: nc.scalar.activation(out=r2[:], in_=r2[:], func=AF.Exp, scale=-1.0), reads=["r2"], writes=["r2"])
                        T.op("dve", lambda: nc.vector.tensor_tensor(out=fa[:], in0=ps[4][:], in1=r1[:], op=ALU.mult),
                             reads=[PK(4), "r1"], writes=["fa"])
                        T.op("dve", lambda: nc.vector.tensor_tensor(out=fb[:], in0=ps[6][:], in1=r2[:], op=ALU.mult),
                             reads=[PK(6), "r2"], writes=["fb"])
                        T.op("dve", lambda: nc.vector.scalar_tensor_tensor(out=fa[:], in0=fb[:], scalar=neglam[:, 0:1], in1=fa[:],
                                                                           op0=ALU.mult, op1=ALU.add),
                             reads=["fa", "fb", "neglam"], writes=["fa"])
                        T.op("act", lambda: nc.scalar.activation(out=fsq[:].bitcast(F32R), in_=fa[:], func=AF.Square),
                             reads=["fa"], writes=["fsq"])
                        T.op("pe", lambda: nc.tensor.matmul(ps[0][:], lhsT=onesdiv[:].bitcast(F32R), rhs=fsq[:].bitcast(F32R), start=True, stop=True),
                             reads=["onesdiv", "fsq"], writes=[PK(0)])
                        T.op("act", lambda: nc.scalar.activation(out=frs[:], in_=ps[0][:], func=AF.Ln, bias=epscol[:, 0:1]),
                             reads=[PK(0), "epscol"], writes=["frs"])
                        T.op("act", lambda: nc.scalar.activation(out=frs[:], in_=frs[:], func=AF.Exp, scale=-0.5), reads=["frs"], writes=["frs"])
                        ai = qc % 2
                        T.op("dve", lambda: nc.vector.scalar_tensor_tensor(out=ao[ai][:], in0=fa[:], scalar=gcol[:, 0:1], in1=frs[:],
                                                                           op0=ALU.mult, op1=ALU.mult),
                             reads=["fa", "frs", "gcol"], writes=[("ao", ai)])
                        T.op("pool", lambda: nc.gpsimd.dma_start(out=ATT[h, :, q0:q0 + 512], in_=ao[ai][:]),
                             reads=[("ao", ai)], writes=[("ATT", h, qc)], dsem=f"sta{ai}")

            with contextlib.ExitStack() as st:
                wout = sb("wout", [128, 8, D], BF16, st)
                wr = sb("wr", [128, 8, 36], BF16, st)
                brb = sb("brb", [128, 36], F32, st)
                l1g = sb("l1g", [128, D], F32, st)
                l1b = sb("l1b", [128, D], F32, st)
                mixT = [sb(f"mixT{i}", [128, 8, 512], BF16, st) for i in range(2)]
                xg = [sb(f"xgc{i}", [128, 4, D], F32, st) for i in range(2)]
                x1g = [sb(f"x1g{i}", [128, 4, D], F32, st) for i in range(2)]
                h2T = [sb(f"h2T{i}", [128, 8, 512], BF16, st) for i in range(2)]
                ytmp2 = [sb(f"ytmp{i}", [128, D], F32, st) for i in range(2)]
                stats2 = [sb(f"statsc{i}", [128, 2, 6], F32, st) for i in range(2)]
                mv2 = [sb(f"mvc{i}", [128, 2], F32, st) for i in range(2)]
                rstd2 = [sb(f"rstdc{i}", [128, 1], F32, st) for i in range(2)]
                rg_s = []
                for i in range(2):
                    rg_s.append((sb(f"rlg{i}", [128, 4, 36], F32, st), sb(f"rs4{i}", [128, 4, 16], F32, st), sb(f"rohg{i}", [128, 4, 4], F32, st),
                                 sb(f"rgexp{i}", [128, 4, 4], F32, st), sb(f"rsel{i}", [128, 4, 8], F32, st), sb(f"roh1{i}", [128, 4, 8], F32, st),
                                 sb(f"roh2{i}", [128, 4, 8], F32, st), sb(f"rsel2{i}", [128, 4, 8], F32, st), sb(f"rohs{i}", [128, 4, 8], F32, st),
                                 sb(f"rRg{i}", [128, 4, 8], F32, st), sb(f"rt8{i}", [128, 4, 8], F32, st), sb(f"rt4{i}", [128, 4, 4], F32, st),
                                 sb(f"rprod{i}", [128, 4, 8, 4], F32, st), sb(f"rtmp32{i}", [128, 32], F32, st)))
                ohb4 = [sb(f"ohb4{i}", [128, 4, 32], BF16, st) for i in range(2)]
                h2f = sb("h2f", [128, D], F32, st)
                mdK = sb("mdK", [128, NTILE, 32], F32, st)
                mdP = sb("mdP", [128, NT, 2, 32], F32, st)
                h2tok = [sb(f"h2tok{i}", [128, 4, D], BF16, st) for i in range(2)]
                T.op("dve", lambda: nc.vector.memset(cum[:], 0.0), writes=["cum"])
                T.op("dve", lambda: nc.vector.memset(cumb[0][:], 0.0), writes=[("cumb", 0)])
                T.op("pool", lambda: nc.gpsimd.dma_start(out=wout[:], in_=w_out[l].rearrange("(k p) n -> p k n", p=128),
                                                         max_dma_last_dim=4096),
                     writes=["wout"], dsem="wout")
                T.op("pool", lambda: nc.gpsimd.dma_start(out=wr[:], in_=w_r[l].rearrange("(k p) n -> p k n", p=128)),
                     writes=["wr"], dsem="wr")
                for dst, key, src in ((brb, "brb", b_r), (l1g, "l1g", ln1_g), (l1b, "l1b", ln1_b)):
                    T.op(DQ, lambda: DQE.dma_start(out=dst[:], in_=src[l].partition_broadcast(128)),
                         writes=[key], dsem=key)
                def c_loads(g_):
                    b_ = g_ % 2
                    T.op(DQ, lambda: DQE.dma_start(out=mixT[b_][:, 0:4, :], in_=ATT[:, :, g_ * 512:(g_ + 1) * 512].rearrange("h p t -> p h t")),
                         reads=[("ATT", h, g_) for h in range(4)], writes=[("mixTa", b_)], dsem=f"mixTa{b_}")
                    T.op(DQ, lambda: DQE.dma_start(out=mixT[b_][:, 4:8, :], in_=SGT[:, :, g_ * 512:(g_ + 1) * 512].rearrange("h p t -> p h t")),
                         reads=[("SGT", g_)], writes=[("mixTb", b_)], dsem=f"mixTb{b_}")
                    T.op(DQ, lambda: DQE.dma_start(out=xg[b_][:], in_=xsrc[g_ * 512:(g_ + 1) * 512, :].rearrange("(t p) d -> p t d", p=128)),
                         reads=[("X", l, g_)] if l > 0 else [], writes=[("xgc", b_)], dsem=f"xgc{b_}")

                for g in range(NG):
                    b = g % 2
                    t0 = g * 512
                    if g == 0:
                        c_loads(0)
                    if g + 1 < NG:
                        c_loads(g + 1)
                    for t in range(4):
                        yp = t % 2
                        ytmp, stats, mv, rstd = ytmp2[yp], stats2[yp], mv2[yp], rstd2[yp]
                        YK, SK, MK, RK = ("ytmp", yp), ("statsc", yp), ("mvc", yp), ("rstdc", yp)
                        for half in range(2):
                            bank = 2 + (t * 2 + half) % 6
                            for kc in range(8):
                                T.op("pe", lambda: nc.tensor.matmul(
                                    ps[bank][:], lhsT=mixT[b][:, kc, t * 128:(t + 1) * 128], rhs=wout[:, kc, half * 512:(half + 1) * 512],
                                    start=(kc == 0), stop=(kc == 7)),
                                    reads=[("mixTa", b), ("mixTb", b), "wout"], writes=[PK(bank)])
                            T.op("dve", lambda: nc.vector.tensor_tensor(
                                out=ytmp[:, half * 512:(half + 1) * 512], in0=ps[bank][:], in1=gt1p[:, half * 512:(half + 1) * 512], op=ALU.mult),
                                reads=[PK(bank), "gt1p"], writes=[YK])
                        T.op("dve", lambda: nc.vector.scalar_tensor_tensor(out=ytmp[:], in0=xg[b][:, t, :], scalar=ALPHA, in1=ytmp[:],
                                                                           op0=ALU.mult, op1=ALU.add),
                             reads=[("xgc", b), YK], writes=[YK])
                        for half in range(2):
                            T.op("dve", lambda: nc.vector.bn_stats(out=stats[:, half, :], in_=ytmp[:, half * 512:(half + 1) * 512]),
                                 reads=[YK], writes=[SK])
                        T.op("dve", lambda: nc.vector.bn_aggr(out=mv[:], in_=stats[:].rearrange("p a b -> p (a b)")),
                             reads=[SK], writes=[MK])
                        rsqrt_small(st, "r", rstd[:], mv[:, 1:2], 1, [MK], RK)
                        T.op("dve", lambda: nc.vector.tensor_scalar(out=ytmp[:], in0=ytmp[:], scalar1=mv[:, 0:1], scalar2=rstd[:, 0:1],
                                                                    op0=ALU.subtract, op1=ALU.mult),
                             reads=[YK, MK, RK], writes=[YK])
                        T.op("pool", lambda: nc.gpsimd.tensor_tensor(out=ytmp[:], in0=ytmp[:], in1=l1g[:], op=ALU.mult),
                             reads=[YK, "l1g"], writes=[YK])
                        T.op("pool", lambda: nc.gpsimd.tensor_tensor(out=x1g[b][:, t, :], in0=ytmp[:], in1=l1b[:], op=ALU.add),
                             reads=[YK, "l1b"], writes=[("x1g", b, t)])
                    X1G = [("x1g", b, t) for t in range(4)]
                    for kc in range(8):
                        bank = kc % 2
                        for t in range(4):
                            T.op("pe", lambda: nc.tensor.transpose(ps[bank][:, t * 128:(t + 1) * 128],
                                                                   x1g[b][:, t, kc * 128:(kc + 1) * 128], ident[:]),
                                 reads=[("x1g", b, t), "ident"], writes=[PK(bank)])
                        T.op("act", lambda: nc.scalar.activation(out=h2T[b][:, kc, :], in_=ps[bank][:], func=AF.Identity,
                                                                 bias=modcol[:, 16 + kc:17 + kc], scale=modcol[:, 24 + kc:25 + kc]),
                             reads=[PK(bank), "modcol"], writes=[("h2T", b)])
                    gq = g % 2
                    RK_ = [("rg", gq)]
                    lg, s4, ohg, gexp, sel, oh1, oh2, sel2, ohs8, Rg, t8, t4, prod, tmp32 = rg_s[gq]
                    LB = 2 + gq
                    RB = 4 + gq

                    def dv(fn, extra_r=(), w=None):
                        T.op("dve", fn, reads=RK_ + list(extra_r), writes=(RK_ if w is None else w))

                    def bc(ap, shape):
                        return ap.to_broadcast(shape)
                    for t in range(4):
                        for kc in range(8):
                            T.op("pe", lambda: nc.tensor.matmul(
                                ps[LB][:, t * 64:t * 64 + 36], lhsT=h2T[b][:, kc, t * 128:(t + 1) * 128], rhs=wr[:, kc, :],
                                start=(kc == 0), stop=(kc == 7)),
                                reads=[("h2T", b), "wr"], writes=[PK(LB)])
                    dv(lambda: nc.vector.tensor_tensor(
                        out=lg[:], in0=ps[LB][:, 0:256].rearrange("p (t c) -> p t c", c=64)[:, :, 0:36],
                        in1=bc(brb[:].unsqueeze(1), [128, 4, 36]), op=ALU.add), [PK(LB), "brb"])
                    dv(lambda: nc.vector.tensor_reduce(out=s4[:, :, 0], in_=lg[:, :, 0:4], axis=AX.X, op=ALU.max))
                    dv(lambda: nc.vector.tensor_tensor(out=ohg[:], in0=lg[:, :, 0:4], in1=bc(s4[:, :, 0:1], [128, 4, 4]), op=ALU.is_equal))
                    dv(lambda: nc.vector.tensor_tensor(out=gexp[:], in0=lg[:, :, 0:4], in1=bc(s4[:, :, 0:1], [128, 4, 4]), op=ALU.subtract))
                    T.op("act", lambda: nc.scalar.activation(out=gexp[:], in_=gexp[:], func=AF.Exp), reads=RK_, writes=RK_)
                    dv(lambda: nc.vector.tensor_reduce(out=s4[:, :, 1], in_=gexp[:], axis=AX.X, op=ALU.add))
                    dv(lambda: nc.vector.reciprocal(out=s4[:, :, 2], in_=s4[:, :, 1]))
                    dv(lambda: nc.vector.tensor_tensor(
                        out=prod[:], in0=lg[:, :, 4:36].rearrange("p t (g e) -> p t e g", g=4),
                        in1=bc(ohg[:].unsqueeze(2), [128, 4, 8, 4]), op=ALU.mult))
                    dv(lambda: nc.vector.tensor_reduce(out=sel[:], in_=prod[:], axis=AX.X, op=ALU.add))
                    dv(lambda: nc.vector.tensor_reduce(out=s4[:, :, 3], in_=sel[:], axis=AX.X, op=ALU.max))
                    dv(lambda: nc.vector.tensor_tensor(out=oh1[:], in0=sel[:], in1=bc(s4[:, :, 3:4], [128, 4, 8]), op=ALU.is_equal))
                    dv(lambda: nc.vector.scalar_tensor_tensor(
                        out=sel2[:].rearrange("p t e -> p (t e)"), in0=oh1[:].rearrange("p t e -> p (t e)"), scalar=NEG_BIG,
                        in1=sel[:].rearrange("p t e -> p (t e)"), op0=ALU.mult, op1=ALU.add))
                    dv(lambda: nc.vector.tensor_reduce(out=s4[:, :, 4], in_=sel2[:], axis=AX.X, op=ALU.max))
                    dv(lambda: nc.vector.tensor_tensor(out=oh2[:], in0=sel2[:], in1=bc(s4[:, :, 4:5], [128, 4, 8]), op=ALU.is_equal))
                    dv(lambda: nc.vector.tensor_tensor(out=s4[:, :, 5], in0=s4[:, :, 4], in1=s4[:, :, 3], op=ALU.subtract))
                    T.op("act", lambda: nc.scalar.activation(out=s4[:, :, 5], in_=s4[:, :, 5], func=AF.Exp), reads=RK_, writes=RK_)
                    dv(lambda: nc.vector.tensor_scalar(out=s4[:, :, 5], in0=s4[:, :, 5], scalar1=1.0, scalar2=None, op0=ALU.add))
                    dv(lambda: nc.vector.reciprocal(out=s4[:, :, 6], in_=s4[:, :, 5]))
                    dv(lambda: nc.vector.tensor_scalar(out=s4[:, :, 7], in0=s4[:, :, 6], scalar1=-1.0, scalar2=1.0, op0=ALU.mult, op1=ALU.add))
                    dv(lambda: nc.vector.tensor_tensor(out=s4[:, :, 6:8], in0=s4[:, :, 6:8], in1=bc(s4[:, :, 2:3], [128, 4, 2]), op=ALU.mult))
                    dv(lambda: nc.vector.tensor_tensor(out=ohs8[:], in0=oh1[:], in1=oh2[:], op=ALU.add))
                    dv(lambda: nc.vector.tensor_tensor(
                        out=ohb4[gq][:].rearrange("p t (g e) -> p t g e", g=4), in0=bc(ohs8[:].unsqueeze(2), [128, 4, 4, 8]),
                        in1=bc(ohg[:].unsqueeze(3), [128, 4, 4, 8]), op=ALU.mult), w=RK_ + [("ohb4", gq)])
                    for t in range(4):
                        mms = [(triu1_bf, ohb4[gq][:, t, :], "triu1")] + [(ones_bf, ohb4[gq][:, t2, :], "ones_bf") for t2 in range(t)] \
                            + [(ones_bf, cumb[gq][:], "ones_bf")]
                        for mi, (lh, rh, lk) in enumerate(mms):
                            T.op("pe", lambda: nc.tensor.matmul(ps[RB][:, t * 32:(t + 1) * 32], lhsT=lh[:], rhs=rh,
                                                                start=(mi == 0), stop=(mi == len(mms) - 1)),
                                 reads=[("ohb4", gq), ("cumb", gq), lk], writes=[PK(RB)])
                    T.op("dve", lambda: nc.vector.tensor_reduce(out=tmp32[:], in_=ohb4[gq][:].rearrange("p t e -> p e t"), axis=AX.X, op=ALU.add),
                         reads=[("ohb4", gq)] + RK_, writes=RK_)
                    T.op("dve", lambda: nc.vector.tensor_tensor(out=cum[:], in0=cum[:], in1=tmp32[:], op=ALU.add),
                         reads=["cum"] + RK_, writes=["cum"])
                    T.op("dve", lambda: nc.vector.tensor_copy(out=cumb[1 - gq][:], in_=cum[:]),
                         reads=["cum"], writes=[("cumb", 1 - gq)])
                    dv(lambda: nc.vector.tensor_tensor(
                        out=prod[:], in0=ps[RB][:, 0:128].rearrange("p (t g e) -> p t e g", t=4, g=4),
                        in1=bc(ohg[:].unsqueeze(2), [128, 4, 8, 4]), op=ALU.mult), [PK(RB)])
                    dv(lambda: nc.vector.tensor_reduce(out=Rg[:], in_=prod[:], axis=AX.X, op=ALU.add))
                    dv(lambda: nc.vector.tensor_tensor(out=t8[:], in0=oh1[:], in1=Rg[:], op=ALU.mult))
                    dv(lambda: nc.vector.tensor_reduce(out=s4[:, :, 13], in_=t8[:], axis=AX.X, op=ALU.add))
                    dv(lambda: nc.vector.tensor_tensor(out=t8[:], in0=oh2[:], in1=Rg[:], op=ALU.mult))
                    dv(lambda: nc.vector.tensor_reduce(out=s4[:, :, 14], in_=t8[:], axis=AX.X, op=ALU.add))
                    dv(lambda: nc.vector.tensor_tensor(out=t4[:], in0=ohg[:], in1=bc(iota32[:, 0:4].unsqueeze(1), [128, 4, 4]), op=ALU.mult), ["iota32"])
                    dv(lambda: nc.vector.tensor_reduce(out=s4[:, :, 8], in_=t4[:], axis=AX.X, op=ALU.add))
                    dv(lambda: nc.vector.tensor_tensor(out=t8[:], in0=oh1[:], in1=bc(iota32[:, 0:8].unsqueeze(1), [128, 4, 8]), op=ALU.mult), ["iota32"])
                    dv(lambda: nc.vector.tensor_reduce(out=s4[:, :, 9], in_=t8[:], axis=AX.X, op=ALU.add))
                    dv(lambda: nc.vector.tensor_tensor(out=t8[:], in0=oh2[:], in1=bc(iota32[:, 0:8].unsqueeze(1), [128, 4, 8]), op=ALU.mult), ["iota32"])
                    dv(lambda: nc.vector.tensor_reduce(out=s4[:, :, 10], in_=t8[:], axis=AX.X, op=ALU.add))
                    dv(lambda: nc.vector.tensor_scalar(out=s4[:, :, 8], in0=s4[:, :, 8], scalar1=8.0, scalar2=None, op0=ALU.mult))
                    dv(lambda: nc.vector.tensor_tensor(out=s4[:, :, 11:13], in0=s4[:, :, 9:11], in1=bc(s4[:, :, 8:9], [128, 4, 2]), op=ALU.add))
                    TMK = [("tokmeta", g * 4 + t) for t in range(4)]
                    dv(lambda: nc.vector.tensor_copy(out=tokmeta[:, g * 4:(g + 1) * 4, 0:2], in_=s4[:, :, 11:13]), w=RK_ + TMK)
                    dv(lambda: nc.vector.tensor_copy(out=tokmeta[:, g * 4:(g + 1) * 4, 2:4], in_=s4[:, :, 13:15]), w=RK_ + TMK)
                    dv(lambda: nc.vector.tensor_copy(out=tokmeta[:, g * 4:(g + 1) * 4, 4:6], in_=s4[:, :, 6:8]), w=RK_ + TMK)
                    for t in range(4):
                        T.op("pool", lambda: nc.gpsimd.tensor_tensor(out=h2f[:], in0=x1g[b][:, t, :], in1=sc2b[:], op=ALU.mult),
                             reads=[("x1g", b, t), "sc2b"], writes=["h2f"])
                        T.op("pool", lambda: nc.gpsimd.tensor_tensor(out=h2tok[b][:, t, :], in0=h2f[:], in1=sh2b[:], op=ALU.add),
                             reads=["h2f", "sh2b"], writes=[("h2tok", b, t)])
                    T.op("pool", lambda: nc.gpsimd.dma_start(out=H2[t0:t0 + 512, :].rearrange("(t p) d -> p t d", p=128), in_=h2tok[b][:]),
                         reads=[("h2tok", b, t) for t in range(4)], writes=[("H2", g)], dsem=f"sth2{b}")
                    T.op("pool", lambda: nc.gpsimd.dma_start(out=X1[t0:t0 + 512, :].rearrange("(t p) d -> p t d", p=128), in_=x1g[b][:]),
                         reads=X1G, writes=[("X1", g)], dsem=f"stx1{b}")

                MB = 5
                fin = NG % 2
                T.op("pe", lambda: nc.tensor.matmul(ps[MB][:, 0:32], lhsT=ones_bf[:], rhs=cumb[fin][:], start=True, stop=True),
                     reads=[("cumb", fin), "ones_bf"], writes=[PK(MB)])
                M_ = ["md"]

                def dm(fn, extra_r=(), w=M_):
                    T.op("dve", fn, reads=M_ + list(extra_r), writes=w)
                cnt_, ntl_, off_, end_, tmp_, tmp2_ = (md[:, i * 32:(i + 1) * 32] for i in range(6))
                dm(lambda: nc.vector.tensor_copy(out=cnt_, in_=ps[MB][:, 0:32]), [PK(MB)])
                dm(lambda: nc.vector.tensor_scalar(out=ntl_, in0=cnt_, scalar1=0.0, scalar2=None, op0=ALU.is_gt))
                for j in range(1, NG):
                    dm(lambda: nc.vector.scalar_tensor_tensor(out=ntl_, in0=cnt_, scalar=j * 512.0, in1=ntl_, op0=ALU.is_gt, op1=ALU.add))
                dm(lambda: nc.vector.tensor_scalar(out=ntl_, in0=ntl_, scalar1=512.0, scalar2=None, op0=ALU.mult))
                dm(lambda: nc.vector.memset(md[:, 64:65], 0.0))
                for e in range(1, NE):
                    dm(lambda: nc.vector.tensor_tensor(out=md[:, 64 + e:65 + e], in0=md[:, 63 + e:64 + e], in1=md[:, 31 + e:32 + e], op=ALU.add))
                dm(lambda: nc.vector.tensor_tensor(out=end_, in0=off_, in1=ntl_, op=ALU.add))
                dm(lambda: nc.vector.tensor_tensor(out=mdK[:], in0=end_.unsqueeze(1).to_broadcast([128, NTILE, 32]),
                                                   in1=kt512[:, 0:NTILE].unsqueeze(2).to_broadcast([128, NTILE, 32]), op=ALU.is_le),
                   ["kt512"], w=M_ + ["mdK"])
                dm(lambda: nc.vector.tensor_reduce(out=tl[:, 0, :], in_=mdK[:], axis=AX.X, op=ALU.add), ["mdK"], w=M_ + ["tl"])
                TL = ["tl"]

                def dt_(fn, extra_r=(), w=TL):
                    T.op("dve", fn, reads=TL + list(extra_r), writes=w)
                dt_(lambda: nc.vector.tensor_scalar(out=tl[:, 2, :], in0=tl[:, 0, :], scalar1=31.5, scalar2=None, op0=ALU.is_le))
                dt_(lambda: nc.vector.tensor_scalar(out=tl[:, 3, :], in0=tl[:, 0, :], scalar1=31.0, scalar2=None, op0=ALU.min))
                dt_(lambda: nc.vector.tensor_scalar(out=widxf[:], in0=tl[:, 3, :], scalar1=128.0, scalar2=pcol[:, 0:1], op0=ALU.mult, op1=ALU.add),
                    ["pcol"], w=TL + ["widxf"])
                T.op("dve", lambda: nc.vector.tensor_copy(out=widx_i[:], in_=widxf[:]), reads=["widxf"], writes=["widx"])
                TMALL = [("tokmeta", i_) for i_ in range(NT)]
                dm(lambda: nc.vector.tensor_tensor(out=mdP[:], in0=iota32[:].unsqueeze(1).unsqueeze(1).to_broadcast([128, NT, 2, 32]),
                                                   in1=tokmeta[:, :, 0:2].unsqueeze(3).to_broadcast([128, NT, 2, 32]), op=ALU.is_equal),
                   ["iota32"] + TMALL, w=M_ + ["mdP"])
                dm(lambda: nc.vector.tensor_tensor(out=mdP[:], in0=mdP[:], in1=off_.unsqueeze(1).unsqueeze(1).to_broadcast([128, NT, 2, 32]), op=ALU.mult),
                   ["mdP"], w=M_ + ["mdP"])
                dm(lambda: nc.vector.tensor_reduce(out=poscf[:], in_=mdP[:], axis=AX.X, op=ALU.add), ["mdP"], w=M_ + ["poscf"])
                T.op("dve", lambda: nc.vector.tensor_tensor(out=poscf[:], in0=poscf[:], in1=tokmeta[:, :, 2:4], op=ALU.add),
                     reads=["poscf"] + [("tokmeta", i_) for i_ in range(NT)], writes=["poscf"])
                T.op("dve", lambda: nc.vector.tensor_copy(out=posc_i[:], in_=poscf[:].rearrange("p n j -> p (n j)")),
                     reads=["poscf"], writes=["posc"])

                for g in range(NG):
                    b = g % 2
                    t0 = g * 512
                    T.op("pool", lambda: nc.gpsimd.dma_start(out=h2tok[b][:], in_=H2[t0:t0 + 512, :].rearrange("(t p) d -> p t d", p=128)),
                         reads=[("H2", g)], writes=[("h2tok", b, t) for t in range(4)], dsem=f"ldh2{b}")
                    for t in range(4):
                        tile_i = g * 4 + t
                        for j in range(2):
                            T.op("pool", lambda: nc.gpsimd.indirect_dma_start(
                                out=HS, out_offset=bass.IndirectOffsetOnAxis(ap=posc_i[:, tile_i * 2 + j:tile_i * 2 + j + 1], axis=0),
                                in_=h2tok[b][:, t, :], in_offset=None, bounds_check=reg_hs, oob_is_err=False),
                                reads=[("h2tok", b, t), "posc"], writes=[("HS", tile_i, j)], dsem=f"sc{b}{j}")

            HSKEYS = [("HS", i_, j) for i_ in range(NT) for j in range(2)]
            with contextlib.ExitStack() as st:
                hs = [sb(f"hs{i}", [128, 4, D], BF16, st) for i in range(2)]
                wall = [sb(f"wall{i}", [128, 12288], BF16, st) for i in range(2)]
                hTs = [sb(f"hTs{i}", [128, 8, 512], BF16, st) for i in range(2)]
                aT = [sb(f"aT{i}", [128, 4, 512], BF16, st) for i in range(2)]
                sgt = [sb(f"sgt{i}", [128, 512], F32, st) for i in range(2)]
                yout = [sb(f"yout{i}", [128, 4, D], F32, st) for i in range(2)]
                psb = [ps[i][:].bitcast(BF16) for i in range(2)]
                WBKEYS = [("WB", e) for e in range(NE)]
                ev = 0

                def d_loads(k_):
                    hb_ = k_ % 2
                    T.op("pool", lambda: nc.gpsimd.dma_start(out=hs[hb_][:], in_=HS[k_ * 512:(k_ + 1) * 512, :].rearrange("(s p) d -> p s d", p=128)),
                         reads=HSKEYS, writes=[("hs", hb_)], dsem=f"hs{hb_}")
                    T.op("pool", lambda: nc.gpsimd.indirect_dma_start(
                        out=wall[hb_][:], out_offset=None, in_=WB,
                        in_offset=bass.IndirectOffsetOnAxis(ap=widx_i[:, k_:k_ + 1], axis=0),
                        bounds_check=reg_w, oob_is_err=False),
                        reads=["widx"] + WBKEYS, writes=[("wall", hb_)], dsem=f"wall{hb_}")

                for k in range(NTILE):
                    hb = k % 2
                    if k == 0:
                        d_loads(0)
                    if k + 1 < NTILE:
                        d_loads(k + 1)
                    for kc in range(8):
                        bank = kc % 2
                        for s_ in range(4):
                            T.op("pe", lambda: nc.tensor.transpose(psb[bank][:, s_ * 128:(s_ + 1) * 128],
                                                                   hs[hb][:, s_, kc * 128:(kc + 1) * 128], ident_bf[:]),
                                 reads=[("hs", hb), "ident_bf"], writes=[PK(bank)])
                        if kc % 2 == 0:
                            T.op("act", lambda: nc.scalar.copy(out=hTs[hb][:, kc, :], in_=psb[bank][:, 0:512]),
                                 reads=[PK(bank)], writes=[("hTs", hb, kc)])
                        else:
                            T.op("dve", lambda: nc.vector.tensor_copy(out=hTs[hb][:, kc, :], in_=psb[bank][:, 0:512]),
                                 reads=[PK(bank)], writes=[("hTs", hb, kc)])
                    for fc in range(4):
                        bg = 2 + (fc % 2) * 2
                        bu = 3 + (fc % 2) * 2
                        for kc in range(8):
                            T.op("pe", lambda: nc.tensor.matmul(
                                ps[bg][:], lhsT=wall[hb][:, kc * 512 + fc * 128: kc * 512 + (fc + 1) * 128], rhs=hTs[hb][:, kc, :],
                                start=(kc == 0), stop=(kc == 7)), reads=[("wall", hb), ("hTs", hb, kc)], writes=[PK(bg)])
                        for kc in range(8):
                            T.op("pe", lambda: nc.tensor.matmul(
                                ps[bu][:], lhsT=wall[hb][:, 4096 + kc * 512 + fc * 128: 4096 + kc * 512 + (fc + 1) * 128], rhs=hTs[hb][:, kc, :],
                                start=(kc == 0), stop=(kc == 7)), reads=[("wall", hb), ("hTs", hb, kc)], writes=[PK(bu)])
                        si = fc % 2
                        T.op("act", lambda: nc.scalar.activation(out=sgt[si][:], in_=ps[bg][:], func=AF.Silu),
                             reads=[PK(bg)], writes=[("sgt", si)])
                        T.op("dve", lambda: nc.vector.tensor_tensor(out=aT[hb][:, fc, :], in0=ps[bu][:], in1=sgt[si][:], op=ALU.mult),
                             reads=[PK(bu), ("sgt", si)], writes=[("aT", hb)])
                    for t in range(4):
                        for half in range(2):
                            bank = 6 + ev % 2
                            for fc in range(4):
                                T.op("pe", lambda: nc.tensor.matmul(
                                    ps[bank][:], lhsT=aT[hb][:, fc, t * 128:(t + 1) * 128],
                                    rhs=wall[hb][:, 8192 + fc * 1024 + half * 512: 8192 + fc * 1024 + (half + 1) * 512],
                                    start=(fc == 0), stop=(fc == 3)), reads=[("aT", hb), ("wall", hb)], writes=[PK(bank)])
                            if ev % 2 == 0:
                                T.op("act", lambda: nc.scalar.copy(out=yout[hb][:, t, half * 512:(half + 1) * 512], in_=ps[bank][:]),
                                     reads=[PK(bank)], writes=[("yout", hb, t, half)])
                            else:
                                T.op("dve", lambda: nc.vector.tensor_copy(out=yout[hb][:, t, half * 512:(half + 1) * 512], in_=ps[bank][:]),
                                     reads=[PK(bank)], writes=[("yout", hb, t, half)])
                            ev += 1
                    T.op("pool", lambda: nc.gpsimd.dma_start(out=YS[k * 512:(k + 1) * 512, :].rearrange("(s p) d -> p s d", p=128), in_=yout[hb][:]),
                         reads=[("yout", hb, t, half) for t in range(4) for half in range(2)], writes=[("YS", k)], dsem=f"sty{hb}")

            YSKEYS = [("YS", k) for k in range(NTILE)]
            with contextlib.ExitStack() as st:
                l2g = sb("l2g", [128, D], F32, st)
                l2b = sb("l2b", [128, D], F32, st)
                x1t = [sb(f"x1t{i}", [128, D], F32, st) for i in range(2)]
                yg = [sb(f"yg{i}", [128, 2, D], F32, st) for i in range(2)]
                ff = [sb(f"ff{i}", [128, D], F32, st) for i in range(2)]
                yo = [sb(f"yo{i}", [128, D], F32, st) for i in range(2)]
                stats = sb("statsd", [128, 2, 6], F32, st)
                mv = sb("mvd", [128, 2], F32, st)
                rstd = sb("rstdd", [128, 1], F32, st)
                for dst, key, src in ((l2g, "l2g", ln2_g), (l2b, "l2b", ln2_b)):
                    T.op(DQ, lambda: DQE.dma_start(out=dst[:], in_=src[l].partition_broadcast(128)),
                         writes=[key], dsem=key)
                def e_loads(gt_i):
                    r0 = gt_i * 128
                    xb = gt_i % 2
                    T.op(DQ, lambda: DQE.dma_start(out=x1t[xb][:], in_=X1[r0:r0 + 128, :]),
                         reads=[("X1", gt_i // 4)], writes=[("x1t", xb)], dsem=f"x1t{xb}")
                    for j in range(2):
                        T.op("pool", lambda: nc.gpsimd.indirect_dma_start(
                            out=yg[xb][:, j, :], out_offset=None, in_=YS,
                            in_offset=bass.IndirectOffsetOnAxis(ap=posc_i[:, gt_i * 2 + j:gt_i * 2 + j + 1], axis=0),
                            bounds_check=reg_ys, oob_is_err=False),
                            reads=YSKEYS + ["posc"], writes=[("yg", xb)], dsem=f"yg{xb}", nowaw=True)

                e_loads(0)
                for gt_i in range(NT):
                    r0 = gt_i * 128
                    xb = gt_i % 2
                    if gt_i + 1 < NT:
                        e_loads(gt_i + 1)
                    A = [("ff", xb)]
                    T.op("act", lambda: nc.scalar.activation(out=ff[xb][:], in_=yg[xb][:, 0, :], func=AF.Identity, scale=tokmeta[:, gt_i, 4:5]),
                         reads=[("yg", xb), ("tokmeta", gt_i)], writes=A)
                    T.op("act", lambda: nc.scalar.activation(out=yg[xb][:, 1, :], in_=yg[xb][:, 1, :], func=AF.Identity, scale=tokmeta[:, gt_i, 5:6]),
                         reads=[("yg", xb), ("tokmeta", gt_i)], writes=[("yg", xb)])
                    T.op("dve", lambda: nc.vector.tensor_tensor(out=ff[xb][:], in0=ff[xb][:], in1=yg[xb][:, 1, :], op=ALU.add),
                         reads=[("yg", xb)] + A, writes=A)
                    T.op("dve", lambda: nc.vector.tensor_tensor(out=ff[xb][:], in0=ff[xb][:], in1=gt2p[:], op=ALU.mult),
                         reads=A + ["gt2p"], writes=A)
                    T.op("dve", lambda: nc.vector.scalar_tensor_tensor(out=ff[xb][:], in0=x1t[xb][:], scalar=ALPHA, in1=ff[xb][:],
                                                                       op0=ALU.mult, op1=ALU.add),
                         reads=A + [("x1t", xb)], writes=A)
                    for half in range(2):
                        T.op("dve", lambda: nc.vector.bn_stats(out=stats[:, half, :], in_=ff[xb][:, half * 512:(half + 1) * 512]),
                             reads=A, writes=["statsd"])
                    T.op("dve", lambda: nc.vector.bn_aggr(out=mv[:], in_=stats[:].rearrange("p a b -> p (a b)")),
                         reads=["statsd"], writes=["mvd"])
                    rsqrt_small(st, "rstdd", rstd[:], mv[:, 1:2], 1, ["mvd"], "rstdd")
                    T.op("dve", lambda: nc.vector.scalar_tensor_tensor(out=mv[:, 1:2], in0=mv[:, 0:1], scalar=-1.0, in1=rstd[:, 0:1],
                                                                       op0=ALU.mult, op1=ALU.mult),
                         reads=["mvd", "rstdd"], writes=["mvd"])
                    T.op("act", lambda: nc.scalar.activation(out=ff[xb][:], in_=ff[xb][:], func=AF.Identity,
                                                             scale=rstd[:, 0:1], bias=mv[:, 1:2]),
                         reads=A + ["mvd", "rstdd"], writes=A)
                    T.op("pool", lambda: nc.gpsimd.tensor_tensor(out=ff[xb][:], in0=ff[xb][:], in1=l2g[:], op=ALU.mult),
                         reads=A + ["l2g"], writes=A)
                    T.op("pool", lambda: nc.gpsimd.tensor_tensor(out=yo[xb][:], in0=ff[xb][:], in1=l2b[:], op=ALU.add),
                         reads=A + ["l2b"], writes=[("yo", xb)])
                    T.op(DQ, lambda: DQE.dma_start(out=xdst[r0:r0 + 128, :], in_=yo[xb][:]),
                         reads=[("yo", xb)], writes=[("Xt", l + 1, gt_i)], dsem=f"sto{xb}")
                for g in range(NG):
                    T.writers[("X", l + 1, g)] = {}
                    for lt4 in range(4):
                        for s_, v in T.writers.get(("Xt", l + 1, g * 4 + lt4), {}).items():
                            if T.writers[("X", l + 1, g)].get(s_, 0) < v:
                                T.writers[("X", l + 1, g)][s_] = v

        T.final_wait(DQ, [("Xt", L, i) for i in range(NT)])
        print(f"[build] ops={T.n_ops} waits={T.n_waits} sems={len(T.sem)}")
    return nc


def rope_tables(S):
    inv = 1.0 / (10000.0 ** (np.arange(0, 64, 2, dtype=np.float32) / 64.0))
    ang = np.arange(S, dtype=np.float32)[:, None] * inv[None, :].astype(np.float32)
    cos = np.cos(ang).astype(np.float32)
    sin = np.sin(ang).astype(np.float32)
    cosT = np.empty((128, S), np.float32)
    sinT = np.empty((128, S), np.float32)
    for p in range(128):
        d = p % 64
        cosT[p] = cos[:, d % 32]
        sinT[p] = -sin[:, d % 32] if d < 32 else sin[:, d % 32]
    return cosT, sinT


def prep_shared(inp, S, L):
    f = lambda a: np.ascontiguousarray(np.asarray(a, dtype=np.float32))
    w_in = np.asarray(inp["w_in"], dtype=np.float32)[:L]
    q = w_in[:, :, 0:512].reshape(L, D, 4, 2, 64)
    k = w_in[:, :, 512:1024].reshape(L, D, 4, 2, 64)
    swap = lambda t: np.concatenate([t[..., 32:], t[..., :32]], axis=-1).reshape(L, D, 512)
    w_in_x = np.concatenate([w_in[:, :, 0:512], swap(q), w_in[:, :, 512:1024], swap(k), w_in[:, :, 1024:2560]], axis=-1)
    lam4 = np.stack([inp["lambda_q1"], inp["lambda_k1"], inp["lambda_q2"], inp["lambda_k2"]], axis=1)[:L]
    w_r = np.concatenate([np.asarray(inp["w_group"])[:L], np.transpose(np.asarray(inp["w_router"])[:L], (0, 2, 1, 3)).reshape(L, D, 32)], axis=-1)
    b_r = np.concatenate([np.asarray(inp["b_group"])[:L], np.asarray(inp["b_router"])[:L].reshape(L, 32)], axis=-1)
    wg = np.asarray(inp["w_gate"], dtype=np.float32)[:L].reshape(L, NE, 8, 128, DFF).transpose(0, 1, 3, 2, 4).reshape(L, NE, 128, 8 * DFF)
    wu = np.asarray(inp["w_up"], dtype=np.float32)[:L].reshape(L, NE, 8, 128, DFF).transpose(0, 1, 3, 2, 4).reshape(L, NE, 128, 8 * DFF)
    wd = np.asarray(inp["w_down"], dtype=np.float32)[:L].reshape(L, NE, 4, 128, D).transpose(0, 1, 3, 2, 4).reshape(L, NE, 128, 4 * D)
    wall = np.ascontiguousarray(np.concatenate([wg, wu, wd], axis=-1).reshape(L, NE * 128, 12288))
    del wg, wu, wd
    cosT, sinT = rope_tables(S)
    tri = np.triu(np.ones((128, 128), np.float32))
    sh = {
        "w_ada": f(inp["w_ada"][:L]), "b_ada": f(inp["b_ada"][:L]), "w_in": f(w_in_x), "lam4": f(lam4),
        "subln_g": f(inp["subln_g"][:L]), "sg_ln_g": f(np.asarray(inp["sg_ln_g"])[:L].reshape(L, 512)),
        "sg_ln_b": f(np.asarray(inp["sg_ln_b"])[:L].reshape(L, 512)),
        "w_spT": f(np.transpose(np.asarray(inp["w_spatial"])[:L], (0, 1, 3, 2))),
        "b_sp": f(np.asarray(inp["b_spatial"])[:L].reshape(L, 512)),
        "w_out": f(inp["w_out"][:L]), "ln1_g": f(inp["ln1_g"][:L]), "ln1_b": f(inp["ln1_b"][:L]),
        "w_r": f(w_r), "b_r": f(b_r),
        "wall": wall, "triu1": np.triu(np.ones((128, 128), np.float32), 1),
        "pcol": np.stack([np.arange(128, dtype=np.float32), 6.0 * np.arange(128, dtype=np.float32)], axis=1),
        "iota32": np.ascontiguousarray(np.broadcast_to(np.arange(32, dtype=np.float32), (128, 32))),
        "kt512": np.ascontiguousarray(np.broadcast_to(512.0 * np.arange(64, dtype=np.float32), (128, 64))),
        "ln2_g": f(inp["ln2_g"][:L]), "ln2_b": f(inp["ln2_b"][:L]),
        "ident": np.eye(128, dtype=np.float32), "tri": tri, "csT": np.ascontiguousarray(np.stack([cosT, sinT], axis=1)),
    }
    return sh


def run(inp, S, L, n_cores, dbg=False, trace=False):
    nc = build_program(S, L, dbg=dbg)
    sh = prep_shared(inp, S, L)
    x = np.asarray(inp["x"], dtype=np.float32)
    c = np.asarray(inp["c"], dtype=np.float32)
    in_maps = []
    for b in range(n_cores):
        m = dict(sh)
        m["x"] = np.ascontiguousarray(x[b])
        m["cT"] = np.ascontiguousarray(c[b].reshape(8, 128).T)
        in_maps.append(m)
    res = run_bass_kernel_spmd(nc, in_maps, core_ids=list(range(n_cores)), trace=trace)
    return res


def kernel(**inputs):
    S = inputs["x"].shape[1]
    res = run(inputs, S, 2, 8)
    return np.stack([np.asarray(r["out"], dtype=np.float32) for r in res.results], axis=0)
```
